# Optimizing a Trainium2 kernel written in Bass

```python
import jax
import jax.numpy as jnp
from jax import lax
import numpy as np

D_MODEL = 2048
BATCH = 4
SEQ = 4096
DEPTH = 4

GRID_W = 64
Q_BLOCK = 128
ROPE_THETA = 10000.0
NORM_EPS = 1e-6
GN_EPS = 64e-5
N_MOD = 6
N_BRANCH = 4
BRANCH_W = D_MODEL // N_BRANCH
CONV_W = BRANCH_W
CONV_K = 3
GQA_HEAD_DIM = 64
GQA_HEADS = BRANCH_W // GQA_HEAD_DIM
GQA_KV_HEADS = GQA_HEADS // 4
RWKV_HEAD = 64
RWKV_W = BRANCH_W
RWKV_HEADS = RWKV_W // RWKV_HEAD
RWKV_DECAY_LORA = 64
RWKV_A_LORA = 64
RWKV_G_LORA = 128
MLA_V = 64
MLA_HEADS = BRANCH_W // MLA_V
MLA_NOPE = 64
MLA_ROPE = 32
MLA_Q_LORA = 3 * BRANCH_W // 4
MLA_KV_LORA = BRANCH_W // 2
N_EXPERTS = 16
EC_CAPACITY = 2
EXPERT_HIDDEN = D_MODEL // 2

CONV_IN = 3 * CONV_W
GQA_IN = (GQA_HEADS + 2 * GQA_KV_HEADS) * GQA_HEAD_DIM
RWKV_IN = 3 * RWKV_W + 2 * RWKV_DECAY_LORA + 2 * RWKV_A_LORA + RWKV_G_LORA
MLA_IN = MLA_Q_LORA + MLA_KV_LORA + MLA_ROPE
GATE_IN = N_BRANCH * D_MODEL
IN_SIZES = (CONV_IN, GQA_IN, RWKV_IN, MLA_IN, GATE_IN)
IN_WIDTH = CONV_IN + GQA_IN + RWKV_IN + MLA_IN + GATE_IN

kernel_name = 'hybrid_parallel_mixer_ec_moe_encoder'


def _split(x, sizes):
    cuts, acc = [], 0
    for sz in sizes[:-1]:
        acc += sz
        cuts.append(acc)
    return jnp.split(x, cuts, axis=-1)


def rms_norm(x, g):
    xf = x.astype(jnp.float32)
    y = xf * lax.rsqrt(jnp.mean(xf * xf, axis=-1, keepdims=True) + NORM_EPS)
    return (y * g.astype(jnp.float32)).astype(x.dtype)


def axial_rope_tables(seq, n_pairs):
    rows = seq // GRID_W
    row = jnp.repeat(jnp.arange(rows), GRID_W).astype(jnp.float32)
    col = (jnp.arange(rows * GRID_W) % GRID_W).astype(jnp.float32)
    n_axis = n_pairs // 2
    freqs = jnp.power(ROPE_THETA, -jnp.arange(n_axis, dtype=jnp.float32) / n_axis)
    ang = jnp.concatenate([row[:, None] * freqs, col[:, None] * freqs], axis=-1)
    return jnp.cos(ang), jnp.sin(ang)


def apply_rope(x, cos, sin):
    n = cos.shape[-1]
    c = cos[None, :, None, :].astype(x.dtype)
    s = sin[None, :, None, :].astype(x.dtype)
    x1, x2 = x[..., :n], x[..., n:]
    return jnp.concatenate([x1 * c - x2 * s, x1 * s + x2 * c], axis=-1)


def blocked_attention(q, k, v):
    b, s, hk, g, dq = q.shape
    scale = dq ** -0.5
    qb = jnp.moveaxis(q.reshape(b, s // Q_BLOCK, Q_BLOCK, hk, g, dq), 1, 0)

    def one_block(q_blk):
        sc = jnp.einsum('bqhgd,bkhd->bhgqk', q_blk, k, preferred_element_type=jnp.float32) * scale
        p = jax.nn.softmax(sc, axis=-1).astype(v.dtype)
        return jnp.einsum('bhgqk,bkhd->bqhgd', p, v)

    o = lax.map(one_block, qb)
    return jnp.moveaxis(o, 0, 1).reshape(b, s, hk, g, v.shape[-1])


def short_conv_mixer(slab, conv_w):
    b_gate, c_gate, u = jnp.split(slab, 3, axis=-1)
    z = c_gate * u
    y = lax.conv_general_dilated(z, conv_w[:, None, :].astype(z.dtype), window_strides=(1,),
                                 padding=((CONV_K // 2, CONV_K // 2),),
                                 dimension_numbers=('NWC', 'WIO', 'NWC'), feature_group_count=CONV_W)
    return b_gate * y


def gqa_mixer(slab, q_norm, k_norm, cos, sin):
    b, s, _ = slab.shape
    q, k, v = _split(slab, (GQA_HEADS * GQA_HEAD_DIM, GQA_KV_HEADS * GQA_HEAD_DIM, GQA_KV_HEADS * GQA_HEAD_DIM))
    q = apply_rope(rms_norm(q.reshape(b, s, GQA_HEADS, GQA_HEAD_DIM), q_norm), cos, sin)
    k = apply_rope(rms_norm(k.reshape(b, s, GQA_KV_HEADS, GQA_HEAD_DIM), k_norm), cos, sin)
    v = v.reshape(b, s, GQA_KV_HEADS, GQA_HEAD_DIM)
    q = q.reshape(b, s, GQA_KV_HEADS, GQA_HEADS // GQA_KV_HEADS, GQA_HEAD_DIM)
    return blocked_attention(q, k, v).reshape(b, s, GQA_HEADS * GQA_HEAD_DIM)


def mla_mixer(slab, qc_norm, kvc_norm, w_uq, w_ukv, q_norm, k_norm, cos, sin):
    b, s, _ = slab.shape
    q_c, kv_c, k_pe = _split(slab, (MLA_Q_LORA, MLA_KV_LORA, MLA_ROPE))
    q = (rms_norm(q_c, qc_norm) @ w_uq).reshape(b, s, MLA_HEADS, MLA_NOPE + MLA_ROPE)
    kv = (rms_norm(kv_c, kvc_norm) @ w_ukv).reshape(b, s, MLA_HEADS, MLA_NOPE + MLA_V)
    k_nope, v = kv[..., :MLA_NOPE], kv[..., MLA_NOPE:]
    k_pe = jnp.broadcast_to(k_pe[:, :, None, :], (b, s, MLA_HEADS, MLA_ROPE))
    q = rms_norm(q, q_norm)
    k = rms_norm(jnp.concatenate([k_nope, k_pe], axis=-1), k_norm)
    q = jnp.concatenate([q[..., :MLA_NOPE], apply_rope(q[..., MLA_NOPE:], cos, sin)], axis=-1)
    k = jnp.concatenate([k[..., :MLA_NOPE], apply_rope(k[..., MLA_NOPE:], cos, sin)], axis=-1)
    o = blocked_attention(q[:, :, :, None, :], k, v)
    return o.reshape(b, s, MLA_HEADS * MLA_V)


def _dir_time_major(t):
    t = jnp.transpose(t, (1, 2, 0, 3, 4))
    return jnp.stack([t[:, 0], jnp.flip(t[:, 1], axis=0)], axis=1)


def _dir_batch_major(t):
    t = jnp.stack([t[:, 0], jnp.flip(t[:, 1], axis=0)], axis=1)
    return jnp.transpose(t, (2, 0, 1, 3, 4))


def _rwkv7_step(state, inp):
    r, w, kk, bb, v, k = inp
    sa = jnp.einsum('dbhij,dbhj->dbhi', state, -kk)
    state = state * w[..., None, :] + sa[..., :, None] * bb[..., None, :] + v[..., :, None] * k[..., None, :]
    return state, jnp.einsum('dbhij,dbhj->dbhi', state, r)


def rwkv7_mixer(slab, mu, w0, w2, a0, a2, g2, k_k, k_a, r_k, ln_w, ln_b):
    b, s, _ = slab.shape
    f32 = jnp.float32
    H, N = RWKV_HEADS, RWKV_HEAD
    prev = jnp.pad(slab, ((0, 0), (1, 0), (0, 0)))[:, :s]
    nxt = jnp.pad(slab, ((0, 0), (0, 1), (0, 0)))[:, 1:]
    z = slab + mu[0] * (prev - slab) + mu[1] * (nxt - slab)
    r, k, v, wd, ad, gd = _split(z, (RWKV_W, RWKV_W, RWKV_W, 2 * RWKV_DECAY_LORA, 2 * RWKV_A_LORA, RWKV_G_LORA))
    wd = wd.reshape(b, s, 2, RWKV_DECAY_LORA)
    ad = ad.reshape(b, s, 2, RWKV_A_LORA)
    w_log = -jax.nn.softplus(-(w0 + jnp.einsum('bsdl,dlc->bsdc', jnp.tanh(wd), w2))) - 0.5
    decay = jnp.exp(-jnp.exp(w_log.astype(f32)))
    a = jax.nn.sigmoid(a0 + jnp.einsum('bsdl,dlc->bsdc', ad, a2)).astype(f32)
    g = jax.nn.sigmoid(gd) @ g2
    rf, kf, vf = r.astype(f32), k.astype(f32), v.astype(f32)
    kk = (kf * k_k.astype(f32)).reshape(b, s, H, N)
    kk = kk / jnp.maximum(jnp.sqrt(jnp.sum(kk * kk, axis=-1, keepdims=True)), 1e-12)
    k_dir = kf[:, :, None, :] * (1.0 + (a - 1.0) * k_a.astype(f32))

    def heads(t):
        return t.reshape(b, s, 2, H, N)

    def both(t):
        return jnp.broadcast_to(t.reshape(b, s, 1, H, N), (b, s, 2, H, N))

    kk2 = both(kk)
    xs = (_dir_time_major(both(rf)), _dir_time_major(heads(decay)), _dir_time_major(kk2),
          _dir_time_major(heads(a) * kk2), _dir_time_major(both(vf)), _dir_time_major(heads(k_dir)))
    state0 = jnp.zeros((2, b, H, N, N), f32)
    _, ys = lax.scan(_rwkv7_step, state0, xs)
    y = jnp.sum(_dir_batch_major(ys), axis=2)
    mean = jnp.mean(y, axis=-1, keepdims=True)
    var = jnp.mean(jnp.square(y - mean), axis=-1, keepdims=True)
    y = ((y - mean) * lax.rsqrt(var + GN_EPS)).reshape(b, s, RWKV_W) * ln_w.astype(f32) + ln_b.astype(f32)
    bonus = jnp.sum(rf.reshape(b, s, 1, H, N) * heads(k_dir) * r_k.astype(f32), axis=(2, 4))[..., None] * vf.reshape(b, s, H, N)
    return ((y + bonus.reshape(b, s, RWKV_W)) * g.astype(f32)).astype(slab.dtype)


def expert_choice_moe(h, w_router, w1, w3, w2):
    b, s, d = h.shape
    cap = EC_CAPACITY * s // N_EXPERTS
    aff = jax.nn.softmax((h @ w_router).astype(jnp.float32), axis=-1)
    gate_vals, tok_idx = lax.top_k(jnp.swapaxes(aff, 1, 2), cap)
    xg = jax.vmap(lambda hb, ib: hb[ib])(h, tok_idx)
    hid = jax.nn.silu(jnp.einsum('becd,edf->becf', xg, w1)) * jnp.einsum('becd,edf->becf', xg, w3)
    out = jnp.einsum('becf,efd->becd', hid, w2) * gate_vals[..., None].astype(h.dtype)
    return jax.vmap(lambda ib, ob: jnp.zeros((s, d), ob.dtype).at[ib.reshape(-1)].add(ob.reshape(-1, d)))(tok_idx, out)


def setup_inputs(seed: int = 0) -> dict:
    key = jax.random.key(seed)
    ks = iter(jax.random.split(key, 40))
    f32 = jnp.float32
    L, D = DEPTH, D_MODEL

    def nrm(shape, scale):
        return jax.random.normal(next(ks), shape, f32) * scale

    def gain(shape, scale=0.02):
        return 1.0 + scale * jax.random.normal(next(ks), shape, f32)

    return {
        'x': nrm((BATCH, SEQ, D), 1.0),
        'c': nrm((BATCH, D), 1.0),
        'w_mod': nrm((D, N_MOD * D), 0.5 * D ** -0.5),
        'mod_table': nrm((L, N_MOD, D), 0.1),
        'norm1_g': gain((L, D)),
        'norm2_g': gain((L, D)),
        'w_in': nrm((L, D, IN_WIDTH), D ** -0.5),
        'conv_w': nrm((L, CONV_K, CONV_W), CONV_K ** -0.5),
        'gqa_q_norm': gain((L, GQA_HEAD_DIM)),
        'gqa_k_norm': gain((L, GQA_HEAD_DIM)),
        'rwkv_mu': jax.random.uniform(next(ks), (L, 2, RWKV_IN), f32, 0.0, 0.5),
        'rwkv_w0': jax.random.uniform(next(ks), (L, 2, RWKV_W), f32, -6.0, 1.0),
        'rwkv_w2': nrm((L, 2, RWKV_DECAY_LORA, RWKV_W), 0.1 * RWKV_DECAY_LORA ** -0.5),
        'rwkv_a0': nrm((L, 2, RWKV_W), 0.5),
        'rwkv_a2': nrm((L, 2, RWKV_A_LORA, RWKV_W), 0.1 * RWKV_A_LORA ** -0.5),
        'rwkv_g2': nrm((L, RWKV_G_LORA, RWKV_W), RWKV_G_LORA ** -0.5),
        'rwkv_k_k': gain((L, RWKV_W), 0.1),
        'rwkv_k_a': gain((L, RWKV_W), 0.1),
        'rwkv_r_k': nrm((L, RWKV_HEADS, RWKV_HEAD), 0.1),
        'rwkv_ln_w': gain((L, RWKV_W)),
        'rwkv_ln_b': nrm((L, RWKV_W), 0.01),
        'mla_qc_norm': gain((L, MLA_Q_LORA)),
        'mla_kvc_norm': gain((L, MLA_KV_LORA)),
        'mla_w_uq': nrm((L, MLA_Q_LORA, MLA_HEADS * (MLA_NOPE + MLA_ROPE)), MLA_Q_LORA ** -0.5),
        'mla_w_ukv': nrm((L, MLA_KV_LORA, MLA_HEADS * (MLA_NOPE + MLA_V)), MLA_KV_LORA ** -0.5),
        'mla_q_norm': gain((L, MLA_NOPE + MLA_ROPE)),
        'mla_k_norm': gain((L, MLA_NOPE + MLA_ROPE)),
        'w_branch': nrm((L, N_BRANCH, BRANCH_W, D), BRANCH_W ** -0.5),
        'w_out': nrm((L, D, D), D ** -0.5),
        'w_router': nrm((L, D, N_EXPERTS), D ** -0.5),
        'moe_w1': nrm((L, N_EXPERTS, D, EXPERT_HIDDEN), D ** -0.5),
        'moe_w3': nrm((L, N_EXPERTS, D, EXPERT_HIDDEN), D ** -0.5),
        'moe_w2': nrm((L, N_EXPERTS, EXPERT_HIDDEN, D), EXPERT_HIDDEN ** -0.5),
    }


def reference(x, c, w_mod, mod_table, norm1_g, norm2_g, w_in, conv_w, gqa_q_norm, gqa_k_norm,
              rwkv_mu, rwkv_w0, rwkv_w2, rwkv_a0, rwkv_a2, rwkv_g2, rwkv_k_k, rwkv_k_a, rwkv_r_k,
              rwkv_ln_w, rwkv_ln_b, mla_qc_norm, mla_kvc_norm, mla_w_uq, mla_w_ukv, mla_q_norm,
              mla_k_norm, w_branch, w_out, w_router, moe_w1, moe_w3, moe_w2):
    b, s, d = x.shape
    cos_g, sin_g = axial_rope_tables(s, GQA_HEAD_DIM // 2)
    cos_m, sin_m = axial_rope_tables(s, MLA_ROPE // 2)
    mod = (jax.nn.silu(c) @ w_mod).reshape(b, N_MOD, d)
    for l in range(DEPTH):
        m = mod + mod_table[l]
        shift1, scale1, gate1, shift2, scale2, gate2 = (m[:, i, None, :] for i in range(N_MOD))
        h = rms_norm(x, norm1_g[l]) * (1.0 + scale1) + shift1
        s_conv, s_gqa, s_rwkv, s_mla, s_gate = _split(h @ w_in[l], IN_SIZES)
        branches = (
            short_conv_mixer(s_conv, conv_w[l]),
            gqa_mixer(s_gqa, gqa_q_norm[l], gqa_k_norm[l], cos_g, sin_g),
            rwkv7_mixer(s_rwkv, rwkv_mu[l], rwkv_w0[l], rwkv_w2[l], rwkv_a0[l], rwkv_a2[l], rwkv_g2[l],
                        rwkv_k_k[l], rwkv_k_a[l], rwkv_r_k[l], rwkv_ln_w[l], rwkv_ln_b[l]),
            mla_mixer(s_mla, mla_qc_norm[l], mla_kvc_norm[l], mla_w_uq[l], mla_w_ukv[l],
                      mla_q_norm[l], mla_k_norm[l], cos_m, sin_m),
        )
        gates = jax.nn.sigmoid(s_gate).reshape(b, s, N_BRANCH, d)
        merged = sum(gates[:, :, i] * (branches[i] @ w_branch[l, i]) for i in range(N_BRANCH))
        x = x + gate1 * (merged @ w_out[l])
        h2 = rms_norm(x, norm2_g[l]) * (1.0 + scale2) + shift2
        x = x + gate2 * expert_choice_moe(h2, w_router[l], moe_w1[l], moe_w3[l], moe_w2[l])
    return x
```

```python
import contextlib
import numpy as np
import concourse.bass as bass
import concourse.mybir as mybir
from concourse.bass_utils import run_bass_kernel_spmd

F32 = mybir.dt.float32
BF16 = mybir.dt.bfloat16
ALU = mybir.AluOpType
AF = mybir.ActivationFunctionType
AX = mybir.AxisListType

D = 2048
NKC = D // 128
NORM_EPS = 1e-6
GN_EPS = 64e-5
IN_WIDTH = 13088
C_CONV, C_GQA, C_RWKV, C_MLA, C_GATE = 0, 1536, 2304, 4224, 4896
NE = 16
EH = 1024
CH = 64

NDMA_SEMS = 12


class Prog:
    ENGS = ("pe", "act", "dve", "pool", "sp")

    def __init__(self, nc):
        self.nc = nc
        self.ops = {e: [] for e in self.ENGS}
        self.cnt = {e: 0 for e in self.ENGS}
        self.seen = {e: {} for e in self.ENGS}
        self.state = {}
        self.dma_n = {e: 0 for e in self.ENGS}
        self.dma_exp = {}
        self.pending = {e: [] for e in self.ENGS}
        self.nops = 0

    def _deps(self, reads, writes):
        toks = []
        for k in reads:
            st = self.state.get(k)
            if st and st[0] is not None:
                toks.append(st[0])
        for k in writes:
            st = self.state.get(k)
            if st:
                if st[0] is not None:
                    toks.append(st[0])
                toks.extend(st[1])
        return toks

    def _commit(self, tok, reads, writes):
        for k in reads:
            st = self.state.setdefault(k, [None, []])
            st[1].append(tok)
            if len(st[1]) > 64:
                st[1] = st[1][-64:] if False else self._compress(st[1])
        for k in writes:
            self.state[k] = [tok, []]

    @staticmethod
    def _compress(toks):
        best = {}
        for t in toks:
            sid = t[:3]
            if sid not in best or best[sid][3] < t[3]:
                best[sid] = t
        return list(best.values())

    def _waits(self, eng, toks, same_engine_ok=False):
        need = {}
        for t in toks:
            kind, e, idx, val = t
            if kind == "c" and e == eng and same_engine_ok:
                continue
            sid = (kind, e, idx)
            if need.get(sid, 0) < val:
                need[sid] = val
        out = []
        for sid, val in need.items():
            if self.seen[eng].get(sid, 0) >= val:
                continue
            self.seen[eng][sid] = val
            out.append((sid, val))
        return out

    def op(self, eng, fn, reads=(), writes=()):
        toks = self._deps(reads, writes) + self.pending[eng]
        self.pending[eng] = []
        waits = self._waits(eng, toks, same_engine_ok=(eng == "pe"))
        self.cnt[eng] += 1
        tok = ("c", eng, 0, self.cnt[eng])
        self.ops[eng].append((waits, fn, ("c", eng, 0)))
        self._commit(tok, reads, writes)
        self.nops += 1

    def dma(self, eng, fn, reads=(), writes=()):
        toks = self._deps(reads, writes) + self.pending[eng]
        self.pending[eng] = []
        i = self.dma_n[eng]
        self.dma_n[eng] += 1
        slot = i % NDMA_SEMS
        gen = i // NDMA_SEMS
        if gen > 0:
            toks = toks + [("d", eng, slot, 16 * gen)]
        waits = self._waits(eng, toks)
        tok = ("d", eng, slot, 16 * (gen + 1))
        self.dma_exp[(eng, slot)] = 16 * (gen + 1)
        self.ops[eng].append((waits, fn, ("d", eng, slot)))
        self._commit(tok, reads, writes)
        self.nops += 1

    def barrier(self):
        toks = [("c", e, 0, self.cnt[e]) for e in self.ENGS if self.cnt[e] > 0]
        toks += [("d", e, s, v) for (e, s), v in self.dma_exp.items()]
        for e in self.ENGS:
            self.pending[e] = list(toks)
        self.state = {}

    def emit(self):
        nc = self.nc
        self.barrier()
        fin_waits = self._waits("sp", self.pending["sp"])
        with contextlib.ExitStack() as es:
            sems = {}
            for e in self.ENGS:
                sems[("c", e, 0)] = es.enter_context(nc.semaphore("c_" + e))
            for e in ("sp", "act", "pool"):
                for s in range(NDMA_SEMS):
                    sems[("d", e, s)] = es.enter_context(nc.semaphore("d_%s_%d" % (e, s)))
            block = es.enter_context(nc.Block())

            def run(engname, engobj):
                for waits, fn, inc in self.ops[engname]:
                    for sid, val in waits:
                        engobj.wait_ge(sems[sid], val)
                    ins = fn(engobj)
                    ins.then_inc(sems[inc], 16 if inc[0] == "d" else 1)
                if engname == "sp":
                    for sid, val in fin_waits:
                        engobj.wait_ge(sems[sid], val)

            @block.sync
            def _(e):
                run("sp", e)

            @block.tensor
            def _(e):
                run("pe", e)

            @block.scalar
            def _(e):
                run("act", e)

            @block.vector
            def _(e):
                run("dve", e)

            @block.gpsimd
            def _(e):
                run("pool", e)


class Cfg:
    def __init__(self, S=4096, L=4, phases=None, ext=()):
        self.S = S
        self.L = L
        self.phases = phases
        self.ext = set(ext)
        self.cap = 2 * S // NE


class Ctx:
    pass


def build(cfg):
    nc = bass.Bass("TRN2", target_bir_lowering=False)
    K = Ctx()
    K.nc, K.cfg = nc, cfg
    K.P = Prog(nc)
    S, L = cfg.S, cfg.L
    K.S, K.L = S, L

    def din(name, shape, dt=F32):
        return nc.dram_tensor(name, list(shape), dt, kind="ExternalInput").ap()

    def dscr(name, shape, dt=F32):
        kind = "Internal"
        if name in cfg.ext:
            kind = "ExternalOutput"
        if ("in:" + name) in cfg.ext:
            kind = "ExternalInput"
        return nc.dram_tensor(name, list(shape), dt, kind=kind).ap()

    shapes = {
        "x": [S, D], "c": [D], "w_mod": [D, 6 * D], "mod_table": [L, 6 * D], "norm1_g": [L, D], "norm2_g": [L, D],
        "w_in": [L, D, IN_WIDTH], "conv_w": [L, 3, 512], "gqa_q_norm": [L, 64], "gqa_k_norm": [L, 64],
        "rwkv_mu": [L, 2, 1920], "rwkv_w0": [L, 2, 512], "rwkv_w2": [L, 128, 512], "rwkv_a0": [L, 2, 512],
        "rwkv_a2": [L, 128, 512], "rwkv_g2": [L, 128, 512], "rwkv_k_k": [L, 512], "rwkv_k_a": [L, 512],
        "rwkv_r_k": [L, 512], "rwkv_ln_w": [L, 512], "rwkv_ln_b": [L, 512], "mla_qc_norm": [L, 384],
        "mla_kvc_norm": [L, 256], "mla_w_uq": [L, 384, 768], "mla_w_ukv": [L, 256, 1024], "mla_q_norm": [L, 96],
        "mla_k_norm": [L, 96], "w_branch": [L, 4 * 512, D], "w_out": [L, D, D], "w_router": [L, D, NE],
        "moe_w1": [L, NE, D, EH], "moe_w3": [L, NE, D, EH], "moe_w2": [L, NE, EH, D],
        "rope_g": [S, 64], "rope_m": [S, 32],
    }

    class LazyIn(dict):
        def __missing__(self, name):
            v = din(name, shapes[name])
            self[name] = v
            return v
    I = LazyIn()
    K.I = I
    K.out = nc.dram_tensor("out", [S, D], F32, kind="ExternalOutput").ap()

    Sc = {}
    Sc["modv"] = dscr("modv", [L * 6, D])
    Sc["fm"] = dscr("fm", [1536 + 1920, S])
    Sc["gt"] = dscr("gt", [4 * D, S], BF16)
    Sc["tm"] = dscr("tm", [S, 768 + 672])
    Sc["br"] = dscr("br", [4 * 512, S], BF16)
    Sc["gqT"] = dscr("gqT", [640, S], BF16)
    Sc["gv"] = dscr("gv", [S, 2, 65], BF16)
    Sc["mT"] = dscr("mT", [D, S], BF16)
    Sc["h2"] = dscr("h2", [S, D], BF16)
    Sc["aff"] = dscr("aff", [S, NE])
    Sc["affT"] = dscr("affT", [NE, S])
    Sc["rank"] = dscr("rank", [S, NE])
    Sc["rankT"] = dscr("rankT", [NE, S])
    Sc["ybuf"] = dscr("ybuf", [NE, S // 8, D], BF16)
    Sc["rg"] = dscr("rg", [512, S])
    Sc["rbon"] = dscr("rbon", [512, S])
    Sc["rv"] = dscr("rv", [S, 512])
    Sc["rgc"] = [dscr("rgc%d" % d_, [512, S // CH]) for d_ in range(2)]
    Sc["rf"] = [[dscr("rf%d_%d" % (d_, i_), [512, S]) for i_ in range(4)] for d_ in range(2)]
    Sc["rt"] = [[dscr("rt%d_%d" % (d_, i_), [S, 512]) for i_ in range(2)] for d_ in range(2)]
    Sc["ry"] = [dscr("ry%d" % d_, [S, 512]) for d_ in range(2)]
    Sc["mqT"] = dscr("mqT", [8 * 96, S], BF16)
    Sc["mkT"] = dscr("mkT", [8 * 96, S], BF16)
    Sc["mv"] = dscr("mv", [S, 8, 65], BF16)
    K.Sc = Sc
    K.dscr = dscr

    K.xres = K.out
    with contextlib.ExitStack() as glob:
        K.glob = glob
        setup_consts(K)
        ph = cfg.phases
        if ph is None or "copyx" in ph:
            xin = K.I["x"]
            for t in range(0, S, 512):
                n = min(512, S - t)
                K.P.dma("sp", lambda e, t=t, n=n: e.dma_start(out=K.out[t:t + n, :], in_=xin[t:t + n, :]), writes=["xres"])
            K.P.barrier()
        if ph is None or "p0" in ph:
            phase0(K)
        for l in range(L):
            if ph is None or "p1" in ph:
                phase1(K, l)
            if ph is None or "conv" in ph:
                phase_conv(K, l)
            if ph is None or "gqa" in ph:
                phase_gqa_prep(K, l)
                phase_gqa_attn(K, l)
            if ph is None or "mla" in ph or "mla_prep" in ph:
                phase_mla_prep(K, l)
            if ph is None or "mla" in ph or "mla_attn" in ph:
                phase_mla_attn(K, l)
            if ph is None or "rwkv" in ph or "rwkv_prep" in ph:
                phase_rwkv_prep(K, l)
            if ph is None or "rwkv" in ph or "rwkv_scan" in ph:
                phase_rwkv_scan(K, l)
            if ph is None or "rwkv" in ph or "rwkv_post" in ph:
                phase_rwkv_post(K, l)
            if ph is None or "p3a" in ph:
                phase3a(K, l)
            if ph is None or "p3b" in ph:
                phase3b(K, l)
            if ph is None or "moe" in ph:
                phase_moe(K, l)
        K.P.emit()
    return nc


class Phase:
    def __init__(self, K):
        self.K = K
        self.es = contextlib.ExitStack()

    def __enter__(self):
        self.es.__enter__()
        return self

    def __exit__(self, *a):
        self.K.P.barrier()
        return self.es.__exit__(*a)

    _uid = [0]

    def sb(self, name, shape, dt=F32):
        Phase._uid[0] += 1
        return self.es.enter_context(self.K.nc.sbuf_tensor("%s_u%d" % (name, Phase._uid[0]), list(shape), dt))

    def ps(self, name, shape, dt=F32):
        Phase._uid[0] += 1
        return self.es.enter_context(self.K.nc.psum_tensor("%s_u%d" % (name, Phase._uid[0]), list(shape), dt))


def setup_consts(K):
    nc, P = K.nc, K.P
    g = K.glob

    def sb(name, shape, dt=F32):
        return g.enter_context(nc.sbuf_tensor(name, list(shape), dt))
    io = sb("c_io", [128, 512])
    iop = sb("c_iop", [128, 1])
    K.ident = sb("c_ident", [128, 128])
    K.identb = sb("c_identb", [128, 128], BF16)
    P.op("pool", lambda e: e.iota(io[:], pattern=[[1, 512]], base=0, channel_multiplier=0,
                                  allow_small_or_imprecise_dtypes=True), writes=["c_io"])
    P.op("pool", lambda e: e.iota(iop[:], pattern=[[0, 1]], base=0, channel_multiplier=1,
                                  allow_small_or_imprecise_dtypes=True), writes=["c_iop"])
    P.op("dve", lambda e: e.tensor_scalar(out=K.ident[:], in0=io[:, 0:128], scalar1=iop[:, 0:1], scalar2=None,
                                          op0=ALU.is_equal), reads=["c_io", "c_iop"], writes=["c_ident"])
    P.op("dve", lambda e: e.tensor_copy(out=K.identb[:], in_=K.ident[:]), reads=["c_ident"], writes=["c_identb"])
    K.io, K.iop = io, iop


def phase0(K):
    nc, P, I, L = K.nc, K.P, K.I, K.L
    with Phase(K) as ph:
        cT = ph.sb("cT", [128, NKC])
        sc = ph.sb("sc", [128, NKC])
        modrow = ph.sb("modrow", [1, 6 * D])
        P.dma("sp", lambda e: e.dma_start(out=cT[:], in_=I["c"].rearrange("(k p) -> p k", p=128),
                                          allow_slow_non_contiguous=True), writes=["cT"])
        P.op("act", lambda e: e.activation(out=sc[:], in_=cT[:], func=AF.Silu), reads=["cT"], writes=["sc"])
        wm = [ph.sb("wm%d" % i, [128, NKC, 512]) for i in range(2)]
        pm = [ph.ps("pm%d" % i, [1, 512]) for i in range(2)]
        wv = I["w_mod"].rearrange("(k p) n -> p k n", p=128)
        for ci in range(6 * D // 512):
            b = ci % 2
            P.dma("sp", lambda e, b=b, ci=ci: e.dma_start(out=wm[b][:], in_=wv[:, :, ci * 512:(ci + 1) * 512]),
                  writes=["wm%d" % b])
            for kc in range(NKC):
                P.op("pe", lambda e, b=b, kc=kc: e.matmul(pm[b][:], lhsT=sc[:, kc:kc + 1], rhs=wm[b][:, kc, :],
                                                          start=(kc == 0), stop=(kc == NKC - 1)),
                     reads=["sc", "wm%d" % b], writes=["pm%d" % b])
            P.op("act", lambda e, b=b, ci=ci: e.copy(out=modrow[:, ci * 512:(ci + 1) * 512], in_=pm[b][:]),
                 reads=["pm%d" % b], writes=["modrow"])
        tb = ph.sb("tb", [1, 6 * D])
        g12 = ph.sb("g12", [1, 2 * D])
        for l in range(L):
            P.dma("sp", lambda e, l=l: e.dma_start(out=tb[:], in_=I["mod_table"][l:l + 1, :]), writes=["tb"])
            P.dma("sp", lambda e, l=l: e.dma_start(out=g12[:, 0:D], in_=I["norm1_g"][l:l + 1, :]), writes=["g1"])
            P.dma("sp", lambda e, l=l: e.dma_start(out=g12[:, D:2 * D], in_=I["norm2_g"][l:l + 1, :]), writes=["g2"])
            P.op("dve", lambda e: e.tensor_tensor(out=tb[:], in0=tb[:], in1=modrow[:], op=ALU.add),
                 reads=["tb", "modrow"], writes=["tb"])
            for sub in range(2):
                o = sub * 3 * D
                P.op("dve", lambda e, o=o, sub=sub: e.scalar_tensor_tensor(
                    out=tb[:, o + D:o + 2 * D], in0=tb[:, o + D:o + 2 * D], scalar=1.0, in1=g12[:, sub * D:(sub + 1) * D],
                    op0=ALU.add, op1=ALU.mult), reads=["tb", "g1", "g2"], writes=["tb"])
            P.dma("sp", lambda e, l=l: e.dma_start(out=K.Sc["modv"][l * 6:(l + 1) * 6, :].rearrange("a d -> (a d)").unsqueeze(0),
                                                   in_=tb[:]), reads=["tb"], writes=["modv"])


def rms_rstd(P, ph, eng_sq, x_ap, junk_ap, ss, rstd, n, eps, keys_r, tag):
    P.op("act", lambda e: e.activation(out=junk_ap, in_=x_ap, func=AF.Square, accum_out=ss[:, 0:1]),
         reads=keys_r, writes=[tag + "_junk", tag + "_ss"])
    P.op("act", lambda e: e.activation(out=rstd[:, 0:1], in_=ss[:, 0:1], func=AF.Sqrt, scale=1.0 / n, bias=K_EPS[eps][:, 0:1]),
         reads=[tag + "_ss"], writes=[tag + "_rstd"])
    P.op("dve", lambda e: e.reciprocal(out=rstd[:, 0:1], in_=rstd[:, 0:1]), reads=[tag + "_rstd"], writes=[tag + "_rstd"])


K_EPS = {}

IN_CHUNKS = []


def _mk_chunks():
    out = []
    for c0 in range(C_CONV, C_GQA, 512):
        out.append((c0, 512, "fm", c0 - C_CONV))
    out.append((C_GQA, 512, "tm", 0))
    out.append((C_GQA + 512, 256, "tm", 512))
    c0 = C_RWKV
    while c0 < C_MLA:
        n = min(512, C_MLA - c0)
        out.append((c0, n, "fm", 1536 + c0 - C_RWKV))
        c0 += n
    out.append((C_MLA, 512, "tm", 768))
    out.append((C_MLA + 512, 160, "tm", 768 + 512))
    for c0 in range(C_GATE, IN_WIDTH, 512):
        out.append((c0, 512, "gt", c0 - C_GATE))
    return out


IN_CHUNKS = _mk_chunks()


def load_bcast_row(K, ph, name, row_ap, n):
    t = ph.sb(name, [128, n])
    K.P.dma("sp", lambda e: e.dma_start(out=t[:], in_=row_ap.to_broadcast([128, n])), reads=["modv"], writes=[name])
    return t


def norm_to_hT(K, ph, l, sub, x_dram, tok0, ntok, hT, A_b, B_b, bufs, h_keep=None):
    P = K.P
    xt, hb, junk, ss, rstd, pt = bufs
    for tt in range(ntok // 128):
        t0 = tok0 + tt * 128
        b = tt % 2
        P.dma("sp", lambda e, b=b, t0=t0: e.dma_start(out=xt[b][:], in_=x_dram[t0:t0 + 128, :]),
              reads=["xdram"], writes=["xt%d" % b])
        P.op("act", lambda e, b=b: e.activation(out=junk[:], in_=xt[b][:], func=AF.Square, accum_out=ss[:, 0:1]),
             reads=["xt%d" % b], writes=["junk", "ss"])
        P.op("act", lambda e: e.activation(out=rstd[:, 0:1], in_=ss[:, 0:1], func=AF.Sqrt, scale=1.0 / D,
                                           bias=K.eps_norm[:, 0:1]), reads=["ss"], writes=["rstd"])
        P.op("dve", lambda e: e.reciprocal(out=rstd[:, 0:1], in_=rstd[:, 0:1]), reads=["rstd"], writes=["rstd"])
        P.op("dve", lambda e, b=b: e.scalar_tensor_tensor(out=xt[b][:], in0=xt[b][:], scalar=rstd[:, 0:1], in1=A_b[:],
                                                          op0=ALU.mult, op1=ALU.mult),
             reads=["xt%d" % b, "rstd", "A_b"], writes=["xt%d" % b])
        P.op("pool", lambda e, b=b: e.tensor_tensor(out=hb[b][:], in0=xt[b][:], in1=B_b[:], op=ALU.add),
             reads=["xt%d" % b, "B_b"], writes=["hb%d" % b])
        if h_keep is not None:
            h_keep(tt, t0, xt[b], hb[b], "xt%d" % b, "hb%d" % b)
        for g4 in range(NKC // 4):
            pb = g4 % 2
            for j in range(4):
                kc = g4 * 4 + j
                P.op("pe", lambda e, b=b, pb=pb, j=j, kc=kc: e.transpose(pt[pb][:, j * 128:(j + 1) * 128],
                                                                         hb[b][:, kc * 128:(kc + 1) * 128], K.identb[:]),
                     reads=["hb%d" % b, "c_identb"], writes=["pt%d" % pb])
            P.op("act", lambda e, pb=pb, g4=g4, tt=tt: e.copy(
                out=hT[:, g4 * 4:(g4 + 1) * 4, tt * 128:(tt + 1) * 128],
                in_=pt[pb][:].rearrange("p (j t) -> p j t", j=4)),
                reads=["pt%d" % pb], writes=["hT"])


def phase1(K, l):
    nc, P, I, S = K.nc, K.P, K.I, K.S
    TS = min(1024, S)
    NT = min(512, TS)
    xd = K.out if (l > 0 or K.cfg.phases is None or "copyx" in K.cfg.phases) else I["x"]
    with Phase(K) as ph:
        K.eps_norm = ph.sb("eps_norm", [128, 1])
        P.op("dve", lambda e: e.memset(K.eps_norm[:], NORM_EPS), writes=["eps_norm"])
        A_b = load_bcast_row(K, ph, "A_b", K.Sc["modv"][l * 6 + 1:l * 6 + 2, :], D)
        B_b = load_bcast_row(K, ph, "B_b", K.Sc["modv"][l * 6 + 0:l * 6 + 1, :], D)
        hT = ph.sb("hT", [128, NKC, TS], BF16)
        xt = [ph.sb("xt%d" % i, [128, D]) for i in range(2)]
        hb = [ph.sb("hb%d" % i, [128, D], BF16) for i in range(2)]
        junk = ph.sb("junk", [128, D], BF16)
        ss = ph.sb("ss", [128, 1])
        rstd = ph.sb("rstd", [128, 1])
        pt = [ph.ps("pt%d" % i, [128, 512], BF16) for i in range(2)]
        wf = [ph.sb("wf%d" % i, [128, NKC, 512]) for i in range(2)]
        wb = [ph.sb("wb%d" % i, [128, NKC, 512], BF16) for i in range(2)]
        stg = [ph.sb("stg%d" % i, [128, 512]) for i in range(4)]
        stgb = [ph.sb("stgb%d" % i, [128, 512], BF16) for i in range(2)]
        pm = [ph.ps("pm%d" % i, [128, 512]) for i in range(4)]
        wv = I["w_in"][l].rearrange("(k p) n -> p k n", p=128)
        nmm = 0
        work = [(st, ci) for st in range(S // TS) for ci in range(len(IN_CHUNKS))]

        def issue_w(wi):
            c0, ncol, _, _ = IN_CHUNKS[work[wi][1]]
            b = wi % 2
            P.dma("sp", lambda e, b=b, c0=c0, ncol=ncol: e.dma_start(out=wf[b][:, :, 0:ncol], in_=wv[:, :, c0:c0 + ncol]),
                  writes=["wf%d" % b])
            P.op("pool", lambda e, b=b, ncol=ncol: e.tensor_copy(out=wb[b][:, :, 0:ncol], in_=wf[b][:, :, 0:ncol]),
                 reads=["wf%d" % b], writes=["wb%d" % b])

        issue_w(0)
        for wi, (st, ci) in enumerate(work):
            tok0 = st * TS
            if ci == 0:
                norm_to_hT(K, ph, l, 0, xd, tok0, TS, hT, A_b, B_b, (xt, hb, junk, ss, rstd, pt))
            if wi + 1 < len(work):
                issue_w(wi + 1)
            if True:
                (c0, ncol, kind, doff) = IN_CHUNKS[ci]
                b = wi % 2
                if kind in ("fm", "gt"):
                    for j in range(ncol // 128):
                        for th in range(TS // NT):
                            pi = nmm % 4
                            nmm += 1
                            for kc in range(NKC):
                                P.op("pe", lambda e, pi=pi, b=b, kc=kc, j=j, th=th: e.matmul(
                                    pm[pi][:, 0:NT], lhsT=wb[b][:, kc, j * 128:(j + 1) * 128], rhs=hT[:, kc, th * NT:(th + 1) * NT],
                                    start=(kc == 0), stop=(kc == NKC - 1)),
                                    reads=["wb%d" % b, "hT"], writes=["pm%d" % pi])
                            r0 = doff + j * 128
                            t0 = tok0 + th * NT
                            if kind == "fm":
                                P.op("dve", lambda e, pi=pi: e.tensor_copy(out=stg[pi][:, 0:NT], in_=pm[pi][:, 0:NT]),
                                     reads=["pm%d" % pi], writes=["stg%d" % pi])
                                P.dma("sp", lambda e, pi=pi, r0=r0, t0=t0: e.dma_start(
                                    out=K.Sc["fm"][r0:r0 + 128, t0:t0 + NT], in_=stg[pi][:, 0:NT]),
                                    reads=["stg%d" % pi], writes=["fm"])
                            else:
                                sb_i = pi % 2
                                P.op("act", lambda e, pi=pi, sb_i=sb_i: e.activation(out=stgb[sb_i][:, 0:NT], in_=pm[pi][:, 0:NT], func=AF.Sigmoid),
                                     reads=["pm%d" % pi], writes=["stgb%d" % sb_i])
                                P.dma("sp", lambda e, sb_i=sb_i, r0=r0, t0=t0: e.dma_start(
                                    out=K.Sc["gt"][r0:r0 + 128, t0:t0 + NT], in_=stgb[sb_i][:, 0:NT]),
                                    reads=["stgb%d" % sb_i], writes=["gt"])
                else:
                    for tt in range(TS // 128):
                        pi = nmm % 4
                        nmm += 1
                        for kc in range(NKC):
                            P.op("pe", lambda e, pi=pi, b=b, kc=kc, tt=tt, ncol=ncol: e.matmul(
                                pm[pi][:, 0:ncol], lhsT=hT[:, kc, tt * 128:(tt + 1) * 128], rhs=wb[b][:, kc, 0:ncol],
                                start=(kc == 0), stop=(kc == NKC - 1)),
                                reads=["wb%d" % b, "hT"], writes=["pm%d" % pi])
                        t0 = tok0 + tt * 128
                        P.op("dve", lambda e, pi=pi, ncol=ncol: e.tensor_copy(out=stg[pi][:, 0:ncol], in_=pm[pi][:, 0:ncol]),
                             reads=["pm%d" % pi], writes=["stg%d" % pi])
                        P.dma("sp", lambda e, pi=pi, t0=t0, doff=doff, ncol=ncol: e.dma_start(
                            out=K.Sc["tm"][t0:t0 + 128, doff:doff + ncol], in_=stg[pi][:, 0:ncol]),
                            reads=["stg%d" % pi], writes=["tm"])


def phase_conv(K, l):
    P, I, S = K.P, K.I, K.S
    fm, br = K.Sc["fm"], K.Sc["br"]
    with Phase(K) as ph:
        cw = ph.sb("cw", [128, 4, 3])
        cwv = I["conv_w"][l]
        for ct in range(4):
            P.dma("sp", lambda e, ct=ct: e.dma_start(out=cw[:, ct, :], in_=cwv[:, ct * 128:(ct + 1) * 128].rearrange("k p -> p k"),
                                                     allow_slow_non_contiguous=True), writes=["cw"])
        bg = ph.sb("bg", [128, S])
        cg = ph.sb("cg", [128, S])
        zp = ph.sb("zp", [128, S + 2])
        y = ph.sb("y", [128, S])
        ob = ph.sb("ob", [128, S], BF16)
        for ct in range(4):
            k = "cv_"
            P.dma("sp", lambda e, ct=ct: e.dma_start(out=bg[:], in_=fm[ct * 128:(ct + 1) * 128, :]), reads=["fm"], writes=[k + "bg"])
            P.dma("sp", lambda e, ct=ct: e.dma_start(out=cg[:], in_=fm[512 + ct * 128:512 + (ct + 1) * 128, :]), reads=["fm"], writes=[k + "cg"])
            P.dma("sp", lambda e, ct=ct: e.dma_start(out=zp[:, 1:S + 1], in_=fm[1024 + ct * 128:1024 + (ct + 1) * 128, :]), reads=["fm"], writes=[k + "zp"])
            P.op("pool", lambda e: e.memset(zp[:, 0:1], 0.0), writes=[k + "z0"])
            P.op("pool", lambda e: e.memset(zp[:, S + 1:S + 2], 0.0), writes=[k + "z1"])
            P.op("dve", lambda e: e.tensor_tensor(out=zp[:, 1:S + 1], in0=zp[:, 1:S + 1], in1=cg[:], op=ALU.mult),
                 reads=[k + "zp", k + "cg"], writes=[k + "zp"])
            P.op("dve", lambda e, ct=ct: e.tensor_scalar(out=y[:], in0=zp[:, 0:S], scalar1=cw[:, ct, 0:1], scalar2=None, op0=ALU.mult),
                 reads=[k + "zp", k + "z0", "cw"], writes=[k + "y"])
            P.op("dve", lambda e, ct=ct: e.scalar_tensor_tensor(out=y[:], in0=zp[:, 1:S + 1], scalar=cw[:, ct, 1:2], in1=y[:],
                                                                op0=ALU.mult, op1=ALU.add), reads=[k + "zp", k + "y"], writes=[k + "y"])
            P.op("dve", lambda e, ct=ct: e.scalar_tensor_tensor(out=y[:], in0=zp[:, 2:S + 2], scalar=cw[:, ct, 2:3], in1=y[:],
                                                                op0=ALU.mult, op1=ALU.add), reads=[k + "zp", k + "z1", k + "y"], writes=[k + "y"])
            P.op("pool", lambda e: e.tensor_tensor(out=ob[:], in0=y[:], in1=bg[:], op=ALU.mult), reads=[k + "y", k + "bg"], writes=[k + "ob"])
            P.dma("sp", lambda e, ct=ct: e.dma_start(out=br[ct * 128:(ct + 1) * 128, :], in_=ob[:]), reads=[k + "ob"], writes=["br"])


def bcast_rows(K, ph, name, row_ap, n, reps):
    t = ph.sb(name, [128, reps, n])
    for r in range(reps):
        K.P.dma("sp", lambda e, r=r: e.dma_start(out=t[:, r, :], in_=row_ap.to_broadcast([128, n])), writes=[name + str(r)])
    return t, [name + str(r) for r in range(reps)]


def head_rms(P, x3, nh, hd, sq3, ssq, eps_t, keys_in, tag):
    P.op("dve", lambda e: e.tensor_tensor(out=sq3, in0=x3, in1=x3, op=ALU.mult), reads=keys_in, writes=[tag + "sq"])
    P.op("dve", lambda e: e.tensor_reduce(out=ssq, in_=sq3, axis=AX.X, op=ALU.add), reads=[tag + "sq"], writes=[tag + "ssq"])
    P.op("act", lambda e: e.activation(out=ssq, in_=ssq, func=AF.Sqrt, scale=1.0 / hd, bias=eps_t[:, 0:1]),
         reads=[tag + "ssq"], writes=[tag + "ssq"])
    P.op("dve", lambda e: e.reciprocal(out=ssq, in_=ssq), reads=[tag + "ssq"], writes=[tag + "ssq"])
    P.op("dve", lambda e: e.tensor_tensor(out=x3, in0=x3, in1=ssq.unsqueeze(2).to_broadcast([128, nh, hd]), op=ALU.mult),
         reads=keys_in + [tag + "ssq"], writes=keys_in)


def rope3(P, x1, x2, o1, o2, cs, sn, t1, t2, shape, keys_in, keys_out, tag):
    P.op("dve", lambda e: e.tensor_tensor(out=t1, in0=x1, in1=cs, op=ALU.mult), reads=keys_in, writes=[tag + "t1"])
    P.op("pool", lambda e: e.tensor_tensor(out=t2, in0=x2, in1=sn, op=ALU.mult), reads=keys_in, writes=[tag + "t2"])
    P.op("dve", lambda e: e.tensor_tensor(out=o1, in0=t1, in1=t2, op=ALU.subtract), reads=[tag + "t1", tag + "t2"], writes=[keys_out[0]])
    P.op("dve", lambda e: e.tensor_tensor(out=t1, in0=x1, in1=sn, op=ALU.mult), reads=keys_in + [keys_out[0]], writes=[tag + "t1"])
    P.op("pool", lambda e: e.tensor_tensor(out=t2, in0=x2, in1=cs, op=ALU.mult), reads=keys_in + [keys_out[0]], writes=[tag + "t2"])
    P.op("dve", lambda e: e.tensor_tensor(out=o2, in0=t1, in1=t2, op=ALU.add), reads=[tag + "t1", tag + "t2"], writes=[keys_out[1]])


def phase_gqa_prep(K, l):
    P, I, S = K.P, K.I, K.S
    tm = K.Sc["tm"]
    rope_g = I["rope_g"]
    with Phase(K) as ph:
        eps_t = ph.sb("eps_t", [128, 1])
        P.op("dve", lambda e: e.memset(eps_t[:], NORM_EPS), writes=["eps_t"])
        gb = ph.sb("gb", [128, 10, 64])
        gkeys = []
        for r in range(10):
            src = I["gqa_q_norm"][l:l + 1, :] if r < 8 else I["gqa_k_norm"][l:l + 1, :]
            P.dma("sp", lambda e, r=r, src=src: e.dma_start(out=gb[:, r, :], in_=src.to_broadcast([128, 64])), writes=["gb%d" % r])
            gkeys.append("gb%d" % r)
        for tt in range(S // 128):
            b = tt % 2
            t0 = tt * 128
            if tt < 2:
                K_ = {}
                K_["x"] = ph.sb("gx%d" % b, [128, 768])
                K_["rp"] = ph.sb("grp%d" % b, [128, 64])
                K_["sq"] = ph.sb("gsq%d" % b, [128, 640])
                K_["ssq"] = ph.sb("gssq%d" % b, [128, 10])
                K_["t1"] = ph.sb("gt1%d" % b, [128, 320])
                K_["t2"] = ph.sb("gt2%d" % b, [128, 320])
                K_["qk"] = ph.sb("gqk%d" % b, [128, 640], BF16)
                K_["v"] = ph.sb("gv%d" % b, [128, 2, 65], BF16)
                K_["pt"] = ph.ps("gpt%d" % b, [128, 1024], BF16)
                K_["qkT"] = ph.sb("gqkT%d" % b, [128, 640], BF16)
                if tt == 0:
                    bufs = [K_, None]
                else:
                    bufs[1] = K_
            B = bufs[b]
            k = "g%d_" % b
            x, rp = B["x"], B["rp"]
            P.dma("sp", lambda e, x=x, t0=t0: e.dma_start(out=x[:], in_=tm[t0:t0 + 128, 0:768]), reads=["tm"], writes=[k + "x"])
            P.dma("sp", lambda e, rp=rp, t0=t0: e.dma_start(out=rp[:], in_=rope_g[t0:t0 + 128, :]), writes=[k + "rp"])
            x3 = x[:, 0:640].rearrange("p (h d) -> p h d", h=10)
            head_rms(P, x3, 10, 64, B["sq"][:].rearrange("p (h d) -> p h d", h=10), B["ssq"][:], eps_t, [k + "x"], k)
            P.op("pool", lambda e, x3=x3: e.tensor_tensor(out=x3, in0=x3, in1=gb[:], op=ALU.mult), reads=[k + "x"] + gkeys, writes=[k + "x"])
            x4 = x[:, 0:640].rearrange("p (h two d) -> p h two d", h=10, two=2)
            qk4 = B["qk"][:].rearrange("p (h two d) -> p h two d", h=10, two=2)
            cs = rp[:, 0:32].unsqueeze(1).to_broadcast([128, 10, 32])
            sn = rp[:, 32:64].unsqueeze(1).to_broadcast([128, 10, 32])
            t1 = B["t1"][:].rearrange("p (h d) -> p h d", h=10)
            t2 = B["t2"][:].rearrange("p (h d) -> p h d", h=10)
            rope3(P, x4[:, :, 0, :], x4[:, :, 1, :], qk4[:, :, 0, :], qk4[:, :, 1, :], cs, sn, t1, t2, None,
                  [k + "x", k + "rp"], [k + "qk0", k + "qk1"], k)
            v = B["v"]
            P.op("pool", lambda e, v=v: e.memset(v[:, :, 64:65], 1.0), writes=[k + "v1"])
            P.op("act", lambda e, v=v, x=x: e.copy(out=v[:, :, 0:64], in_=x[:, 640:768].rearrange("p (h d) -> p h d", h=2)),
                 reads=[k + "x"], writes=[k + "v"])
            P.dma("sp", lambda e, v=v, t0=t0: e.dma_start(out=K.Sc["gv"][t0:t0 + 128, :, :], in_=v[:]), reads=[k + "v", k + "v1"], writes=["gv"])
            pt, qkT = B["pt"], B["qkT"]
            for j in range(5):
                P.op("pe", lambda e, pt=pt, j=j, qk=B["qk"]: e.transpose(pt[:, j * 128:(j + 1) * 128], qk[:, j * 128:(j + 1) * 128], K.identb[:]),
                     reads=[k + "qk0", k + "qk1", "c_identb"], writes=[k + "pt"])
            P.op("act", lambda e, pt=pt, qkT=qkT: e.copy(out=qkT[:], in_=pt[:, 0:640]), reads=[k + "pt"], writes=[k + "qkT"])
            P.dma("sp", lambda e, qkT=qkT, t0=t0: e.dma_start(
                out=K.Sc["gqT"].rearrange("(j p) s -> p j s", p=128)[:, :, t0:t0 + 128],
                in_=qkT[:].rearrange("p (j t) -> p j t", j=5)), reads=[k + "qkT"], writes=["gqT"])


def attention(K, heads, dq, scale, tag):
    P, S = K.P, K.S
    QN = min(512, S)
    NKT = S // 128
    with Phase(K) as ph:
        ones = ph.sb("a_ones", [128, 64])
        P.op("dve", lambda e: e.memset(ones[:], 1.0), writes=["a_ones"])
        kT = [ph.sb("a_kT%d" % i, [dq, S], BF16) for i in range(2)]
        vt = [ph.sb("a_v%d" % i, [128, NKT, 65], BF16) for i in range(2)]
        qT = [ph.sb("a_qT%d" % i, [dq, QN], BF16) for i in range(2)]
        pT = [ph.sb("a_pT%d" % i, [128, QN], BF16) for i in range(3)]
        rrow = ph.sb("a_rrow", [128, QN])
        rb = ph.sb("a_rb", [64, QN])
        ob = [ph.sb("a_ob%d" % i, [64, QN], BF16) for i in range(2)]
        ps_s = [ph.ps("a_ps%d" % i, [128, QN]) for i in range(3)]
        ps_o = [ph.ps("a_po%d" % i, [65, QN]) for i in range(2)]
        ps_b = ph.ps("a_pb", [64, QN])
        nq = 0
        ns = 0
        for hi, (q_ap, k_ap, v_ap, o_ap) in enumerate(heads):
            hb = hi % 2
            P.dma("sp", lambda e, hb=hb, k_ap=k_ap: e.dma_start(out=kT[hb][:], in_=k_ap), reads=[tag + "kT_d"], writes=["a_kT%d" % hb])
            P.dma("sp", lambda e, hb=hb, v_ap=v_ap: e.dma_start(out=vt[hb][:], in_=v_ap.rearrange("(kc p) e -> p kc e", p=128)),
                  reads=[tag + "v_d"], writes=["a_v%d" % hb])
            for qc in range(S // QN):
                qb = nq % 2
                nq += 1
                q0 = qc * QN
                P.dma("sp", lambda e, qb=qb, q_ap=q_ap, q0=q0: e.dma_start(out=qT[qb][:], in_=q_ap[:, q0:q0 + QN]),
                      reads=[tag + "qT_d"], writes=["a_qT%d" % qb])
                sbs = []
                for kc in range(NKT + 1):
                    if kc < NKT:
                        sb_ = ns % 3
                        ns += 1
                        sbs.append(sb_)
                        P.op("pe", lambda e, sb_=sb_, hb=hb, kc=kc, qb=qb: e.matmul(
                            ps_s[sb_][:], lhsT=kT[hb][:, kc * 128:(kc + 1) * 128], rhs=qT[qb][:], start=True, stop=True),
                            reads=["a_kT%d" % hb, "a_qT%d" % qb], writes=["a_ps%d" % sb_])
                        P.op("act", lambda e, sb_=sb_: e.activation(out=pT[sb_][:], in_=ps_s[sb_][:], func=AF.Exp, scale=scale),
                             reads=["a_ps%d" % sb_], writes=["a_pT%d" % sb_])
                    if kc >= 1:
                        kp = kc - 1
                        sp_ = sbs[kp]
                        P.op("pe", lambda e, sp_=sp_, hb=hb, kp=kp, qb=qb: e.matmul(
                            ps_o[qb][:], lhsT=vt[hb][:, kp, :], rhs=pT[sp_][:], start=(kp == 0), stop=(kp == NKT - 1)),
                            reads=["a_v%d" % hb, "a_pT%d" % sp_], writes=["a_po%d" % qb])
                P.op("dve", lambda e, qb=qb: e.reciprocal(out=rrow[64:65, :], in_=ps_o[qb][64:65, :]), reads=["a_po%d" % qb], writes=["a_rrow"])
                P.op("pe", lambda e: e.matmul(ps_b[:], lhsT=ones[64:65, 0:64], rhs=rrow[64:65, :], start=True, stop=True),
                     reads=["a_ones", "a_rrow"], writes=["a_pb"])
                P.op("act", lambda e: e.copy(out=rb[:], in_=ps_b[:]), reads=["a_pb"], writes=["a_rb"])
                P.op("dve", lambda e, qb=qb: e.tensor_tensor(out=ob[qb][:], in0=ps_o[qb][0:64, :], in1=rb[:], op=ALU.mult),
                     reads=["a_po%d" % qb, "a_rb"], writes=["a_ob%d" % qb])
                P.dma("sp", lambda e, qb=qb, o_ap=o_ap, q0=q0: e.dma_start(out=o_ap[:, q0:q0 + QN], in_=ob[qb][:]),
                      reads=["a_ob%d" % qb], writes=["br"])


def phase_gqa_attn(K, l):
    gqT, gv, br = K.Sc["gqT"], K.Sc["gv"], K.Sc["br"]
    heads = []
    for h in range(8):
        kv = h // 4
        heads.append((gqT[h * 64:(h + 1) * 64, :], gqT[512 + kv * 64:512 + (kv + 1) * 64, :], gv[:, kv, :],
                      br[512 + h * 64:512 + (h + 1) * 64, :]))
    attention(K, heads, 64, 64 ** -0.5, "g")


def phase_mla_prep(K, l):
    P, I, S = K.P, K.I, K.S
    tm = K.Sc["tm"]
    rope_m = I["rope_m"]
    with Phase(K) as ph:
        eps_t = ph.sb("eps_t", [128, 1])
        P.op("dve", lambda e: e.memset(eps_t[:], NORM_EPS), writes=["eps_t"])
        gc, gckeys = bcast_rows(K, ph, "m_gc", I["mla_qc_norm"][l:l + 1, :], 384, 1)
        gkv, gkvkeys = bcast_rows(K, ph, "m_gkv", I["mla_kvc_norm"][l:l + 1, :], 256, 1)
        gq, gqkeys = bcast_rows(K, ph, "m_gq", I["mla_q_norm"][l:l + 1, :], 96, 8)
        gk, gkkeys = bcast_rows(K, ph, "m_gk", I["mla_k_norm"][l:l + 1, :], 96, 8)
        wqf = ph.sb("m_wqf", [128, 3, 768])
        wkf = ph.sb("m_wkf", [128, 2, 1024])
        wq = ph.sb("m_wq", [128, 3, 768], BF16)
        wk = ph.sb("m_wk", [128, 2, 1024], BF16)
        P.dma("sp", lambda e: e.dma_start(out=wqf[:], in_=I["mla_w_uq"][l].rearrange("(k p) n -> p k n", p=128)), writes=["m_wqf"])
        P.dma("sp", lambda e: e.dma_start(out=wkf[:], in_=I["mla_w_ukv"][l].rearrange("(k p) n -> p k n", p=128)), writes=["m_wkf"])
        P.op("pool", lambda e: e.tensor_copy(out=wq[:], in_=wqf[:]), reads=["m_wqf"], writes=["m_wq"])
        P.op("pool", lambda e: e.tensor_copy(out=wk[:], in_=wkf[:]), reads=["m_wkf"], writes=["m_wk"])
        x = ph.sb("m_x", [128, 672])
        rp = ph.sb("m_rp", [128, 32])
        junk = ph.sb("m_junk", [128, 384])
        ss = ph.sb("m_ss", [128, 2])
        cn = ph.sb("m_cn", [128, 640], BF16)
        pt = ph.ps("m_pt", [128, 1024], BF16)
        cnT = ph.sb("m_cnT", [128, 5, 128], BF16)
        pq = [ph.ps("m_pq%d" % i, [128, 512]) for i in range(2)]
        pkv = [ph.ps("m_pkv%d" % i, [128, 512]) for i in range(2)]
        q = ph.sb("m_q", [128, 8, 96])
        kk = ph.sb("m_k", [128, 8, 96])
        sq = ph.sb("m_sq", [128, 8, 96])
        ssq = ph.sb("m_ssq", [128, 8])
        t1 = ph.sb("m_t1", [128, 8, 16])
        t2 = ph.sb("m_t2", [128, 8, 16])
        qb = ph.sb("m_qb", [128, 8, 96], BF16)
        kb = ph.sb("m_kb", [128, 8, 96], BF16)
        v = ph.sb("m_v", [128, 8, 65], BF16)
        ptq = ph.ps("m_ptq", [96, 8, 128], BF16)
        ptk = ph.ps("m_ptk", [96, 8, 128], BF16)
        qT = ph.sb("m_qT", [96, 8, 128], BF16)
        kT = ph.sb("m_kT", [96, 8, 128], BF16)
        P.op("pool", lambda e: e.memset(v[:, :, 64:65], 1.0), writes=["m_v1"])
        for tt in range(S // 128):
            t0 = tt * 128
            P.dma("sp", lambda e, t0=t0: e.dma_start(out=x[:], in_=tm[t0:t0 + 128, 768:1440]), reads=["tm"], writes=["m_x"])
            P.dma("sp", lambda e, t0=t0: e.dma_start(out=rp[:], in_=rope_m[t0:t0 + 128, :]), writes=["m_rp"])
            for i, (c0, n, g_t, gkeys_) in enumerate(((0, 384, gc, gckeys), (384, 256, gkv, gkvkeys))):
                P.op("act", lambda e, c0=c0, n=n, i=i: e.activation(out=junk[:, 0:n], in_=x[:, c0:c0 + n], func=AF.Square,
                                                                    accum_out=ss[:, i:i + 1]), reads=["m_x"], writes=["m_junk", "m_ss%d" % i])
                P.op("act", lambda e, n=n, i=i: e.activation(out=ss[:, i:i + 1], in_=ss[:, i:i + 1], func=AF.Sqrt, scale=1.0 / n,
                                                             bias=eps_t[:, 0:1]), reads=["m_ss%d" % i, "eps_t"], writes=["m_ss%d" % i])
                P.op("dve", lambda e, i=i: e.reciprocal(out=ss[:, i:i + 1], in_=ss[:, i:i + 1]), reads=["m_ss%d" % i], writes=["m_ss%d" % i])
                P.op("dve", lambda e, c0=c0, n=n, i=i, g_t=g_t: e.scalar_tensor_tensor(
                    out=cn[:, c0:c0 + n], in0=x[:, c0:c0 + n], scalar=ss[:, i:i + 1], in1=g_t[:, 0, :], op0=ALU.mult, op1=ALU.mult),
                    reads=["m_x", "m_ss%d" % i] + gkeys_, writes=["m_cn%d" % i])
            for j in range(5):
                P.op("pe", lambda e, j=j: e.transpose(pt[:, j * 128:(j + 1) * 128], cn[:, j * 128:(j + 1) * 128], K.identb[:]),
                     reads=["m_cn0", "m_cn1", "c_identb"], writes=["m_pt"])
            P.op("act", lambda e: e.copy(out=cnT[:], in_=pt[:, 0:640].rearrange("p (j t) -> p j t", j=5)), reads=["m_pt"], writes=["m_cnT"])
            if getattr(K.cfg, "dbg", 0) == 1:
                continue
            for half in range(2):
                for kc in range(3):
                    P.op("pe", lambda e, half=half, kc=kc: e.matmul(pq[half][:, 0:384], lhsT=cnT[:, kc, :], rhs=wq[:, kc, half * 384:(half + 1) * 384],
                                                                    start=(kc == 0), stop=(kc == 2)), reads=["m_cnT", "m_wq"], writes=["m_pq%d" % half])
                for kc in range(2):
                    P.op("pe", lambda e, half=half, kc=kc: e.matmul(pkv[half][:], lhsT=cnT[:, 3 + kc, :], rhs=wk[:, kc, half * 512:(half + 1) * 512],
                                                                    start=(kc == 0), stop=(kc == 1)), reads=["m_cnT", "m_wk"], writes=["m_pkv%d" % half])
            if getattr(K.cfg, "dbg", 0) == 5:
                continue
            for half in range(2):
                P.op("act", lambda e, half=half: e.copy(out=q[:, half * 4:(half + 1) * 4, :], in_=pq[half][:, 0:384].rearrange("p (h d) -> p h d", h=4)),
                     reads=["m_pq%d" % half], writes=["m_q%d" % half])
                pk4 = pkv[half][:].rearrange("p (h d) -> p h d", h=4)
                if getattr(K.cfg, "dbg", 0) == 6:
                    continue
                P.op("dve", lambda e, half=half, pk4=pk4: e.tensor_copy(out=kk[:, half * 4:(half + 1) * 4, 0:64], in_=pk4[:, :, 0:64]),
                     reads=["m_pkv%d" % half], writes=["m_kn%d" % half])
                if getattr(K.cfg, "dbg", 0) == 7:
                    continue
                P.op("dve", lambda e, half=half, pk4=pk4: e.tensor_copy(out=v[:, half * 4:(half + 1) * 4, 0:64], in_=pk4[:, :, 64:128]),
                     reads=["m_pkv%d" % half], writes=["m_v%d" % half])
            if getattr(K.cfg, "dbg", 0) == 2:
                continue
            P.op("pool", lambda e: e.tensor_copy(out=kk[:, :, 64:96], in_=x[:, 640:672].unsqueeze(1).to_broadcast([128, 8, 32])),
                 reads=["m_x"], writes=["m_kpe"])
            if getattr(K.cfg, "dbg", 0) == 3:
                continue
            P.dma("sp", lambda e, t0=t0: e.dma_start(out=K.Sc["mv"][t0:t0 + 128, :, :], in_=v[:]), reads=["m_v0", "m_v1", "m_v1"], writes=["mv"])
            cs = rp[:, 0:16].unsqueeze(1).to_broadcast([128, 8, 16])
            sn = rp[:, 16:32].unsqueeze(1).to_broadcast([128, 8, 16])
            for (t_, keys_, g_t, gkeys_, ob_, okey, pt_, T_, dkey) in (
                    (q, ["m_q0", "m_q1"], gq, gqkeys, qb, "m_qb", ptq, qT, "mqT"),
                    (kk, ["m_kn0", "m_kn1", "m_kpe"], gk, gkkeys, kb, "m_kb", ptk, kT, "mkT")):
                kx = okey + "x"
                P.op("dve", lambda e, t_=t_: e.tensor_tensor(out=sq[:], in0=t_[:], in1=t_[:], op=ALU.mult), reads=keys_, writes=["m_sq"])
                P.op("dve", lambda e: e.tensor_reduce(out=ssq[:], in_=sq[:], axis=AX.X, op=ALU.add), reads=["m_sq"], writes=["m_ssq"])
                P.op("act", lambda e: e.activation(out=ssq[:], in_=ssq[:], func=AF.Sqrt, scale=1.0 / 96, bias=eps_t[:, 0:1]),
                     reads=["m_ssq", "eps_t"], writes=["m_ssq"])
                P.op("dve", lambda e: e.reciprocal(out=ssq[:], in_=ssq[:]), reads=["m_ssq"], writes=["m_ssq"])
                P.op("dve", lambda e, t_=t_: e.tensor_tensor(out=t_[:], in0=t_[:], in1=ssq[:].unsqueeze(2).to_broadcast([128, 8, 96]), op=ALU.mult),
                     reads=keys_ + ["m_ssq"], writes=[kx])
                P.op("pool", lambda e, t_=t_, g_t=g_t: e.tensor_tensor(out=t_[:], in0=t_[:], in1=g_t[:], op=ALU.mult), reads=[kx] + gkeys_, writes=[kx])
                P.op("act", lambda e, t_=t_, ob_=ob_: e.copy(out=ob_[:, :, 0:64], in_=t_[:, :, 0:64]), reads=[kx], writes=[okey + "n"])
                rope3(P, t_[:, :, 64:80], t_[:, :, 80:96], ob_[:, :, 64:80], ob_[:, :, 80:96], cs, sn, t1[:], t2[:], None,
                      [kx, "m_rp"], [okey + "r0", okey + "r1"], okey)
                if getattr(K.cfg, "dbg", 0) == 4:
                    continue
                for h in range(8):
                    P.op("pe", lambda e, h=h, pt_=pt_, ob_=ob_: e.transpose(pt_[:, h, :], ob_[:, h, :], K.identb[:]),
                         reads=[okey + "n", okey + "r0", okey + "r1", "c_identb"], writes=[okey + "pt"])
                P.op("act", lambda e, pt_=pt_, T_=T_: e.copy(out=T_[:], in_=pt_[:]), reads=[okey + "pt"], writes=[okey + "T"])
                P.dma("sp", lambda e, T_=T_, t0=t0, dkey=dkey: e.dma_start(
                    out=K.Sc[dkey].rearrange("(h d) s -> d h s", d=96)[:, :, t0:t0 + 128], in_=T_[:]), reads=[okey + "T"], writes=[dkey])


def phase_mla_attn(K, l):
    mqT, mkT, mv, br = K.Sc["mqT"], K.Sc["mkT"], K.Sc["mv"], K.Sc["br"]
    heads = []
    for h in range(8):
        heads.append((mqT[h * 96:(h + 1) * 96, :], mkT[h * 96:(h + 1) * 96, :], mv[:, h, :], br[1536 + h * 64:1536 + (h + 1) * 64, :]))
    attention(K, heads, 96, 96 ** -0.5, "m")


def load_cast(K, src_ap, stage_ap, dst_ap, skey, dkey, eng="pool"):
    P = K.P
    P.dma("sp", lambda e: e.dma_start(out=stage_ap, in_=src_ap), writes=[skey])
    P.op(eng, lambda e: e.tensor_copy(out=dst_ap, in_=stage_ap), reads=[skey], writes=[dkey])


def phase3a(K, l):
    P, I, S = K.P, K.I, K.S
    br, gt, mT = K.Sc["br"], K.Sc["gt"], K.Sc["mT"]
    NT = min(512, S)
    with Phase(K) as ph:
        wbf = ph.sb("wbf", [128, 4, D])
        wb = ph.sb("wb3", [128, 16, D], BF16)
        for i in range(4):
            load_cast(K, I["w_branch"][l][i * 512:(i + 1) * 512, :].rearrange("(kc p) n -> p kc n", p=128), wbf[:],
                      wb[:, i * 4:(i + 1) * 4, :], "wbf", "wb3_%d" % i)
        wkeys = ["wb3_%d" % i for i in range(4)]
        brT = [ph.sb("brT%d" % i, [128, 16, NT], BF16) for i in range(2)]
        gts = [ph.sb("gts%d" % i, [128, 4, NT], BF16) for i in range(2)]
        tmp = [ph.sb("tmp3_%d" % i, [128, NT]) for i in range(4)]
        ms = [ph.sb("ms%d" % i, [128, NT], BF16) for i in range(2)]
        ps = [ph.ps("p3_%d" % i, [128, 512]) for i in range(8)]
        gtv = gt.rearrange("(i n) s -> n i s", i=4)
        brv = br.rearrange("(k p) s -> p k s", p=128)
        n = 0
        for st in range(S // NT):
            tok0 = st * NT
            bb = st % 2
            P.dma("sp", lambda e, bb=bb, tok0=tok0: e.dma_start(out=brT[bb][:], in_=brv[:, :, tok0:tok0 + NT]), reads=["br"], writes=["brT%d" % bb])
            for nt in range(16):
                gb = n % 2
                P.dma("sp", lambda e, gb=gb, nt=nt, tok0=tok0: e.dma_start(out=gts[gb][:], in_=gtv[nt * 128:(nt + 1) * 128, :, tok0:tok0 + NT]),
                      reads=["gt"], writes=["gts%d" % gb])
                for i in range(4):
                    pi = (n % 2) * 4 + i
                    for kc in range(4):
                        P.op("pe", lambda e, pi=pi, i=i, kc=kc, nt=nt, bb=bb: e.matmul(
                            ps[pi][:, 0:NT], lhsT=wb[:, i * 4 + kc, nt * 128:(nt + 1) * 128], rhs=brT[bb][:, i * 4 + kc, :],
                            start=(kc == 0), stop=(kc == 3)), reads=wkeys + ["brT%d" % bb], writes=["p3_%d" % pi])
                    P.op("dve", lambda e, pi=pi, i=i, gb=gb: e.tensor_tensor(out=tmp[i][:], in0=ps[pi][:, 0:NT], in1=gts[gb][:, i, :], op=ALU.mult),
                         reads=["p3_%d" % pi, "gts%d" % gb], writes=["tmp3_%d" % i])
                P.op("pool", lambda e: e.tensor_tensor(out=tmp[0][:], in0=tmp[0][:], in1=tmp[1][:], op=ALU.add),
                     reads=["tmp3_0", "tmp3_1"], writes=["tmp3_0"])
                P.op("pool", lambda e: e.tensor_tensor(out=tmp[2][:], in0=tmp[2][:], in1=tmp[3][:], op=ALU.add),
                     reads=["tmp3_2", "tmp3_3"], writes=["tmp3_2"])
                P.op("pool", lambda e, gb=gb: e.tensor_tensor(out=ms[gb][:], in0=tmp[0][:], in1=tmp[2][:], op=ALU.add),
                     reads=["tmp3_0", "tmp3_2"], writes=["ms%d" % gb])
                P.dma("sp", lambda e, gb=gb, nt=nt, tok0=tok0: e.dma_start(out=mT[nt * 128:(nt + 1) * 128, tok0:tok0 + NT], in_=ms[gb][:]),
                      reads=["ms%d" % gb], writes=["mT"])
                n += 1


def phase3b(K, l):
    P, I, S = K.P, K.I, K.S
    mT, modv = K.Sc["mT"], K.Sc["modv"]
    xd = K.xres
    NTT = S // 128
    MT = min(512, S)
    with Phase(K) as ph:
        eps_t = ph.sb("eps3", [128, 1])
        P.op("dve", lambda e: e.memset(eps_t[:], NORM_EPS), writes=["eps3"])
        wof = ph.sb("wof", [128, 4, D])
        wo = ph.sb("wo", [128, 16, D], BF16)
        for i in range(4):
            load_cast(K, I["w_out"][l][i * 512:(i + 1) * 512, :].rearrange("(kc p) n -> p kc n", p=128), wof[:],
                      wo[:, i * 4:(i + 1) * 4, :], "wof", "wo_%d" % i)
        wkeys = ["wo_%d" % i for i in range(4)]
        wr = ph.sb("wr", [128, 16, NE])
        P.dma("sp", lambda e: e.dma_start(out=wr[:], in_=I["w_router"][l].rearrange("(k p) n -> p k n", p=128)), writes=["wr"])
        G1 = load_bcast_row(K, ph, "G1_b", modv[l * 6 + 2:l * 6 + 3, :], D)
        B2 = load_bcast_row(K, ph, "B2_b", modv[l * 6 + 3:l * 6 + 4, :], D)
        A2 = load_bcast_row(K, ph, "A2_b", modv[l * 6 + 4:l * 6 + 5, :], D)
        mTt = [ph.sb("mTt%d" % i, [128, 16, MT], BF16) for i in range(2)]
        xt = [ph.sb("x3t%d" % i, [128, D]) for i in range(2)]
        tmp = [ph.sb("tmp3b%d" % i, [128, 512]) for i in range(2)]
        junk = ph.sb("junk3", [128, D], BF16)
        h2b = [ph.sb("h2b%d" % i, [128, D], BF16) for i in range(2)]
        ss = ph.sb("ss3", [128, 1])
        rstd = ph.sb("rstd3", [128, 1])
        h2T = ph.sb("h2T", [128, 16, 128])
        lg = ph.sb("lg", [128, NE])
        mx = ph.sb("mx3", [128, 1])
        sm = ph.sb("sm3", [128, 1])
        aff = ph.sb("aff_all", [128, NTT, NE])
        affT = [ph.sb("affT_s%d" % i, [NE, 128]) for i in range(2)]
        po = [ph.ps("p3o%d" % i, [128, 512]) for i in range(2)]
        ptr = [ph.ps("p3t%d" % i, [128, 512]) for i in range(2)]
        pr = ph.ps("p3r", [128, 512])
        pat = ph.ps("p3at", [128, 512])
        mTv = mT.rearrange("(k p) s -> p k s", p=128)
        n = 0
        for st in range(S // MT):
            mb = st % 2
            P.dma("sp", lambda e, mb=mb, st=st: e.dma_start(out=mTt[mb][:], in_=mTv[:, :, st * MT:(st + 1) * MT]), reads=["mT"], writes=["mTt%d" % mb])
            for t in range(MT // 128):
                tt = st * (MT // 128) + t
                t0 = tt * 128
                xb = tt % 2
                P.dma("sp", lambda e, xb=xb, t0=t0: e.dma_start(out=xt[xb][:], in_=xd[t0:t0 + 128, :]), reads=["xres"], writes=["x3t%d" % xb])
                for c4 in range(4):
                    pi = n % 2
                    n += 1
                    for kc in range(16):
                        P.op("pe", lambda e, pi=pi, kc=kc, mb=mb, t=t, c4=c4: e.matmul(
                            po[pi][:], lhsT=mTt[mb][:, kc, t * 128:(t + 1) * 128], rhs=wo[:, kc, c4 * 512:(c4 + 1) * 512],
                            start=(kc == 0), stop=(kc == 15)), reads=wkeys + ["mTt%d" % mb], writes=["p3o%d" % pi])
                    P.op("dve", lambda e, pi=pi, c4=c4: e.tensor_tensor(out=tmp[pi][:], in0=po[pi][:], in1=G1[:, c4 * 512:(c4 + 1) * 512], op=ALU.mult),
                         reads=["p3o%d" % pi, "G1_b"], writes=["tmp3b%d" % pi])
                    P.op("pool", lambda e, pi=pi, c4=c4, xb=xb: e.tensor_tensor(out=xt[xb][:, c4 * 512:(c4 + 1) * 512], in0=xt[xb][:, c4 * 512:(c4 + 1) * 512],
                                                                                in1=tmp[pi][:], op=ALU.add),
                         reads=["tmp3b%d" % pi, "x3t%d" % xb], writes=["x3t%d" % xb])
                P.dma("sp", lambda e, xb=xb, t0=t0: e.dma_start(out=xd[t0:t0 + 128, :], in_=xt[xb][:]), reads=["x3t%d" % xb], writes=["xres"])
                P.op("act", lambda e, xb=xb: e.activation(out=junk[:], in_=xt[xb][:], func=AF.Square, accum_out=ss[:, 0:1]),
                     reads=["x3t%d" % xb], writes=["junk3", "ss3"])
                P.op("act", lambda e: e.activation(out=rstd[:], in_=ss[:], func=AF.Sqrt, scale=1.0 / D, bias=eps_t[:, 0:1]),
                     reads=["ss3", "eps3"], writes=["rstd3"])
                P.op("dve", lambda e: e.reciprocal(out=rstd[:], in_=rstd[:]), reads=["rstd3"], writes=["rstd3"])
                P.op("dve", lambda e, xb=xb: e.scalar_tensor_tensor(out=xt[xb][:], in0=xt[xb][:], scalar=rstd[:, 0:1], in1=A2[:], op0=ALU.mult, op1=ALU.mult),
                     reads=["x3t%d" % xb, "rstd3", "A2_b"], writes=["x3t%d" % xb])
                P.op("pool", lambda e, xb=xb: e.tensor_tensor(out=xt[xb][:], in0=xt[xb][:], in1=B2[:], op=ALU.add),
                     reads=["x3t%d" % xb, "B2_b"], writes=["x3t%d" % xb])
                P.op("act", lambda e, xb=xb: e.copy(out=h2b[xb][:], in_=xt[xb][:]), reads=["x3t%d" % xb], writes=["h2b%d" % xb])
                P.dma("sp", lambda e, xb=xb, t0=t0: e.dma_start(out=K.Sc["h2"][t0:t0 + 128, :], in_=h2b[xb][:]), reads=["h2b%d" % xb], writes=["h2"])
                for g4 in range(4):
                    tb = g4 % 2
                    for j in range(4):
                        kc = g4 * 4 + j
                        P.op("pe", lambda e, tb=tb, j=j, kc=kc, xb=xb: e.transpose(ptr[tb][:, j * 128:(j + 1) * 128], xt[xb][:, kc * 128:(kc + 1) * 128], K.ident[:]),
                             reads=["x3t%d" % xb, "c_ident"], writes=["p3t%d" % tb])
                    P.op("act", lambda e, tb=tb, g4=g4: e.copy(out=h2T[:, g4 * 4:(g4 + 1) * 4, :], in_=ptr[tb][:].rearrange("p (j t) -> p j t", j=4)),
                         reads=["p3t%d" % tb], writes=["h2T"])
                for kc in range(16):
                    P.op("pe", lambda e, kc=kc: e.matmul(pr[:, 0:NE], lhsT=h2T[:, kc, :], rhs=wr[:, kc, :], start=(kc == 0), stop=(kc == 15)),
                         reads=["h2T", "wr"], writes=["p3r"])
                P.op("dve", lambda e: e.tensor_reduce(out=mx[:], in_=pr[:, 0:NE], axis=AX.X, op=ALU.max), reads=["p3r"], writes=["mx3"])
                P.op("dve", lambda e: e.tensor_scalar(out=mx[:], in0=mx[:], scalar1=-1.0, scalar2=None, op0=ALU.mult), reads=["mx3"], writes=["mx3"])
                P.op("act", lambda e: e.activation(out=lg[:], in_=pr[:, 0:NE], func=AF.Exp, bias=mx[:, 0:1], accum_out=sm[:, 0:1]),
                     reads=["p3r", "mx3"], writes=["lg", "sm3"])
                P.op("dve", lambda e: e.reciprocal(out=sm[:], in_=sm[:]), reads=["sm3"], writes=["sm3"])
                P.op("dve", lambda e, tt=tt: e.tensor_scalar(out=aff[:, tt, :], in0=lg[:], scalar1=sm[:, 0:1], scalar2=None, op0=ALU.mult),
                     reads=["lg", "sm3"], writes=["aff%d" % tt])
                P.op("pe", lambda e, tt=tt: e.transpose(pat[0:NE, 0:128], aff[:, tt, :], K.ident[:]), reads=["aff%d" % tt, "c_ident"], writes=["p3at"])
                P.op("act", lambda e, xb=xb: e.copy(out=affT[xb][:], in_=pat[0:NE, 0:128]), reads=["p3at"], writes=["affT%d" % xb])
                P.dma("sp", lambda e, xb=xb, t0=t0: e.dma_start(out=K.Sc["affT"][:, t0:t0 + 128], in_=affT[xb][:]), reads=["affT%d" % xb], writes=["affT_d"])
        akeys = ["aff%d" % tt for tt in range(NTT)]
        P.dma("sp", lambda e: e.dma_start(out=K.Sc["aff"].rearrange("(t p) n -> p t n", p=128), in_=aff[:]), reads=akeys, writes=["aff_d"])


def phase_moe(K, l):
    P, I, S = K.P, K.I, K.S
    NTT = S // 128
    CAP = S // 8
    NSL = min(128, CAP)
    NST = CAP // NSL
    h2, affd, affTd, ybuf, rankTd = K.Sc["h2"], K.Sc["aff"], K.Sc["affT"], K.Sc["ybuf"], K.Sc["rankT"]
    cast_i = [0]

    def cast(dst, src, rk, wk):
        eng = ("pool", "act")[cast_i[0] % 2]
        cast_i[0] += 1
        if eng == "pool":
            P.op("pool", lambda e: e.tensor_copy(out=dst, in_=src), reads=[rk], writes=[wk])
        else:
            P.op("act", lambda e: e.copy(out=dst, in_=src), reads=[rk], writes=[wk])

    with Phase(K) as ph:
        aff = ph.sb("m_aff", [128, NTT, NE])
        rank = ph.sb("m_rank", [128, NTT, NE])
        rankT = ph.sb("m_rankT", [NE, S])
        affb = [ph.sb("m_affb%d" % i, [128, S]) for i in range(2)]
        junk = [ph.sb("m_junk%d" % i, [128, S], BF16) for i in range(2)]
        prt = ph.ps("m_prt", [128, 512])
        P.dma("sp", lambda e: e.dma_start(out=aff[:], in_=affd.rearrange("(t p) n -> p t n", p=128)), reads=["aff_d"], writes=["m_aff"])
        for ex in range(NE):
            b = ex % 2
            P.dma("sp", lambda e, b=b, ex=ex: e.dma_start(out=affb[b][:], in_=affTd[ex:ex + 1, :].to_broadcast([128, S])),
                  reads=["affT_d"], writes=["m_affb%d" % b])
            for tt in range(NTT):
                P.op("dve", lambda e, b=b, tt=tt, ex=ex: e.tensor_scalar(
                    out=junk[b][:], in0=affb[b][:], scalar1=aff[:, tt, ex:ex + 1], scalar2=0.0, op0=ALU.is_gt, op1=ALU.add,
                    accum_out=rank[:, tt, ex:ex + 1]), reads=["m_affb%d" % b, "m_aff"], writes=["m_junk%d" % b, "m_rank_%d_%d" % (tt, ex)])
        rkeys = ["m_rank_%d_%d" % (tt, ex) for tt in range(NTT) for ex in range(NE)]
        for tt in range(NTT):
            P.op("pe", lambda e, tt=tt: e.transpose(prt[0:NE, 0:128], rank[:, tt, :], K.ident[:]), reads=rkeys + ["c_ident"], writes=["m_prt"])
            P.op("act", lambda e, tt=tt: e.copy(out=rankT[:, tt * 128:(tt + 1) * 128], in_=prt[0:NE, 0:128]), reads=["m_prt"], writes=["m_rankT"])
        P.dma("sp", lambda e: e.dma_start(out=rankTd, in_=rankT[:]), reads=["m_rankT"], writes=["rankT_d"])
        P.dma("sp", lambda e: e.dma_start(out=K.Sc["rank"].rearrange("(t p) n -> p t n", p=128), in_=rank[:]), reads=rkeys, writes=["rank_d"])
    with Phase(K) as ph:
        rank = ph.sb("e_rank", [128, NTT, NE])
        P.dma("sp", lambda e: e.dma_start(out=rank[:], in_=K.Sc["rank"].rearrange("(t p) n -> p t n", p=128)), reads=["rank_d"], writes=["e_rank"])
        h2h = ph.sb("e_h2h", [128, NTT, 512], BF16)
        Pm = ph.sb("e_Pm", [128, NTT, CAP], BF16)
        xgT = ph.sb("e_xgT", [128, 16, CAP], BF16)
        hidT = ph.sb("e_hidT", [128, 8, CAP], BF16)
        wst = [ph.sb("e_wst%d" % i, [128, 4096]) for i in range(3)]
        wbf = [ph.sb("e_wbf%d" % i, [128, 4096], BF16) for i in range(5)]
        s1 = [ph.sb("e_s1%d" % i, [128, CAP]) for i in range(2)]
        y = ph.sb("e_y", [NSL, NST, D], BF16)
        pg = [ph.ps("e_pg%d" % i, [128, 512]) for i in range(2)]
        p1 = [ph.ps("e_p1%d" % i, [128, 512]) for i in range(2)]
        p3 = [ph.ps("e_p3%d" % i, [128, 512]) for i in range(2)]
        py = [ph.ps("e_py%d" % i, [128, 512]) for i in range(2)]
        h2v = h2.rearrange("(t p) d -> p t d", p=128)
        nws = [0]
        nwb = [0]

        wl = []
        for ex_ in range(NE):
            for fq in range(4):
                wl.append((I["moe_w1"][l, ex_][:, fq * 256:(fq + 1) * 256], 16, 256))
                wl.append((I["moe_w3"][l, ex_][:, fq * 256:(fq + 1) * 256], 16, 256))
            for dc in range(4):
                wl.append((I["moe_w2"][l, ex_][:, dc * 512:(dc + 1) * 512], 8, 512))
        issued = [0]
        wviews = {}

        def ensure(j):
            while issued[0] <= min(j, len(wl) - 1):
                i = issued[0]
                issued[0] += 1
                src_ap, nk, ncol = wl[i]
                si, bi = i % 3, i % 5
                sv = wst[si][:, 0:nk * ncol].rearrange("p (k n) -> p k n", k=nk)
                bv = wbf[bi][:, 0:nk * ncol].rearrange("p (k n) -> p k n", k=nk)
                P.dma("sp", lambda e, sv=sv, src_ap=src_ap: e.dma_start(out=sv, in_=src_ap.rearrange("(k p) n -> p k n", p=128)),
                      writes=["e_wst%d" % si])
                cast(bv, sv, "e_wst%d" % si, "e_wbf%d" % bi)
                wviews[i] = (bv, "e_wbf%d" % bi)

        wpos = [0]

        def load_w(src_ap, nk, ncol):
            i = wpos[0]
            wpos[0] += 1
            ensure(i + 2)
            return wviews[i]

        for ex in range(NE):
            for tt in range(NTT):
                P.op("dve", lambda e, tt=tt, ex=ex: e.tensor_scalar(out=Pm[:, tt, :], in0=K.io[:, 0:CAP], scalar1=rank[:, tt, ex:ex + 1], scalar2=None,
                                                                    op0=ALU.is_equal), reads=["e_rank", "c_io"], writes=["e_Pm%d" % tt])
            pkeys = ["e_Pm%d" % tt for tt in range(NTT)]
            ng = 0
            ensure(wpos[0] + 1)
            for dh in range(4):
                P.dma("sp", lambda e, dh=dh: e.dma_start(out=h2h[:], in_=h2v[:, :, dh * 512:(dh + 1) * 512]), reads=["h2"], writes=["e_h2h"])
                for dt in range(4):
                    gi = ng % 2
                    ng += 1
                    for tt in range(NTT):
                        P.op("pe", lambda e, gi=gi, tt=tt, dt=dt: e.matmul(pg[gi][:, 0:CAP], lhsT=h2h[:, tt, dt * 128:(dt + 1) * 128], rhs=Pm[:, tt, :],
                                                                           start=(tt == 0), stop=(tt == NTT - 1)), reads=["e_h2h"] + pkeys, writes=["e_pg%d" % gi])
                    P.op("act", lambda e, gi=gi, dh=dh, dt=dt: e.copy(out=xgT[:, dh * 4 + dt, :], in_=pg[gi][:, 0:CAP]), reads=["e_pg%d" % gi], writes=["e_xgT"])
            nf = 0
            for fq in range(4):
                w1v, w1k = load_w(I["moe_w1"][l, ex][:, fq * 256:(fq + 1) * 256], 16, 256)
                w3v, w3k = load_w(I["moe_w3"][l, ex][:, fq * 256:(fq + 1) * 256], 16, 256)
                for j in range(2):
                    fi = nf % 2
                    nf += 1
                    for kc in range(16):
                        P.op("pe", lambda e, fi=fi, kc=kc, j=j, w1v=w1v: e.matmul(p1[fi][:, 0:CAP], lhsT=w1v[:, kc, j * 128:(j + 1) * 128], rhs=xgT[:, kc, :],
                                                                                 start=(kc == 0), stop=(kc == 15)), reads=[w1k, "e_xgT"], writes=["e_p1%d" % fi])
                    for kc in range(16):
                        P.op("pe", lambda e, fi=fi, kc=kc, j=j, w3v=w3v: e.matmul(p3[fi][:, 0:CAP], lhsT=w3v[:, kc, j * 128:(j + 1) * 128], rhs=xgT[:, kc, :],
                                                                                 start=(kc == 0), stop=(kc == 15)), reads=[w3k, "e_xgT"], writes=["e_p3%d" % fi])
                    P.op("act", lambda e, fi=fi: e.activation(out=s1[fi][:], in_=p1[fi][:, 0:CAP], func=AF.Silu), reads=["e_p1%d" % fi], writes=["e_s1%d" % fi])
                    P.op("dve", lambda e, fi=fi, fq=fq, j=j: e.tensor_tensor(out=hidT[:, fq * 2 + j, :], in0=p3[fi][:, 0:CAP], in1=s1[fi][:], op=ALU.mult),
                         reads=["e_p3%d" % fi, "e_s1%d" % fi], writes=["e_hidT"])
            ny = 0
            for dc in range(4):
                w2v, w2k = load_w(I["moe_w2"][l, ex][:, dc * 512:(dc + 1) * 512], 8, 512)
                for st in range(NST):
                    yi = ny % 2
                    ny += 1
                    for fc in range(8):
                        P.op("pe", lambda e, yi=yi, fc=fc, st=st, w2v=w2v: e.matmul(py[yi][0:NSL, :], lhsT=hidT[:, fc, st * NSL:(st + 1) * NSL], rhs=w2v[:, fc, :],
                                                                                   start=(fc == 0), stop=(fc == 7)), reads=[w2k, "e_hidT"], writes=["e_py%d" % yi])
                    P.op("dve", lambda e, yi=yi, st=st, dc=dc: e.tensor_copy(out=y[:, st, dc * 512:(dc + 1) * 512], in_=py[yi][0:NSL, :]),
                         reads=["e_py%d" % yi], writes=["e_y"])
            P.dma("sp", lambda e, ex=ex: e.dma_start(out=ybuf[ex].rearrange("(st p) d -> p st d", p=NSL), in_=y[:]), reads=["e_y"], writes=["ybuf"])
    with Phase(K) as ph:
        TB = min(256, S)
        NTB = TB // 128
        G2 = load_bcast_row(K, ph, "G2_b", K.Sc["modv"][l * 6 + 5:l * 6 + 6, :], D)
        sid = ph.sb("s_sid", [128, NST])
        for st in range(NST):
            P.op("dve", lambda e, st=st: e.tensor_scalar(out=sid[:, st:st + 1], in0=K.iop[:, 0:1], scalar1=float(st * NSL), scalar2=None, op0=ALU.add),
                 reads=["c_iop"], writes=["s_sid"])
        rb = [ph.sb("s_rb%d" % i, [128, TB]) for i in range(2)]
        ab = [ph.sb("s_ab%d" % i, [128, TB]) for i in range(2)]
        pgt = [ph.sb("s_pgt%d" % i, [128, TB], BF16) for i in range(3)]
        yt = [ph.sb("s_yt%d" % i, [128, D], BF16) for i in range(3)]
        xt = [ph.sb("s_xt%d" % i, [128, D]) for i in range(2)]
        tmp = [ph.sb("s_tmp%d" % i, [128, 512]) for i in range(2)]
        pa = [ph.ps("s_pa%d" % i, [128, 512]) for i in range(NTB * 4)]
        nb = 0
        for blk in range(S // TB):
            tok0 = blk * TB
            for ex in range(NE):
                bi = ex % 2
                P.dma("sp", lambda e, bi=bi, ex=ex, tok0=tok0: e.dma_start(out=rb[bi][0:NSL, :], in_=rankTd[ex:ex + 1, tok0:tok0 + TB].to_broadcast([NSL, TB])),
                      reads=["rankT_d"], writes=["s_rb%d" % bi])
                P.dma("sp", lambda e, bi=bi, ex=ex, tok0=tok0: e.dma_start(out=ab[bi][0:NSL, :], in_=affTd[ex:ex + 1, tok0:tok0 + TB].to_broadcast([NSL, TB])),
                      reads=["affT_d"], writes=["s_ab%d" % bi])
                for st in range(NST):
                    ci = nb % 3
                    nb += 1
                    first = (ex == 0 and st == 0)
                    last = (ex == NE - 1 and st == NST - 1)
                    P.op("dve", lambda e, ci=ci, bi=bi, st=st: e.scalar_tensor_tensor(out=pgt[ci][0:NSL, :], in0=rb[bi][0:NSL, :], scalar=sid[0:NSL, st:st + 1],
                                                                                    in1=ab[bi][0:NSL, :], op0=ALU.is_equal, op1=ALU.mult),
                         reads=["s_rb%d" % bi, "s_ab%d" % bi, "s_sid"], writes=["s_pgt%d" % ci])
                    P.dma("sp", lambda e, ci=ci, ex=ex, st=st: e.dma_start(out=yt[ci][0:NSL, :], in_=ybuf[ex][st * NSL:(st + 1) * NSL, :]),
                          reads=["ybuf"], writes=["s_yt%d" % ci])
                    for tb in range(NTB):
                        for dc in range(4):
                            P.op("pe", lambda e, ci=ci, tb=tb, dc=dc, first=first, last=last: e.matmul(
                                pa[tb * 4 + dc][:], lhsT=pgt[ci][0:NSL, tb * 128:(tb + 1) * 128], rhs=yt[ci][0:NSL, dc * 512:(dc + 1) * 512],
                                start=first, stop=last), reads=["s_pgt%d" % ci, "s_yt%d" % ci], writes=["s_pa%d" % (tb * 4 + dc)])
            for tb in range(NTB):
                t0 = tok0 + tb * 128
                xb = tb % 2
                P.dma("sp", lambda e, xb=xb, t0=t0: e.dma_start(out=xt[xb][:], in_=K.xres[t0:t0 + 128, :]), reads=["xres"], writes=["s_xt%d" % xb])
                for dc in range(4):
                    ti = dc % 2
                    P.op("dve", lambda e, ti=ti, tb=tb, dc=dc: e.tensor_tensor(out=tmp[ti][:], in0=pa[tb * 4 + dc][:], in1=G2[:, dc * 512:(dc + 1) * 512], op=ALU.mult),
                         reads=["s_pa%d" % (tb * 4 + dc), "G2_b"], writes=["s_tmp%d" % ti])
                    P.op("pool", lambda e, ti=ti, xb=xb, dc=dc: e.tensor_tensor(out=xt[xb][:, dc * 512:(dc + 1) * 512], in0=xt[xb][:, dc * 512:(dc + 1) * 512],
                                                                                in1=tmp[ti][:], op=ALU.add), reads=["s_tmp%d" % ti, "s_xt%d" % xb], writes=["s_xt%d" % xb])
                P.dma("sp", lambda e, xb=xb, t0=t0: e.dma_start(out=K.xres[t0:t0 + 128, :], in_=xt[xb][:]), reads=["s_xt%d" % xb], writes=["xres"])


def phase_rwkv_prep(K, l):
    P, I, S = K.P, K.I, K.S
    fm = K.Sc["fm"]
    TT = min(512, S)
    NCH = TT // CH
    R0 = 1536
    with Phase(K) as ph:
        mu = ph.sb("r_mu", [128, 15, 3])
        muv = I["rwkv_mu"][l]
        for m_ in range(2):
            for rt in range(15):
                P.dma("sp", lambda e, m_=m_, rt=rt: e.dma_start(out=mu[:, rt, m_:m_ + 1], in_=muv[m_:m_ + 1, rt * 128:(rt + 1) * 128].rearrange("o p -> p o")),
                      writes=["r_mu"])
        P.op("dve", lambda e: e.tensor_tensor(out=mu[:, :, 2:3], in0=mu[:, :, 0:1], in1=mu[:, :, 1:2], op=ALU.add), reads=["r_mu"], writes=["r_mu"])
        P.op("dve", lambda e: e.tensor_scalar(out=mu[:, :, 2:3], in0=mu[:, :, 2:3], scalar1=-1.0, scalar2=1.0, op0=ALU.mult, op1=ALU.add),
             reads=["r_mu"], writes=["r_mu"])
        pc = ph.sb("r_pc", [128, 4, 8])
        srcs = [I["rwkv_w0"][l][0:1, :], I["rwkv_w0"][l][1:2, :], I["rwkv_a0"][l][0:1, :], I["rwkv_a0"][l][1:2, :],
                I["rwkv_k_k"][l:l + 1, :], I["rwkv_k_a"][l:l + 1, :], None, I["rwkv_r_k"][l:l + 1, :]]
        for ci, src in enumerate(srcs):
            if src is None:
                continue
            for ct in range(4):
                P.dma("sp", lambda e, ci=ci, ct=ct, src=src: e.dma_start(out=pc[:, ct, ci:ci + 1], in_=src[:, ct * 128:(ct + 1) * 128].rearrange("o p -> p o")),
                      writes=["r_pc"])
        P.op("dve", lambda e: e.tensor_scalar(out=pc[:, :, 6:7], in0=pc[:, :, 5:6], scalar1=-1.0, scalar2=1.0, op0=ALU.mult, op1=ALU.add),
             reads=["r_pc"], writes=["r_pc"])
        wst = ph.sb("r_wst", [128, 512])
        w2p = [ph.sb("r_w2p%d" % d, [128, 512], BF16) for d in range(2)]
        a2p = [ph.sb("r_a2p%d" % d, [128, 512], BF16) for d in range(2)]
        g2b = ph.sb("r_g2b", [128, 512], BF16)
        for name, dst in (("rwkv_w2", w2p), ("rwkv_a2", a2p)):
            P.dma("sp", lambda e, name=name: e.dma_start(out=wst[:], in_=I[name][l]), writes=["r_wst"])
            for d in range(2):
                P.op("dve", lambda e, d=d, dst=dst: e.memset(dst[d][:], 0.0), writes=["r_lw%s%d" % (name, d)])
                P.op("dve", lambda e, d=d, dst=dst: e.tensor_copy(out=dst[d][d * 64:(d + 1) * 64, :], in_=wst[d * 64:(d + 1) * 64, :]),
                     reads=["r_wst"], writes=["r_lw%s%d" % (name, d)])
        P.dma("sp", lambda e: e.dma_start(out=wst[:], in_=I["rwkv_g2"][l]), writes=["r_wst"])
        P.op("dve", lambda e: e.tensor_copy(out=g2b[:], in_=wst[:]), reads=["r_wst"], writes=["r_g2b"])
        lwk = ["r_lwrwkv_w2%d" % d for d in range(2)] + ["r_lwrwkv_a2%d" % d for d in range(2)] + ["r_g2b"]
        bones = ph.sb("r_bones", [128, 128])
        P.op("dve", lambda e: e.memset(bones[:], 0.0), writes=["r_bones"])
        P.op("dve", lambda e: e.memset(bones[0:64, 0:64], 1.0), writes=["r_bones"])
        P.op("dve", lambda e: e.memset(bones[64:128, 64:128], 1.0), writes=["r_bones"])
        ones = ph.sb("r_ones", [128, CH])
        P.op("dve", lambda e: e.memset(ones[:], 1.0), writes=["r_ones"])
        tiny = ph.sb("r_tiny", [128, 1])
        P.op("dve", lambda e: e.memset(tiny[:], 1e-24), writes=["r_tiny"])
        xr = [ph.sb("r_xr%d" % i, [128, TT + 2]) for i in range(2)]
        z = [ph.sb("r_z%d" % i, [128, TT]) for i in range(15)]
        twb = ph.sb("r_twb", [128, TT], BF16)
        adb = ph.sb("r_adb", [128, TT], BF16)
        sgb = ph.sb("r_sgb", [128, TT], BF16)
        ld = [ph.sb("r_ld%d" % d, [128, TT]) for d in range(2)]
        av = [ph.sb("r_a%d" % d, [128, TT]) for d in range(2)]
        kd = [ph.sb("r_kd%d" % d, [128, TT]) for d in range(2)]
        bb = [ph.sb("r_bb%d" % d, [128, TT]) for d in range(2)]
        gt_ = ph.sb("r_g", [128, TT])
        kq = ph.sb("r_kq", [128, TT])
        kkt = ph.sb("r_kk", [128, TT])
        t1 = ph.sb("r_t1", [128, TT])
        t2 = ph.sb("r_t2", [128, TT])
        lam = ph.sb("r_lam", [128, TT])
        Lt = ph.sb("r_L", [128, TT])
        E = [ph.sb("r_E%d" % i, [128, TT]) for i in range(4)]
        outs = [ph.sb("r_o%d" % i, [128, TT]) for i in range(6)]
        gc = ph.sb("r_gc", [128, NCH])
        tro = [ph.sb("r_tro%d" % i, [128, 128]) for i in range(2)]
        pm = [ph.ps("r_pm%d" % i, [128, 512]) for i in range(4)]
        ptr = [ph.ps("r_ptr%d" % i, [128, 512]) for i in range(2)]
        npm = [0]
        ntr = [0]

        def transpose_store(src, skey, dst_dram, t0):
            for tb in range(TT // 128):
                i = ntr[0] % 2
                ntr[0] += 1
                P.op("pe", lambda e, i=i, tb=tb: e.transpose(ptr[i][:, 0:128], src[:, tb * 128:(tb + 1) * 128], K.ident[:]),
                     reads=[skey, "c_ident"], writes=["r_ptr%d" % i])
                P.op("act", lambda e, i=i: e.copy(out=tro[i][:], in_=ptr[i][:, 0:128]), reads=["r_ptr%d" % i], writes=["r_tro%d" % i])
                P.dma("sp", lambda e, i=i, tb=tb: e.dma_start(out=dst_dram[t0 + tb * 128:t0 + (tb + 1) * 128, :], in_=tro[i][:]),
                      reads=["r_tro%d" % i], writes=["r_scr"])

        for st in range(S // TT):
            t0 = st * TT
            lo, hi = max(t0 - 1, 0), min(t0 + TT + 1, S)
            for rt in range(15):
                b = rt % 2
                k = "r_xr%d" % b
                P.op("pool", lambda e, b=b: e.memset(xr[b][:, 0:1], 0.0), writes=[k])
                P.op("pool", lambda e, b=b: e.memset(xr[b][:, TT + 1:TT + 2], 0.0), writes=[k])
                P.dma("sp", lambda e, b=b, rt=rt, lo=lo, hi=hi, t0=t0: e.dma_start(
                    out=xr[b][:, lo - (t0 - 1):hi - (t0 - 1)], in_=fm[R0 + rt * 128:R0 + (rt + 1) * 128, lo:hi]), reads=["fm"], writes=[k])
                zk = "r_z%d" % rt
                P.op("dve", lambda e, b=b, rt=rt: e.tensor_scalar(out=z[rt][:], in0=xr[b][:, 1:TT + 1], scalar1=mu[:, rt, 2:3], scalar2=None, op0=ALU.mult),
                     reads=[k, "r_mu"], writes=[zk])
                P.op("dve", lambda e, b=b, rt=rt: e.scalar_tensor_tensor(out=z[rt][:], in0=xr[b][:, 0:TT], scalar=mu[:, rt, 0:1], in1=z[rt][:],
                                                                        op0=ALU.mult, op1=ALU.add), reads=[k, zk], writes=[zk])
                P.op("dve", lambda e, b=b, rt=rt: e.scalar_tensor_tensor(out=z[rt][:], in0=xr[b][:, 2:TT + 2], scalar=mu[:, rt, 1:2], in1=z[rt][:],
                                                                        op0=ALU.mult, op1=ALU.add), reads=[k, zk], writes=[zk])
            P.op("act", lambda e: e.activation(out=twb[:], in_=z[12][:], func=AF.Tanh), reads=["r_z12"], writes=["r_twb"])
            P.op("act", lambda e: e.copy(out=adb[:], in_=z[13][:]), reads=["r_z13"], writes=["r_adb"])
            P.op("act", lambda e: e.activation(out=sgb[:], in_=z[14][:], func=AF.Sigmoid), reads=["r_z14"], writes=["r_sgb"])
            for ct in range(4):
                zr, zk_, zv = z[ct], z[4 + ct], z[8 + ct]
                kr, kk_, kv = "r_z%d" % ct, "r_z%d" % (4 + ct), "r_z%d" % (8 + ct)
                cs = slice(ct * 128, (ct + 1) * 128)
                for d in range(2):
                    pi = npm[0] % 4
                    npm[0] += 1
                    P.op("pe", lambda e, pi=pi, d=d, cs=cs: e.matmul(pm[pi][:, 0:TT], lhsT=w2p[d][:, cs], rhs=twb[:], start=True, stop=True),
                         reads=lwk + ["r_twb"], writes=["r_pm%d" % pi])
                    P.op("act", lambda e, pi=pi, d=d, ct=ct: e.activation(out=ld[d][:], in_=pm[pi][:, 0:TT], func=AF.Sigmoid, bias=pc[:, ct, d:d + 1]),
                         reads=["r_pm%d" % pi, "r_pc"], writes=["r_ld%d" % d])
                    P.op("dve", lambda e, d=d: e.tensor_scalar(out=ld[d][:], in0=ld[d][:], scalar1=-0.6065306597126334, scalar2=None, op0=ALU.mult),
                         reads=["r_ld%d" % d], writes=["r_ld%d" % d])
                    pi = npm[0] % 4
                    npm[0] += 1
                    P.op("pe", lambda e, pi=pi, d=d, cs=cs: e.matmul(pm[pi][:, 0:TT], lhsT=a2p[d][:, cs], rhs=adb[:], start=True, stop=True),
                         reads=lwk + ["r_adb"], writes=["r_pm%d" % pi])
                    P.op("act", lambda e, pi=pi, d=d, ct=ct: e.activation(out=av[d][:], in_=pm[pi][:, 0:TT], func=AF.Sigmoid, bias=pc[:, ct, 2 + d:3 + d]),
                         reads=["r_pm%d" % pi, "r_pc"], writes=["r_a%d" % d])
                pi = npm[0] % 4
                npm[0] += 1
                P.op("pe", lambda e, pi=pi, cs=cs: e.matmul(pm[pi][:, 0:TT], lhsT=g2b[:, cs], rhs=sgb[:], start=True, stop=True),
                     reads=lwk + ["r_sgb"], writes=["r_pm%d" % pi])
                P.op("act", lambda e, pi=pi: e.copy(out=gt_[:], in_=pm[pi][:, 0:TT]), reads=["r_pm%d" % pi], writes=["r_g"])
                P.dma("sp", lambda e, cs=cs, t0=t0: e.dma_start(out=K.Sc["rg"][cs, t0:t0 + TT], in_=gt_[:]), reads=["r_g"], writes=["r_scr"])
                P.op("dve", lambda e, ct=ct, zk_=zk_: e.tensor_scalar(out=kq[:], in0=zk_[:], scalar1=pc[:, ct, 4:5], scalar2=None, op0=ALU.mult),
                     reads=[kk_, "r_pc"], writes=["r_kq"])
                P.op("pool", lambda e: e.tensor_tensor(out=t1[:], in0=kq[:], in1=kq[:], op=ALU.mult), reads=["r_kq"], writes=["r_t1"])
                pi = npm[0] % 4
                npm[0] += 1
                P.op("pe", lambda e, pi=pi: e.matmul(pm[pi][:, 0:TT], lhsT=bones[:], rhs=t1[:], start=True, stop=True),
                     reads=["r_bones", "r_t1"], writes=["r_pm%d" % pi])
                P.op("act", lambda e, pi=pi: e.activation(out=t2[:], in_=pm[pi][:, 0:TT], func=AF.Sqrt, bias=tiny[:, 0:1]),
                     reads=["r_pm%d" % pi, "r_tiny"], writes=["r_t2"])
                P.op("dve", lambda e: e.reciprocal(out=t2[:], in_=t2[:]), reads=["r_t2"], writes=["r_t2"])
                P.op("dve", lambda e: e.tensor_tensor(out=kkt[:], in0=kq[:], in1=t2[:], op=ALU.mult), reads=["r_kq", "r_t2"], writes=["r_kk"])
                for d in range(2):
                    P.op("dve", lambda e, d=d, ct=ct: e.tensor_scalar(out=t1[:], in0=av[d][:], scalar1=pc[:, ct, 5:6], scalar2=pc[:, ct, 6:7],
                                                                    op0=ALU.mult, op1=ALU.add), reads=["r_a%d" % d, "r_pc"], writes=["r_t1"])
                    P.op("dve", lambda e, d=d, zk_=zk_: e.tensor_tensor(out=kd[d][:], in0=zk_[:], in1=t1[:], op=ALU.mult), reads=[kk_, "r_t1"], writes=["r_kd%d" % d])
                    P.op("pool", lambda e, d=d: e.tensor_tensor(out=bb[d][:], in0=av[d][:], in1=kkt[:], op=ALU.mult), reads=["r_a%d" % d, "r_kk"], writes=["r_bb%d" % d])
                P.op("pool", lambda e: e.tensor_tensor(out=t1[:], in0=kd[0][:], in1=kd[1][:], op=ALU.add), reads=["r_kd0", "r_kd1", "r_t1"], writes=["r_t1"])
                P.op("dve", lambda e, ct=ct, zr=zr: e.scalar_tensor_tensor(out=t1[:], in0=t1[:], scalar=pc[:, ct, 7:8], in1=zr[:], op0=ALU.mult, op1=ALU.mult),
                     reads=["r_t1", kr, "r_pc"], writes=["r_t1"])
                pi = npm[0] % 4
                npm[0] += 1
                P.op("pe", lambda e, pi=pi: e.matmul(pm[pi][:, 0:TT], lhsT=bones[:], rhs=t1[:], start=True, stop=True),
                     reads=["r_bones", "r_t1"], writes=["r_pm%d" % pi])
                P.op("dve", lambda e, pi=pi, zv=zv: e.tensor_tensor(out=t2[:], in0=pm[pi][:, 0:TT], in1=zv[:], op=ALU.mult), reads=["r_pm%d" % pi, kv], writes=["r_t2"])
                P.dma("sp", lambda e, cs=cs, t0=t0: e.dma_start(out=K.Sc["rbon"][cs, t0:t0 + TT], in_=t2[:]), reads=["r_t2"], writes=["r_scr"])
                transpose_store(zv, kv, K.Sc["rv"][:, cs], t0)
                for d in range(2):
                    for c in range(NCH):
                        P.op("dve", lambda e, c=c, d=d: e.tensor_tensor_scan(out=lam[:, c * CH:(c + 1) * CH], data0=ones[:, 0:CH], data1=ld[d][:, c * CH:(c + 1) * CH],
                                                                             initial=0.0, op0=ALU.mult, op1=ALU.add), reads=["r_ld%d" % d, "r_ones"], writes=["r_lam"])
                    lam3 = lam[:].rearrange("p (c t) -> p c t", t=CH)
                    lamC = lam3[:, :, CH - 1:CH].to_broadcast([128, NCH, CH])
                    L3 = Lt[:].rearrange("p (c t) -> p c t", t=CH)
                    if d == 0:
                        P.op("pool", lambda e: e.tensor_copy(out=Lt[:], in_=lam[:]), reads=["r_lam"], writes=["r_L"])
                    else:
                        P.op("dve", lambda e, L3=L3, lamC=lamC, lam3=lam3: e.tensor_tensor(out=L3, in0=lamC, in1=lam3, op=ALU.subtract), reads=["r_lam"], writes=["r_L"])
                        P.op("dve", lambda e, d=d: e.tensor_tensor(out=Lt[:], in0=Lt[:], in1=ld[d][:], op=ALU.add), reads=["r_L", "r_ld%d" % d], writes=["r_L"])
                    P.op("act", lambda e: e.activation(out=E[0][:], in_=Lt[:], func=AF.Exp, scale=-1.0), reads=["r_L"], writes=["r_E0"])
                    P.op("act", lambda e: e.activation(out=E[2][:], in_=Lt[:], func=AF.Exp), reads=["r_L"], writes=["r_E2"])
                    P.op("dve", lambda e, d=d: e.tensor_tensor(out=t1[:], in0=Lt[:], in1=ld[d][:], op=ALU.subtract), reads=["r_L", "r_ld%d" % d, "r_t1"], writes=["r_t1"])
                    P.op("act", lambda e: e.activation(out=E[1][:], in_=t1[:], func=AF.Exp), reads=["r_t1"], writes=["r_E1"])
                    P.op("dve", lambda e, L3=L3, lamC=lamC: e.tensor_tensor(out=t2[:].rearrange("p (c t) -> p c t", t=CH), in0=lamC, in1=L3, op=ALU.subtract),
                         reads=["r_L", "r_lam", "r_t2"], writes=["r_t2"])
                    P.op("act", lambda e: e.activation(out=E[3][:], in_=t2[:], func=AF.Exp), reads=["r_t2"], writes=["r_E3"])
                    P.op("act", lambda e, lam3=lam3: e.activation(out=gc[:], in_=lam3[:, :, CH - 1], func=AF.Exp), reads=["r_lam"], writes=["r_gc"])
                    P.dma("sp", lambda e, d=d, cs=cs, st=st: e.dma_start(out=K.Sc["rgc"][d][cs, st * NCH:(st + 1) * NCH], in_=gc[:]), reads=["r_gc"], writes=["r_scr"])
                    prods = [(kd[d], "r_kd%d" % d, 0, "dve"), (bb[d], "r_bb%d" % d, 0, "pool"), (kkt, "r_kk", 1, "dve"), (zr, kr, 2, "pool"),
                             (kd[d], "r_kd%d" % d, 3, "dve"), (bb[d], "r_bb%d" % d, 3, "pool")]
                    for oi, (src, skey, ei, eng) in enumerate(prods):
                        P.op(eng, lambda e, oi=oi, src=src, ei=ei: e.tensor_tensor(out=outs[oi][:], in0=src[:], in1=E[ei][:], op=ALU.mult),
                             reads=[skey, "r_E%d" % ei], writes=["r_o%d" % oi])
                    for oi in range(4):
                        P.dma("sp", lambda e, oi=oi, d=d, cs=cs, t0=t0: e.dma_start(out=K.Sc["rf"][d][oi][cs, t0:t0 + TT], in_=outs[oi][:]),
                              reads=["r_o%d" % oi], writes=["r_scr"])
                    transpose_store(outs[4], "r_o4", K.Sc["rt"][d][0][:, cs], t0)
                    transpose_store(outs[5], "r_o5", K.Sc["rt"][d][1][:, cs], t0)


def phase_rwkv_scan(K, l):
    P, S = K.P, K.S
    NC = S // CH
    with Phase(K) as ph:
        msk = {}
        for name, op in (("su", ALU.is_gt), ("iu", ALU.is_ge), ("sl", ALU.is_lt), ("il", ALU.is_le), ("eye", ALU.is_equal)):
            m = ph.sb("k_" + name, [CH, CH])
            P.op("dve", lambda e, m=m, op=op: e.tensor_scalar(out=m[:], in0=K.io[0:CH, 0:CH], scalar1=K.iop[0:CH, 0:1], scalar2=None, op0=op),
                 reads=["c_io", "c_iop"], writes=["k_msk"])
            msk[name] = m
        bc = lambda m: m[:].unsqueeze(1).to_broadcast([CH, 8, CH])
        A = [ph.ps("k_A%d" % i, [CH, 8, CH]) for i in range(5)]
        Bp = [ph.ps("k_B%d" % i, [CH, 8, CH]) for i in range(3)]
        ST = [ph.sb("k_ST%d" % d, [CH, 8, CH]) for d in range(2)]
        for d in range(2):
            P.op("dve", lambda e, d=d: e.memset(ST[d][:], 0.0), writes=["k_ST%d" % d])
        fmt = [[ph.sb("k_f%d_%d" % (d, i), [CH, 8, CH]) for i in range(4)] for d in range(2)]
        tmt = [[ph.sb("k_t%d_%d" % (d, i), [CH, 8 * CH]) for i in range(3)] for d in range(2)]
        gcs = [ph.sb("k_gc%d" % d, [CH, 8]) for d in range(2)]
        Mk = ph.sb("k_Mk", [CH, 8, CH])
        Ak = ph.sb("k_Ak", [CH, 8, CH])
        Abn = ph.sb("k_Abn", [CH, 8, CH])
        Q = [ph.sb("k_Q%d" % i, [CH, 8, CH]) for i in range(2)]
        QT = [ph.sb("k_QT%d" % i, [CH, 8, CH]) for i in range(2)]
        Tc = [ph.sb("k_Tc%d" % i, [CH, 8, CH]) for i in range(2)]
        W = ph.sb("k_W", [CH, 8, CH])
        Un = ph.sb("k_Un", [CH, 8, CH])
        Y = [ph.sb("k_Y%d" % i, [CH, 8 * CH]) for i in range(2)]
        tmpS = ph.sb("k_tmpS", [CH, 8, CH])

        def mm8(out_ps, okey, fn_l, fn_r, rkeys):
            for h in range(8):
                P.op("pe", lambda e, h=h: e.matmul(out_ps[:, h, :], lhsT=fn_l(h), rhs=fn_r(h), start=True, stop=True), reads=rkeys, writes=[okey])

        for step in range(NC):
            for d in range(2):
                c = step if d == 0 else NC - 1 - step
                cs = slice(c * CH, (c + 1) * CH)
                mS, mI, mST = (msk["su"], msk["iu"], msk["sl"]) if d == 0 else (msk["sl"], msk["il"], msk["su"])
                f = fmt[d]
                t = tmt[d]
                fk = ["k_f%d_%d" % (d, i) for i in range(4)]
                tk = ["k_t%d_%d" % (d, i) for i in range(3)]
                for i in range(4):
                    P.dma("sp", lambda e, i=i, d=d, cs=cs: e.dma_start(out=f[i][:], in_=K.Sc["rf"][d][i].rearrange("(h j) s -> j h s", j=CH)[:, :, cs]),
                          reads=["r_scr"], writes=[fk[i]])
                for i in range(2):
                    P.dma("sp", lambda e, i=i, d=d, cs=cs: e.dma_start(out=t[i][:], in_=K.Sc["rt"][d][i][cs, :]), reads=["r_scr"], writes=[tk[i]])
                P.dma("sp", lambda e, cs=cs: e.dma_start(out=t[2][:], in_=K.Sc["rv"][cs, :]), reads=["r_scr"], writes=[tk[2]])
                P.dma("sp", lambda e, d=d, c=c: e.dma_start(out=gcs[d][:], in_=K.Sc["rgc"][d].rearrange("(h j) c -> j h c", j=CH)[:, :, c],
                                                                allow_slow_non_contiguous=True), reads=["r_scr"], writes=["k_gc%d" % d])
                Kt, Bt, Qt, Rt = f
                Kh, Bh, V = t
                hs = lambda h: slice(h * CH, (h + 1) * CH)
                mm8(A[0], "k_A0", lambda h: Kt[:, h, :], lambda h: Qt[:, h, :], [fk[0], fk[2]])
                mm8(A[1], "k_A1", lambda h: Bt[:, h, :], lambda h: Qt[:, h, :], [fk[1], fk[2]])
                mm8(A[2], "k_A2", lambda h: Qt[:, h, :], lambda h: Bt[:, h, :], [fk[1], fk[2]])
                mm8(A[3], "k_A3", lambda h: Kt[:, h, :], lambda h: Rt[:, h, :], [fk[0], fk[3]])
                mm8(A[4], "k_A4", lambda h: Bt[:, h, :], lambda h: Rt[:, h, :], [fk[1], fk[3]])
                P.op("dve", lambda e, mS=mS: e.tensor_tensor(out=Mk[:], in0=A[0][:], in1=bc(mS), op=ALU.mult), reads=["k_A0", "k_msk"], writes=["k_Mk"])
                P.op("dve", lambda e, mS=mS: e.tensor_tensor(out=Q[0][:], in0=A[1][:], in1=bc(mS), op=ALU.mult), reads=["k_A1", "k_msk"], writes=["k_Q0"])
                P.op("dve", lambda e, mST=mST: e.tensor_tensor(out=QT[0][:], in0=A[2][:], in1=bc(mST), op=ALU.mult), reads=["k_A2", "k_msk"], writes=["k_QT0"])
                P.op("dve", lambda e, mI=mI: e.tensor_tensor(out=Ak[:], in0=A[3][:], in1=bc(mI), op=ALU.mult), reads=["k_A3", "k_msk"], writes=["k_Ak"])
                P.op("dve", lambda e, mI=mI: e.tensor_tensor(out=Abn[:], in0=A[4][:], in1=bc(mI), op=ALU.mult),
                     reads=["k_A4", "k_msk"], writes=["k_Abn"])
                P.op("pool", lambda e: e.tensor_tensor(out=Tc[0][:], in0=bc(msk["eye"]), in1=Q[0][:], op=ALU.subtract), reads=["k_Q0", "k_msk"], writes=["k_Tc0"])
                qi, ti = 0, 0
                for lev in range(1, 6):
                    qn = 1 - qi
                    if lev < 5:
                        mm8(A[0], "k_A0", lambda h, qi=qi: QT[qi][:, h, :], lambda h, qi=qi: Q[qi][:, h, :], ["k_Q%d" % qi, "k_QT%d" % qi])
                    mm8(A[1], "k_A1", lambda h, qi=qi: Q[qi][:, h, :], lambda h, qi=qi: QT[qi][:, h, :], ["k_Q%d" % qi, "k_QT%d" % qi])
                    if lev < 5:
                        P.op("act", lambda e, qn=qn: e.copy(out=Q[qn][:], in_=A[0][:]), reads=["k_A0"], writes=["k_Q%d" % qn])
                    P.op("dve", lambda e, qn=qn: e.tensor_copy(out=QT[qn][:], in_=A[1][:]), reads=["k_A1"], writes=["k_QT%d" % qn])
                    tn = 1 - ti
                    mm8(A[2], "k_A2", lambda h, qn=qn: QT[qn][:, h, :], lambda h, ti=ti: Tc[ti][:, h, :], ["k_QT%d" % qn, "k_Tc%d" % ti])
                    P.op("dve", lambda e, tn=tn, ti=ti: e.tensor_tensor(out=Tc[tn][:], in0=A[2][:], in1=Tc[ti][:], op=ALU.add),
                         reads=["k_A2", "k_Tc%d" % ti], writes=["k_Tc%d" % tn])
                    qi, ti = qn, tn
                Tf, Tfk = Tc[ti], "k_Tc%d" % ti
                sk = "k_ST%d" % d
                for h in range(8):
                    P.op("pe", lambda e, h=h, d=d: e.matmul(Bp[0][:, h, :], lhsT=Qt[:, h, :], rhs=ST[d][:, h, :], start=True, stop=False),
                         reads=[fk[2], sk], writes=["k_B0"])
                    P.op("pe", lambda e, h=h: e.matmul(Bp[0][:, h, :], lhsT=Mk[:, h, :], rhs=V[:, hs(h)], start=False, stop=True),
                         reads=["k_Mk", tk[2]], writes=["k_B0"])
                P.op("act", lambda e: e.copy(out=W[:], in_=Bp[0][:]), reads=["k_B0"], writes=["k_W"])
                mm8(Bp[0], "k_B0", lambda h: Tf[:, h, :], lambda h: W[:, h, :], [Tfk, "k_W"])
                P.op("act", lambda e: e.activation(out=Un[:], in_=Bp[0][:], func=AF.Copy, scale=-1.0), reads=["k_B0"], writes=["k_Un"])
                for h in range(8):
                    P.op("pe", lambda e, h=h: e.matmul(Bp[1][:, h, :], lhsT=Kh[:, hs(h)], rhs=V[:, hs(h)], start=True, stop=False),
                         reads=[tk[0], tk[2]], writes=["k_B1"])
                    P.op("pe", lambda e, h=h: e.matmul(Bp[1][:, h, :], lhsT=Bh[:, hs(h)], rhs=Un[:, h, :], start=False, stop=True),
                         reads=[tk[1], "k_Un"], writes=["k_B1"])
                for h in range(8):
                    P.op("pe", lambda e, h=h, d=d: e.matmul(Bp[2][:, h, :], lhsT=Rt[:, h, :], rhs=ST[d][:, h, :], start=True, stop=False),
                         reads=[fk[3], sk], writes=["k_B2"])
                    P.op("pe", lambda e, h=h: e.matmul(Bp[2][:, h, :], lhsT=Ak[:, h, :], rhs=V[:, hs(h)], start=False, stop=False),
                         reads=["k_Ak", tk[2]], writes=["k_B2"])
                    P.op("pe", lambda e, h=h: e.matmul(Bp[2][:, h, :], lhsT=Abn[:, h, :], rhs=Un[:, h, :], start=False, stop=True),
                         reads=["k_Abn", "k_Un"], writes=["k_B2"])
                yi = (step * 2 + d) % 2
                P.op("act", lambda e, yi=yi: e.copy(out=Y[yi][:], in_=Bp[2][:].rearrange("p h i -> p (h i)")), reads=["k_B2"], writes=["k_Y%d" % yi])
                P.dma("sp", lambda e, yi=yi, d=d, cs=cs: e.dma_start(out=K.Sc["ry"][d][cs, :], in_=Y[yi][:]), reads=["k_Y%d" % yi], writes=["r_y"])
                P.op("dve", lambda e, d=d: e.tensor_tensor(out=tmpS[:], in0=ST[d][:], in1=gcs[d][:].unsqueeze(2).to_broadcast([CH, 8, CH]), op=ALU.mult),
                     reads=[sk, "k_gc%d" % d], writes=["k_tmpS"])
                P.op("dve", lambda e, d=d: e.tensor_tensor(out=ST[d][:], in0=tmpS[:], in1=Bp[1][:], op=ALU.add), reads=["k_tmpS", "k_B1"], writes=[sk])


def phase_rwkv_post(K, l):
    P, I, S = K.P, K.I, K.S
    TT = min(512, S)
    with Phase(K) as ph:
        eps_t = ph.sb("q_eps", [128, 1])
        P.op("dve", lambda e: e.memset(eps_t[:], GN_EPS), writes=["q_eps"])
        lnp = ph.sb("q_lnp", [128, 4, 2])
        for ci, name in enumerate(("rwkv_ln_w", "rwkv_ln_b")):
            for ct in range(4):
                P.dma("sp", lambda e, ci=ci, ct=ct, name=name: e.dma_start(out=lnp[:, ct, ci:ci + 1],
                                                                         in_=I[name][l:l + 1, ct * 128:(ct + 1) * 128].rearrange("o p -> p o")), writes=["q_lnp"])
        y0 = [ph.sb("q_y0%d" % i, [128, 512]) for i in range(2)]
        y1 = [ph.sb("q_y1%d" % i, [128, 512]) for i in range(2)]
        sq = ph.sb("q_sq", [128, 512])
        mean = ph.sb("q_mean", [128, 8])
        var = ph.sb("q_var", [128, 8])
        bon = [ph.sb("q_bon%d" % i, [128, TT]) for i in range(2)]
        gg = [ph.sb("q_g%d" % i, [128, TT]) for i in range(2)]
        o1 = [ph.sb("q_o1%d" % i, [128, TT]) for i in range(2)]
        ob = [ph.sb("q_ob%d" % i, [128, TT], BF16) for i in range(2)]
        pT = [ph.ps("q_pT%d" % i, [128, 512]) for i in range(4)]
        for st in range(S // TT):
            t0 = st * TT
            for tb in range(TT // 128):
                b = tb % 2
                r0 = t0 + tb * 128
                P.dma("sp", lambda e, b=b, r0=r0: e.dma_start(out=y0[b][:], in_=K.Sc["ry"][0][r0:r0 + 128, :]), reads=["r_y"], writes=["q_y0%d" % b])
                P.dma("sp", lambda e, b=b, r0=r0: e.dma_start(out=y1[b][:], in_=K.Sc["ry"][1][r0:r0 + 128, :]), reads=["r_y"], writes=["q_y1%d" % b])
                yk = "q_y0%d" % b
                y3 = y0[b][:].rearrange("p (h i) -> p h i", h=8)
                P.op("pool", lambda e, b=b: e.tensor_tensor(out=y0[b][:], in0=y0[b][:], in1=y1[b][:], op=ALU.add), reads=[yk, "q_y1%d" % b], writes=[yk])
                P.op("dve", lambda e, y3=y3: e.tensor_reduce(out=mean[:], in_=y3, axis=AX.X, op=ALU.add), reads=[yk], writes=["q_mean"])
                P.op("dve", lambda e: e.tensor_scalar(out=mean[:], in0=mean[:], scalar1=1.0 / 64, scalar2=None, op0=ALU.mult), reads=["q_mean"], writes=["q_mean"])
                P.op("dve", lambda e, y3=y3: e.tensor_tensor(out=y3, in0=y3, in1=mean[:].unsqueeze(2).to_broadcast([128, 8, 64]), op=ALU.subtract),
                     reads=[yk, "q_mean"], writes=[yk])
                P.op("pool", lambda e, b=b: e.tensor_tensor(out=sq[:], in0=y0[b][:], in1=y0[b][:], op=ALU.mult), reads=[yk], writes=["q_sq"])
                P.op("dve", lambda e: e.tensor_reduce(out=var[:], in_=sq[:].rearrange("p (h i) -> p h i", h=8), axis=AX.X, op=ALU.add), reads=["q_sq"], writes=["q_var"])
                P.op("act", lambda e: e.activation(out=var[:], in_=var[:], func=AF.Sqrt, scale=1.0 / 64, bias=eps_t[:, 0:1]), reads=["q_var", "q_eps"], writes=["q_var"])
                P.op("dve", lambda e: e.reciprocal(out=var[:], in_=var[:]), reads=["q_var"], writes=["q_var"])
                P.op("dve", lambda e, y3=y3: e.tensor_tensor(out=y3, in0=y3, in1=var[:].unsqueeze(2).to_broadcast([128, 8, 64]), op=ALU.mult),
                     reads=[yk, "q_var"], writes=[yk])
                for ct in range(4):
                    P.op("pe", lambda e, ct=ct, b=b, tb=tb: e.transpose(pT[ct][:, tb * 128:(tb + 1) * 128], y0[b][:, ct * 128:(ct + 1) * 128], K.ident[:]),
                         reads=[yk, "c_ident"], writes=["q_pT%d" % ct])
            for ct in range(4):
                b = ct % 2
                cs = slice(ct * 128, (ct + 1) * 128)
                P.dma("sp", lambda e, b=b, cs=cs, t0=t0: e.dma_start(out=bon[b][:], in_=K.Sc["rbon"][cs, t0:t0 + TT]), reads=["r_scr"], writes=["q_bon%d" % b])
                P.dma("sp", lambda e, b=b, cs=cs, t0=t0: e.dma_start(out=gg[b][:], in_=K.Sc["rg"][cs, t0:t0 + TT]), reads=["r_scr"], writes=["q_g%d" % b])
                P.op("dve", lambda e, b=b, ct=ct: e.tensor_scalar(out=o1[b][:], in0=pT[ct][:, 0:TT], scalar1=lnp[:, ct, 0:1], scalar2=lnp[:, ct, 1:2],
                                                                op0=ALU.mult, op1=ALU.add), reads=["q_pT%d" % ct, "q_lnp"], writes=["q_o1%d" % b])
                P.op("pool", lambda e, b=b: e.tensor_tensor(out=o1[b][:], in0=o1[b][:], in1=bon[b][:], op=ALU.add), reads=["q_o1%d" % b, "q_bon%d" % b], writes=["q_o1%d" % b])
                P.op("dve", lambda e, b=b: e.tensor_tensor(out=ob[b][:], in0=o1[b][:], in1=gg[b][:], op=ALU.mult), reads=["q_o1%d" % b, "q_g%d" % b], writes=["q_ob%d" % b])
                P.dma("sp", lambda e, b=b, ct=ct, t0=t0: e.dma_start(out=K.Sc["br"][1024 + ct * 128:1024 + (ct + 1) * 128, t0:t0 + TT], in_=ob[b][:]),
                      reads=["q_ob%d" % b], writes=["br"])


def rope_tables(S):
    t = np.arange(S)
    row = (t // 64).astype(np.float32)
    col = (t % 64).astype(np.float32)
    outs = []
    for n_pairs in (32, 16):
        n_axis = n_pairs // 2
        freqs = np.power(np.float32(10000.0), -np.arange(n_axis, dtype=np.float32) / n_axis).astype(np.float32)
        ang = np.concatenate([row[:, None] * freqs, col[:, None] * freqs], axis=-1).astype(np.float32)
        outs.append(np.concatenate([np.cos(ang), np.sin(ang)], axis=-1).astype(np.float32))
    return outs[0], outs[1]


_NC_CACHE = {}


def kernel(**inputs):
    x = np.ascontiguousarray(np.asarray(inputs["x"], dtype=np.float32))
    B, S, _ = x.shape
    L = int(np.asarray(inputs["w_in"]).shape[0])
    key = (S, L)
    if key not in _NC_CACHE:
        _NC_CACHE[key] = build(Cfg(S=S, L=L))
    nc = _NC_CACHE[key]
    f32 = lambda a: np.ascontiguousarray(np.asarray(a, dtype=np.float32))
    shared = {}
    for name in ("w_mod", "norm1_g", "norm2_g", "w_in", "conv_w", "gqa_q_norm", "gqa_k_norm", "rwkv_mu", "rwkv_w0", "rwkv_a0",
                 "rwkv_g2", "rwkv_k_k", "rwkv_k_a", "rwkv_ln_w", "rwkv_ln_b", "mla_qc_norm", "mla_kvc_norm", "mla_w_uq",
                 "mla_w_ukv", "mla_q_norm", "mla_k_norm", "w_out", "w_router", "moe_w1", "moe_w3", "moe_w2"):
        shared[name] = f32(inputs[name])
    shared["mod_table"] = f32(inputs["mod_table"]).reshape(L, 6 * D)
    shared["rwkv_w2"] = f32(inputs["rwkv_w2"]).reshape(L, 128, 512)
    shared["rwkv_a2"] = f32(inputs["rwkv_a2"]).reshape(L, 128, 512)
    shared["rwkv_r_k"] = f32(inputs["rwkv_r_k"]).reshape(L, 512)
    shared["w_branch"] = f32(inputs["w_branch"]).reshape(L, 4 * 512, D)
    rg, rm = rope_tables(S)
    shared["rope_g"], shared["rope_m"] = rg, rm
    c = f32(inputs["c"])
    in_maps = []
    for b in range(B):
        m = dict(shared)
        m["x"] = x[b]
        m["c"] = c[b]
        in_maps.append(m)
    res = run_bass_kernel_spmd(nc, in_maps, core_ids=list(range(B)))
    return np.stack([np.asarray(r["out"], dtype=np.float32) for r in res.results], axis=0)
```

```python
import contextlib
import numpy as np
import concourse.bass as bass
import concourse.mybir as mybir
from concourse.bass_utils import run_bass_kernel_spmd

F32 = mybir.dt.float32
BF16 = mybir.dt.bfloat16
ALU = mybir.AluOpType
AF = mybir.ActivationFunctionType
AX = mybir.AxisListType

D = 2048
NKC = D // 128
NORM_EPS = 1e-6
GN_EPS = 64e-5
IN_WIDTH = 13088
C_CONV, C_GQA, C_RWKV, C_MLA, C_GATE = 0, 1536, 2304, 4224, 4896
NE = 16
EH = 1024
CH = 64

NDMA_SEMS = 12


class Prog:
    ENGS = ("pe", "act", "dve", "pool", "sp")

    def __init__(self, nc):
        self.nc = nc
        self.ops = {e: [] for e in self.ENGS}
        self.cnt = {e: 0 for e in self.ENGS}
        self.seen = {e: {} for e in self.ENGS}
        self.state = {}
        self.dma_n = {e: 0 for e in self.ENGS}
        self.dma_exp = {}
        self.pending = {e: [] for e in self.ENGS}
        self.nops = 0

    def _deps(self, reads, writes):
        toks = []
        for k in reads:
            st = self.state.get(k)
            if st and st[0] is not None:
                toks.append(st[0])
        for k in writes:
            st = self.state.get(k)
            if st:
                if st[0] is not None:
                    toks.append(st[0])
                toks.extend(st[1])
        return toks

    def _commit(self, tok, reads, writes):
        for k in reads:
            st = self.state.setdefault(k, [None, []])
            st[1].append(tok)
            if len(st[1]) > 64:
                st[1] = st[1][-64:] if False else self._compress(st[1])
        for k in writes:
            self.state[k] = [tok, []]

    @staticmethod
    def _compress(toks):
        best = {}
        for t in toks:
            sid = t[:3]
            if sid not in best or best[sid][3] < t[3]:
                best[sid] = t
        return list(best.values())

    def _waits(self, eng, toks, same_engine_ok=False):
        need = {}
        for t in toks:
            kind, e, idx, val = t
            if kind == "c" and e == eng and same_engine_ok:
                continue
            sid = (kind, e, idx)
            if need.get(sid, 0) < val:
                need[sid] = val
        out = []
        for sid, val in need.items():
            if self.seen[eng].get(sid, 0) >= val:
                continue
            self.seen[eng][sid] = val
            out.append((sid, val))
        return out

    def op(self, eng, fn, reads=(), writes=()):
        toks = self._deps(reads, writes) + self.pending[eng]
        self.pending[eng] = []
        waits = self._waits(eng, toks, same_engine_ok=(eng == "pe"))
        self.cnt[eng] += 1
        tok = ("c", eng, 0, self.cnt[eng])
        self.ops[eng].append((waits, fn, ("c", eng, 0)))
        self._commit(tok, reads, writes)
        self.nops += 1

    def dma(self, eng, fn, reads=(), writes=()):
        toks = self._deps(reads, writes) + self.pending[eng]
        self.pending[eng] = []
        i = self.dma_n[eng]
        self.dma_n[eng] += 1
        slot = i % NDMA_SEMS
        gen = i // NDMA_SEMS
        if gen > 0:
            toks = toks + [("d", eng, slot, 16 * gen)]
        waits = self._waits(eng, toks)
        tok = ("d", eng, slot, 16 * (gen + 1))
        self.dma_exp[(eng, slot)] = 16 * (gen + 1)
        self.ops[eng].append((waits, fn, ("d", eng, slot)))
        self._commit(tok, reads, writes)
        self.nops += 1

    def barrier(self):
        toks = [("c", e, 0, self.cnt[e]) for e in self.ENGS if self.cnt[e] > 0]
        toks += [("d", e, s, v) for (e, s), v in self.dma_exp.items()]
        for e in self.ENGS:
            self.pending[e] = list(toks)
        self.state = {}

    def emit(self):
        nc = self.nc
        self.barrier()
        fin_waits = self._waits("sp", self.pending["sp"])
        with contextlib.ExitStack() as es:
            sems = {}
            for e in self.ENGS:
                sems[("c", e, 0)] = es.enter_context(nc.semaphore("c_" + e))
            for e in ("sp", "act", "pool"):
                for s in range(NDMA_SEMS):
                    sems[("d", e, s)] = es.enter_context(nc.semaphore("d_%s_%d" % (e, s)))
            block = es.enter_context(nc.Block())

            def run(engname, engobj):
                for waits, fn, inc in self.ops[engname]:
                    for sid, val in waits:
                        engobj.wait_ge(sems[sid], val)
                    ins = fn(engobj)
                    ins.then_inc(sems[inc], 16 if inc[0] == "d" else 1)
                if engname == "sp":
                    for sid, val in fin_waits:
                        engobj.wait_ge(sems[sid], val)

            @block.sync
            def _(e):
                run("sp", e)

            @block.tensor
            def _(e):
                run("pe", e)

            @block.scalar
            def _(e):
                run("act", e)

            @block.vector
            def _(e):
                run("dve", e)

            @block.gpsimd
            def _(e):
                run("pool", e)


class Cfg:
    def __init__(self, S=4096, L=4, phases=None, ext=()):
        self.S = S
        self.L = L
        self.phases = phases
        self.ext = set(ext)
        self.cap = 2 * S // NE


class Ctx:
    pass


def build(cfg):
    nc = bass.Bass("TRN2", target_bir_lowering=False)
    K = Ctx()
    K.nc, K.cfg = nc, cfg
    K.P = Prog(nc)
    S, L = cfg.S, cfg.L
    K.S, K.L = S, L

    def din(name, shape, dt=F32):
        return nc.dram_tensor(name, list(shape), dt, kind="ExternalInput").ap()

    def dscr(name, shape, dt=F32):
        kind = "Internal"
        if name in cfg.ext:
            kind = "ExternalOutput"
        if ("in:" + name) in cfg.ext:
            kind = "ExternalInput"
        return nc.dram_tensor(name, list(shape), dt, kind=kind).ap()

    shapes = {
        "x": [S, D], "c": [D], "w_mod": [D, 6 * D], "mod_table": [L, 6 * D], "norm1_g": [L, D], "norm2_g": [L, D],
        "w_in": [L, D, IN_WIDTH], "conv_w": [L, 3, 512], "gqa_q_norm": [L, 64], "gqa_k_norm": [L, 64],
        "rwkv_mu": [L, 2, 1920], "rwkv_w0": [L, 2, 512], "rwkv_w2": [L, 128, 512], "rwkv_a0": [L, 2, 512],
        "rwkv_a2": [L, 128, 512], "rwkv_g2": [L, 128, 512], "rwkv_k_k": [L, 512], "rwkv_k_a": [L, 512],
        "rwkv_r_k": [L, 512], "rwkv_ln_w": [L, 512], "rwkv_ln_b": [L, 512], "mla_qc_norm": [L, 384],
        "mla_kvc_norm": [L, 256], "mla_w_uq": [L, 384, 768], "mla_w_ukv": [L, 256, 1024], "mla_q_norm": [L, 96],
        "mla_k_norm": [L, 96], "w_branch": [L, 4 * 512, D], "w_out": [L, D, D], "w_router": [L, D, NE],
        "moe_w1": [L, NE, D, EH], "moe_w3": [L, NE, D, EH], "moe_w2": [L, NE, EH, D],
        "rope_g": [S, 64], "rope_m": [S, 32],
    }

    class LazyIn(dict):
        def __missing__(self, name):
            v = din(name, shapes[name])
            self[name] = v
            return v
    I = LazyIn()
    K.I = I
    K.out = nc.dram_tensor("out", [S, D], F32, kind="ExternalOutput").ap()

    Sc = {}
    Sc["modv"] = dscr("modv", [L * 6, D])
    Sc["fm"] = dscr("fm", [1536 + 1920, S])
    Sc["gt"] = dscr("gt", [4 * D, S], BF16)
    Sc["tm"] = dscr("tm", [S, 768 + 672])
    Sc["br"] = dscr("br", [4 * 512, S], BF16)
    Sc["gqT"] = dscr("gqT", [640, S], BF16)
    Sc["gv"] = dscr("gv", [S, 2, 65], BF16)
    Sc["mT"] = dscr("mT", [D, S], BF16)
    Sc["h2"] = dscr("h2", [S, D], BF16)
    Sc["aff"] = dscr("aff", [S, NE])
    Sc["affT"] = dscr("affT", [NE, S])
    Sc["rank"] = dscr("rank", [S, NE])
    Sc["rankT"] = dscr("rankT", [NE, S])
    Sc["ybuf"] = dscr("ybuf", [NE, S // 8, D], BF16)
    Sc["rg"] = dscr("rg", [512, S])
    Sc["rbon"] = dscr("rbon", [512, S])
    Sc["rv"] = dscr("rv", [S, 512])
    Sc["rgc"] = [dscr("rgc%d" % d_, [512, S // CH]) for d_ in range(2)]
    Sc["rf"] = [[dscr("rf%d_%d" % (d_, i_), [512, S]) for i_ in range(4)] for d_ in range(2)]
    Sc["rt"] = [[dscr("rt%d_%d" % (d_, i_), [S, 512]) for i_ in range(2)] for d_ in range(2)]
    Sc["ry"] = [dscr("ry%d" % d_, [S, 512]) for d_ in range(2)]
    Sc["mqT"] = dscr("mqT", [8 * 96, S], BF16)
    Sc["mkT"] = dscr("mkT", [8 * 96, S], BF16)
    Sc["mv"] = dscr("mv", [S, 8, 65], BF16)
    K.Sc = Sc
    K.dscr = dscr

    K.xres = K.out
    with contextlib.ExitStack() as glob:
        K.glob = glob
        setup_consts(K)
        ph = cfg.phases
        if ph is None or "copyx" in ph:
            xin = K.I["x"]
            for t in range(0, S, 512):
                n = min(512, S - t)
                K.P.dma("sp", lambda e, t=t, n=n: e.dma_start(out=K.out[t:t + n, :], in_=xin[t:t + n, :]), writes=["xres"])
            K.P.barrier()
        if ph is None or "p0" in ph:
            phase0(K)
        for l in range(L):
            if ph is None or "p1" in ph:
                phase1(K, l)
            if ph is None or "conv" in ph:
                phase_conv(K, l)
            if ph is None or "gqa" in ph:
                phase_gqa_prep(K, l)
            if ph is None or "gqa" in ph or "gqa_attn" in ph:
                phase_gqa_attn(K, l)
            if ph is None or "mla" in ph or "mla_prep" in ph:
                phase_mla_prep(K, l)
            if ph is None or "mla" in ph or "mla_attn" in ph:
                phase_mla_attn(K, l)
            if ph is None or "rwkv" in ph or "rwkv_prep" in ph:
                phase_rwkv_prep(K, l)
            if ph is None or "rwkv" in ph or "rwkv_scan" in ph:
                phase_rwkv_scan(K, l)
            if ph is None or "rwkv" in ph or "rwkv_post" in ph:
                phase_rwkv_post(K, l)
            if ph is None or "p3a" in ph:
                phase3a(K, l)
            if ph is None or "p3b" in ph:
                phase3b(K, l)
            if ph is None or "moe" in ph:
                phase_moe(K, l)
        K.P.emit()
    return nc


class Phase:
    def __init__(self, K):
        self.K = K
        self.es = contextlib.ExitStack()

    def __enter__(self):
        self.es.__enter__()
        return self

    def __exit__(self, *a):
        self.K.P.barrier()
        return self.es.__exit__(*a)

    _uid = [0]

    def sb(self, name, shape, dt=F32):
        Phase._uid[0] += 1
        return self.es.enter_context(self.K.nc.sbuf_tensor("%s_u%d" % (name, Phase._uid[0]), list(shape), dt))

    def ps(self, name, shape, dt=F32):
        Phase._uid[0] += 1
        return self.es.enter_context(self.K.nc.psum_tensor("%s_u%d" % (name, Phase._uid[0]), list(shape), dt))


def setup_consts(K):
    nc, P = K.nc, K.P
    g = K.glob

    def sb(name, shape, dt=F32):
        return g.enter_context(nc.sbuf_tensor(name, list(shape), dt))
    io = sb("c_io", [128, 512])
    iop = sb("c_iop", [128, 1])
    K.ident = sb("c_ident", [128, 128])
    K.identb = sb("c_identb", [128, 128], BF16)
    P.op("pool", lambda e: e.iota(io[:], pattern=[[1, 512]], base=0, channel_multiplier=0,
                                  allow_small_or_imprecise_dtypes=True), writes=["c_io"])
    P.op("pool", lambda e: e.iota(iop[:], pattern=[[0, 1]], base=0, channel_multiplier=1,
                                  allow_small_or_imprecise_dtypes=True), writes=["c_iop"])
    P.op("dve", lambda e: e.tensor_scalar(out=K.ident[:], in0=io[:, 0:128], scalar1=iop[:, 0:1], scalar2=None,
                                          op0=ALU.is_equal), reads=["c_io", "c_iop"], writes=["c_ident"])
    P.op("dve", lambda e: e.tensor_copy(out=K.identb[:], in_=K.ident[:]), reads=["c_ident"], writes=["c_identb"])
    K.io, K.iop = io, iop


def phase0(K):
    nc, P, I, L = K.nc, K.P, K.I, K.L
    with Phase(K) as ph:
        cT = ph.sb("cT", [128, NKC])
        sc = ph.sb("sc", [128, NKC])
        modrow = ph.sb("modrow", [1, 6 * D])
        P.dma("sp", lambda e: e.dma_start(out=cT[:], in_=I["c"].rearrange("(k p) -> p k", p=128),
                                          allow_slow_non_contiguous=True), writes=["cT"])
        P.op("act", lambda e: e.activation(out=sc[:], in_=cT[:], func=AF.Silu), reads=["cT"], writes=["sc"])
        wm = [ph.sb("wm%d" % i, [128, NKC, 512]) for i in range(2)]
        pm = [ph.ps("pm%d" % i, [1, 512]) for i in range(2)]
        wv = I["w_mod"].rearrange("(k p) n -> p k n", p=128)
        for ci in range(6 * D // 512):
            b = ci % 2
            P.dma("sp", lambda e, b=b, ci=ci: e.dma_start(out=wm[b][:], in_=wv[:, :, ci * 512:(ci + 1) * 512]),
                  writes=["wm%d" % b])
            for kc in range(NKC):
                P.op("pe", lambda e, b=b, kc=kc: e.matmul(pm[b][:], lhsT=sc[:, kc:kc + 1], rhs=wm[b][:, kc, :],
                                                          start=(kc == 0), stop=(kc == NKC - 1)),
                     reads=["sc", "wm%d" % b], writes=["pm%d" % b])
            P.op("act", lambda e, b=b, ci=ci: e.copy(out=modrow[:, ci * 512:(ci + 1) * 512], in_=pm[b][:]),
                 reads=["pm%d" % b], writes=["modrow"])
        tb = ph.sb("tb", [1, 6 * D])
        g12 = ph.sb("g12", [1, 2 * D])
        for l in range(L):
            P.dma("sp", lambda e, l=l: e.dma_start(out=tb[:], in_=I["mod_table"][l:l + 1, :]), writes=["tb"])
            P.dma("sp", lambda e, l=l: e.dma_start(out=g12[:, 0:D], in_=I["norm1_g"][l:l + 1, :]), writes=["g1"])
            P.dma("sp", lambda e, l=l: e.dma_start(out=g12[:, D:2 * D], in_=I["norm2_g"][l:l + 1, :]), writes=["g2"])
            P.op("dve", lambda e: e.tensor_tensor(out=tb[:], in0=tb[:], in1=modrow[:], op=ALU.add),
                 reads=["tb", "modrow"], writes=["tb"])
            for sub in range(2):
                o = sub * 3 * D
                P.op("dve", lambda e, o=o, sub=sub: e.scalar_tensor_tensor(
                    out=tb[:, o + D:o + 2 * D], in0=tb[:, o + D:o + 2 * D], scalar=1.0, in1=g12[:, sub * D:(sub + 1) * D],
                    op0=ALU.add, op1=ALU.mult), reads=["tb", "g1", "g2"], writes=["tb"])
            P.dma("sp", lambda e, l=l: e.dma_start(out=K.Sc["modv"][l * 6:(l + 1) * 6, :].rearrange("a d -> (a d)").unsqueeze(0),
                                                   in_=tb[:]), reads=["tb"], writes=["modv"])


def rms_rstd(P, ph, eng_sq, x_ap, junk_ap, ss, rstd, n, eps, keys_r, tag):
    P.op("act", lambda e: e.activation(out=junk_ap, in_=x_ap, func=AF.Square, accum_out=ss[:, 0:1]),
         reads=keys_r, writes=[tag + "_junk", tag + "_ss"])
    P.op("act", lambda e: e.activation(out=rstd[:, 0:1], in_=ss[:, 0:1], func=AF.Sqrt, scale=1.0 / n, bias=K_EPS[eps][:, 0:1]),
         reads=[tag + "_ss"], writes=[tag + "_rstd"])
    P.op("dve", lambda e: e.reciprocal(out=rstd[:, 0:1], in_=rstd[:, 0:1]), reads=[tag + "_rstd"], writes=[tag + "_rstd"])


K_EPS = {}

IN_CHUNKS = []


def _mk_chunks():
    out = []
    for c0 in range(C_CONV, C_GQA, 512):
        out.append((c0, 512, "fm", c0 - C_CONV))
    out.append((C_GQA, 512, "tm", 0))
    out.append((C_GQA + 512, 256, "tm", 512))
    c0 = C_RWKV
    while c0 < C_MLA:
        n = min(512, C_MLA - c0)
        out.append((c0, n, "fm", 1536 + c0 - C_RWKV))
        c0 += n
    out.append((C_MLA, 512, "tm", 768))
    out.append((C_MLA + 512, 160, "tm", 768 + 512))
    for c0 in range(C_GATE, IN_WIDTH, 512):
        out.append((c0, 512, "gt", c0 - C_GATE))
    return out


IN_CHUNKS = _mk_chunks()


def load_bcast_row(K, ph, name, row_ap, n):
    t = ph.sb(name, [128, n])
    K.P.dma("sp", lambda e: e.dma_start(out=t[:], in_=row_ap.to_broadcast([128, n])), reads=["modv"], writes=[name])
    return t


def norm_to_hT(K, ph, l, sub, x_dram, tok0, ntok, hT, A_b, B_b, bufs, h_keep=None):
    P = K.P
    xt, hb, junk, ss, rstd, pt = bufs
    for tt in range(ntok // 128):
        t0 = tok0 + tt * 128
        b = tt % 2
        P.dma("sp", lambda e, b=b, t0=t0: e.dma_start(out=xt[b][:], in_=x_dram[t0:t0 + 128, :]),
              reads=["xdram"], writes=["xt%d" % b])
        P.op("act", lambda e, b=b: e.activation(out=junk[:], in_=xt[b][:], func=AF.Square, accum_out=ss[:, 0:1]),
             reads=["xt%d" % b], writes=["junk", "ss"])
        P.op("act", lambda e: e.activation(out=rstd[:, 0:1], in_=ss[:, 0:1], func=AF.Sqrt, scale=1.0 / D,
                                           bias=K.eps_norm[:, 0:1]), reads=["ss"], writes=["rstd"])
        P.op("dve", lambda e: e.reciprocal(out=rstd[:, 0:1], in_=rstd[:, 0:1]), reads=["rstd"], writes=["rstd"])
        P.op("dve", lambda e, b=b: e.scalar_tensor_tensor(out=xt[b][:], in0=xt[b][:], scalar=rstd[:, 0:1], in1=A_b[:],
                                                          op0=ALU.mult, op1=ALU.mult),
             reads=["xt%d" % b, "rstd", "A_b"], writes=["xt%d" % b])
        P.op("pool", lambda e, b=b: e.tensor_tensor(out=hb[b][:], in0=xt[b][:], in1=B_b[:], op=ALU.add),
             reads=["xt%d" % b, "B_b"], writes=["hb%d" % b])
        if h_keep is not None:
            h_keep(tt, t0, xt[b], hb[b], "xt%d" % b, "hb%d" % b)
        for g4 in range(NKC // 4):
            pb = g4 % 2
            for j in range(4):
                kc = g4 * 4 + j
                P.op("pe", lambda e, b=b, pb=pb, j=j, kc=kc: e.transpose(pt[pb][:, j * 128:(j + 1) * 128],
                                                                         hb[b][:, kc * 128:(kc + 1) * 128], K.identb[:]),
                     reads=["hb%d" % b, "c_identb"], writes=["pt%d" % pb])
            P.op("act", lambda e, pb=pb, g4=g4, tt=tt: e.copy(
                out=hT[:, g4 * 4:(g4 + 1) * 4, tt * 128:(tt + 1) * 128],
                in_=pt[pb][:].rearrange("p (j t) -> p j t", j=4)),
                reads=["pt%d" % pb], writes=["hT"])


def phase1(K, l):
    nc, P, I, S = K.nc, K.P, K.I, K.S
    TS = min(1024, S)
    NT = min(512, TS)
    xd = K.out if (l > 0 or K.cfg.phases is None or "copyx" in K.cfg.phases) else I["x"]
    with Phase(K) as ph:
        K.eps_norm = ph.sb("eps_norm", [128, 1])
        P.op("dve", lambda e: e.memset(K.eps_norm[:], NORM_EPS), writes=["eps_norm"])
        A_b = load_bcast_row(K, ph, "A_b", K.Sc["modv"][l * 6 + 1:l * 6 + 2, :], D)
        B_b = load_bcast_row(K, ph, "B_b", K.Sc["modv"][l * 6 + 0:l * 6 + 1, :], D)
        hT = ph.sb("hT", [128, NKC, TS], BF16)
        xt = [ph.sb("xt%d" % i, [128, D]) for i in range(2)]
        hb = [ph.sb("hb%d" % i, [128, D], BF16) for i in range(2)]
        junk = ph.sb("junk", [128, D], BF16)
        ss = ph.sb("ss", [128, 1])
        rstd = ph.sb("rstd", [128, 1])
        pt = [ph.ps("pt%d" % i, [128, 512], BF16) for i in range(2)]
        wf = [ph.sb("wf%d" % i, [128, NKC, 512]) for i in range(2)]
        wb = [ph.sb("wb%d" % i, [128, NKC, 512], BF16) for i in range(2)]
        stg = [ph.sb("stg%d" % i, [128, 512]) for i in range(4)]
        stgb = [ph.sb("stgb%d" % i, [128, 512], BF16) for i in range(2)]
        pm = [ph.ps("pm%d" % i, [128, 512]) for i in range(4)]
        wv = I["w_in"][l].rearrange("(k p) n -> p k n", p=128)
        nmm = 0
        work = [(st, ci) for st in range(S // TS) for ci in range(len(IN_CHUNKS))]

        def issue_w(wi):
            c0, ncol, _, _ = IN_CHUNKS[work[wi][1]]
            b = wi % 2
            P.dma("sp", lambda e, b=b, c0=c0, ncol=ncol: e.dma_start(out=wf[b][:, :, 0:ncol], in_=wv[:, :, c0:c0 + ncol]),
                  writes=["wf%d" % b])
            P.op("pool", lambda e, b=b, ncol=ncol: e.tensor_copy(out=wb[b][:, :, 0:ncol], in_=wf[b][:, :, 0:ncol]),
                 reads=["wf%d" % b], writes=["wb%d" % b])

        issue_w(0)
        for wi, (st, ci) in enumerate(work):
            tok0 = st * TS
            if ci == 0:
                norm_to_hT(K, ph, l, 0, xd, tok0, TS, hT, A_b, B_b, (xt, hb, junk, ss, rstd, pt))
            if wi + 1 < len(work):
                issue_w(wi + 1)
            if True:
                (c0, ncol, kind, doff) = IN_CHUNKS[ci]
                b = wi % 2
                if kind in ("fm", "gt"):
                    for j in range(ncol // 128):
                        for th in range(TS // NT):
                            pi = nmm % 4
                            nmm += 1
                            for kc in range(NKC):
                                P.op("pe", lambda e, pi=pi, b=b, kc=kc, j=j, th=th: e.matmul(
                                    pm[pi][:, 0:NT], lhsT=wb[b][:, kc, j * 128:(j + 1) * 128], rhs=hT[:, kc, th * NT:(th + 1) * NT],
                                    start=(kc == 0), stop=(kc == NKC - 1)),
                                    reads=["wb%d" % b, "hT"], writes=["pm%d" % pi])
                            r0 = doff + j * 128
                            t0 = tok0 + th * NT
                            if kind == "fm":
                                P.op("dve", lambda e, pi=pi: e.tensor_copy(out=stg[pi][:, 0:NT], in_=pm[pi][:, 0:NT]),
                                     reads=["pm%d" % pi], writes=["stg%d" % pi])
                                P.dma("sp", lambda e, pi=pi, r0=r0, t0=t0: e.dma_start(
                                    out=K.Sc["fm"][r0:r0 + 128, t0:t0 + NT], in_=stg[pi][:, 0:NT]),
                                    reads=["stg%d" % pi], writes=["fm"])
                            else:
                                sb_i = pi % 2
                                P.op("act", lambda e, pi=pi, sb_i=sb_i: e.activation(out=stgb[sb_i][:, 0:NT], in_=pm[pi][:, 0:NT], func=AF.Sigmoid),
                                     reads=["pm%d" % pi], writes=["stgb%d" % sb_i])
                                P.dma("sp", lambda e, sb_i=sb_i, r0=r0, t0=t0: e.dma_start(
                                    out=K.Sc["gt"][r0:r0 + 128, t0:t0 + NT], in_=stgb[sb_i][:, 0:NT]),
                                    reads=["stgb%d" % sb_i], writes=["gt"])
                else:
                    for tt in range(TS // 128):
                        pi = nmm % 4
                        nmm += 1
                        for kc in range(NKC):
                            P.op("pe", lambda e, pi=pi, b=b, kc=kc, tt=tt, ncol=ncol: e.matmul(
                                pm[pi][:, 0:ncol], lhsT=hT[:, kc, tt * 128:(tt + 1) * 128], rhs=wb[b][:, kc, 0:ncol],
                                start=(kc == 0), stop=(kc == NKC - 1)),
                                reads=["wb%d" % b, "hT"], writes=["pm%d" % pi])
                        t0 = tok0 + tt * 128
                        P.op("dve", lambda e, pi=pi, ncol=ncol: e.tensor_copy(out=stg[pi][:, 0:ncol], in_=pm[pi][:, 0:ncol]),
                             reads=["pm%d" % pi], writes=["stg%d" % pi])
                        P.dma("sp", lambda e, pi=pi, t0=t0, doff=doff, ncol=ncol: e.dma_start(
                            out=K.Sc["tm"][t0:t0 + 128, doff:doff + ncol], in_=stg[pi][:, 0:ncol]),
                            reads=["stg%d" % pi], writes=["tm"])


def phase_conv(K, l):
    P, I, S = K.P, K.I, K.S
    fm, br = K.Sc["fm"], K.Sc["br"]
    with Phase(K) as ph:
        cw = ph.sb("cw", [128, 4, 3])
        cwv = I["conv_w"][l]
        for ct in range(4):
            P.dma("sp", lambda e, ct=ct: e.dma_start(out=cw[:, ct, :], in_=cwv[:, ct * 128:(ct + 1) * 128].rearrange("k p -> p k"),
                                                     allow_slow_non_contiguous=True), writes=["cw"])
        bg = ph.sb("bg", [128, S])
        cg = ph.sb("cg", [128, S])
        zp = ph.sb("zp", [128, S + 2])
        y = ph.sb("y", [128, S])
        ob = ph.sb("ob", [128, S], BF16)
        for ct in range(4):
            k = "cv_"
            P.dma("sp", lambda e, ct=ct: e.dma_start(out=bg[:], in_=fm[ct * 128:(ct + 1) * 128, :]), reads=["fm"], writes=[k + "bg"])
            P.dma("sp", lambda e, ct=ct: e.dma_start(out=cg[:], in_=fm[512 + ct * 128:512 + (ct + 1) * 128, :]), reads=["fm"], writes=[k + "cg"])
            P.dma("sp", lambda e, ct=ct: e.dma_start(out=zp[:, 1:S + 1], in_=fm[1024 + ct * 128:1024 + (ct + 1) * 128, :]), reads=["fm"], writes=[k + "zp"])
            P.op("pool", lambda e: e.memset(zp[:, 0:1], 0.0), writes=[k + "z0"])
            P.op("pool", lambda e: e.memset(zp[:, S + 1:S + 2], 0.0), writes=[k + "z1"])
            P.op("dve", lambda e: e.tensor_tensor(out=zp[:, 1:S + 1], in0=zp[:, 1:S + 1], in1=cg[:], op=ALU.mult),
                 reads=[k + "zp", k + "cg"], writes=[k + "zp"])
            P.op("dve", lambda e, ct=ct: e.tensor_scalar(out=y[:], in0=zp[:, 0:S], scalar1=cw[:, ct, 0:1], scalar2=None, op0=ALU.mult),
                 reads=[k + "zp", k + "z0", "cw"], writes=[k + "y"])
            P.op("dve", lambda e, ct=ct: e.scalar_tensor_tensor(out=y[:], in0=zp[:, 1:S + 1], scalar=cw[:, ct, 1:2], in1=y[:],
                                                                op0=ALU.mult, op1=ALU.add), reads=[k + "zp", k + "y"], writes=[k + "y"])
            P.op("dve", lambda e, ct=ct: e.scalar_tensor_tensor(out=y[:], in0=zp[:, 2:S + 2], scalar=cw[:, ct, 2:3], in1=y[:],
                                                                op0=ALU.mult, op1=ALU.add), reads=[k + "zp", k + "z1", k + "y"], writes=[k + "y"])
            P.op("pool", lambda e: e.tensor_tensor(out=ob[:], in0=y[:], in1=bg[:], op=ALU.mult), reads=[k + "y", k + "bg"], writes=[k + "ob"])
            P.dma("sp", lambda e, ct=ct: e.dma_start(out=br[ct * 128:(ct + 1) * 128, :], in_=ob[:]), reads=[k + "ob"], writes=["br"])


def bcast_rows(K, ph, name, row_ap, n, reps):
    t = ph.sb(name, [128, reps, n])
    for r in range(reps):
        K.P.dma("sp", lambda e, r=r: e.dma_start(out=t[:, r, :], in_=row_ap.to_broadcast([128, n])), writes=[name + str(r)])
    return t, [name + str(r) for r in range(reps)]


def head_rms(P, x3, nh, hd, sq3, ssq, eps_t, keys_in, tag):
    P.op("dve", lambda e: e.tensor_tensor(out=sq3, in0=x3, in1=x3, op=ALU.mult), reads=keys_in, writes=[tag + "sq"])
    P.op("dve", lambda e: e.tensor_reduce(out=ssq, in_=sq3, axis=AX.X, op=ALU.add), reads=[tag + "sq"], writes=[tag + "ssq"])
    P.op("act", lambda e: e.activation(out=ssq, in_=ssq, func=AF.Sqrt, scale=1.0 / hd, bias=eps_t[:, 0:1]),
         reads=[tag + "ssq"], writes=[tag + "ssq"])
    P.op("dve", lambda e: e.reciprocal(out=ssq, in_=ssq), reads=[tag + "ssq"], writes=[tag + "ssq"])
    P.op("dve", lambda e: e.tensor_tensor(out=x3, in0=x3, in1=ssq.unsqueeze(2).to_broadcast([128, nh, hd]), op=ALU.mult),
         reads=keys_in + [tag + "ssq"], writes=keys_in)


def rope3(P, x1, x2, o1, o2, cs, sn, t1, t2, shape, keys_in, keys_out, tag):
    P.op("dve", lambda e: e.tensor_tensor(out=t1, in0=x1, in1=cs, op=ALU.mult), reads=keys_in, writes=[tag + "t1"])
    P.op("pool", lambda e: e.tensor_tensor(out=t2, in0=x2, in1=sn, op=ALU.mult), reads=keys_in, writes=[tag + "t2"])
    P.op("dve", lambda e: e.tensor_tensor(out=o1, in0=t1, in1=t2, op=ALU.subtract), reads=[tag + "t1", tag + "t2"], writes=[keys_out[0]])
    P.op("dve", lambda e: e.tensor_tensor(out=t1, in0=x1, in1=sn, op=ALU.mult), reads=keys_in + [keys_out[0]], writes=[tag + "t1"])
    P.op("pool", lambda e: e.tensor_tensor(out=t2, in0=x2, in1=cs, op=ALU.mult), reads=keys_in + [keys_out[0]], writes=[tag + "t2"])
    P.op("dve", lambda e: e.tensor_tensor(out=o2, in0=t1, in1=t2, op=ALU.add), reads=[tag + "t1", tag + "t2"], writes=[keys_out[1]])


def phase_gqa_prep(K, l):
    P, I, S = K.P, K.I, K.S
    tm = K.Sc["tm"]
    rope_g = I["rope_g"]
    with Phase(K) as ph:
        eps_t = ph.sb("eps_t", [128, 1])
        P.op("dve", lambda e: e.memset(eps_t[:], NORM_EPS), writes=["eps_t"])
        gb = ph.sb("gb", [128, 10, 64])
        gkeys = []
        for r in range(10):
            src = I["gqa_q_norm"][l:l + 1, :] if r < 8 else I["gqa_k_norm"][l:l + 1, :]
            P.dma("sp", lambda e, r=r, src=src: e.dma_start(out=gb[:, r, :], in_=src.to_broadcast([128, 64])), writes=["gb%d" % r])
            gkeys.append("gb%d" % r)
        for tt in range(S // 128):
            b = tt % 2
            t0 = tt * 128
            if tt < 2:
                K_ = {}
                K_["x"] = ph.sb("gx%d" % b, [128, 768])
                K_["rp"] = ph.sb("grp%d" % b, [128, 64])
                K_["sq"] = ph.sb("gsq%d" % b, [128, 640])
                K_["ssq"] = ph.sb("gssq%d" % b, [128, 10])
                K_["t1"] = ph.sb("gt1%d" % b, [128, 320])
                K_["t2"] = ph.sb("gt2%d" % b, [128, 320])
                K_["qk"] = ph.sb("gqk%d" % b, [128, 640], BF16)
                K_["v"] = ph.sb("gv%d" % b, [128, 2, 65], BF16)
                K_["pt"] = ph.ps("gpt%d" % b, [128, 1024], BF16)
                K_["qkT"] = ph.sb("gqkT%d" % b, [128, 640], BF16)
                if tt == 0:
                    bufs = [K_, None]
                else:
                    bufs[1] = K_
            B = bufs[b]
            k = "g%d_" % b
            x, rp = B["x"], B["rp"]
            P.dma("sp", lambda e, x=x, t0=t0: e.dma_start(out=x[:], in_=tm[t0:t0 + 128, 0:768]), reads=["tm"], writes=[k + "x"])
            P.dma("sp", lambda e, rp=rp, t0=t0: e.dma_start(out=rp[:], in_=rope_g[t0:t0 + 128, :]), writes=[k + "rp"])
            x3 = x[:, 0:640].rearrange("p (h d) -> p h d", h=10)
            head_rms(P, x3, 10, 64, B["sq"][:].rearrange("p (h d) -> p h d", h=10), B["ssq"][:], eps_t, [k + "x"], k)
            P.op("pool", lambda e, x3=x3: e.tensor_tensor(out=x3, in0=x3, in1=gb[:], op=ALU.mult), reads=[k + "x"] + gkeys, writes=[k + "x"])
            x4 = x[:, 0:640].rearrange("p (h two d) -> p h two d", h=10, two=2)
            qk4 = B["qk"][:].rearrange("p (h two d) -> p h two d", h=10, two=2)
            cs = rp[:, 0:32].unsqueeze(1).to_broadcast([128, 10, 32])
            sn = rp[:, 32:64].unsqueeze(1).to_broadcast([128, 10, 32])
            t1 = B["t1"][:].rearrange("p (h d) -> p h d", h=10)
            t2 = B["t2"][:].rearrange("p (h d) -> p h d", h=10)
            rope3(P, x4[:, :, 0, :], x4[:, :, 1, :], qk4[:, :, 0, :], qk4[:, :, 1, :], cs, sn, t1, t2, None,
                  [k + "x", k + "rp"], [k + "qk0", k + "qk1"], k)
            v = B["v"]
            P.op("pool", lambda e, v=v: e.memset(v[:, :, 64:65], 1.0), writes=[k + "v1"])
            P.op("act", lambda e, v=v, x=x: e.copy(out=v[:, :, 0:64], in_=x[:, 640:768].rearrange("p (h d) -> p h d", h=2)),
                 reads=[k + "x"], writes=[k + "v"])
            P.dma("sp", lambda e, v=v, t0=t0: e.dma_start(out=K.Sc["gv"][t0:t0 + 128, :, :], in_=v[:]), reads=[k + "v", k + "v1"], writes=["gv"])
            pt, qkT = B["pt"], B["qkT"]
            for j in range(5):
                P.op("pe", lambda e, pt=pt, j=j, qk=B["qk"]: e.transpose(pt[:, j * 128:(j + 1) * 128], qk[:, j * 128:(j + 1) * 128], K.identb[:]),
                     reads=[k + "qk0", k + "qk1", "c_identb"], writes=[k + "pt"])
            P.op("act", lambda e, pt=pt, qkT=qkT: e.copy(out=qkT[:], in_=pt[:, 0:640]), reads=[k + "pt"], writes=[k + "qkT"])
            P.dma("sp", lambda e, qkT=qkT, t0=t0: e.dma_start(
                out=K.Sc["gqT"].rearrange("(j p) s -> p j s", p=128)[:, :, t0:t0 + 128],
                in_=qkT[:].rearrange("p (j t) -> p j t", j=5)), reads=[k + "qkT"], writes=["gqT"])


def attention(K, heads, dq, scale, tag):
    P, S = K.P, K.S
    QN = min(512, S)
    NKT = S // 128
    with Phase(K) as ph:
        ones = ph.sb("a_ones", [128, 64])
        P.op("dve", lambda e: e.memset(ones[:], 1.0), writes=["a_ones"])
        pad = getattr(K.cfg, "pad", 1)
        PK = 128 if pad else dq
        PM = 128 if pad else 65
        kT = [ph.sb("a_kT%d" % i, [PK, S], BF16) for i in range(2)]
        vt = [ph.sb("a_v%d" % i, [128, NKT, PM], BF16) for i in range(2)]
        qT = [ph.sb("a_qT%d" % i, [PK, QN], BF16) for i in range(2)]
        if pad:
            for i in range(2):
                P.op("pool", lambda e, i=i: e.memset(kT[i][:], 0.0), writes=["a_kT%d" % i])
                P.op("pool", lambda e, i=i: e.memset(vt[i][:], 0.0), writes=["a_v%d" % i])
                P.op("pool", lambda e, i=i: e.memset(qT[i][:], 0.0), writes=["a_qT%d" % i])
        pT = [ph.sb("a_pT%d" % i, [128, QN], BF16) for i in range(3)]
        rrow = ph.sb("a_rrow", [128, QN])
        rb = ph.sb("a_rb", [64, QN])
        ob = [ph.sb("a_ob%d" % i, [64, QN], BF16) for i in range(2)]
        ps_s = [ph.ps("a_ps%d" % i, [128, QN]) for i in range(3)]
        ps_o = [ph.ps("a_po%d" % i, [PM, QN]) for i in range(2)]
        ps_b = ph.ps("a_pb", [64, QN])
        nq = 0
        ns = 0
        for hi, (q_ap, k_ap, v_ap, o_ap) in enumerate(heads):
            hb = hi % 2
            P.dma("sp", lambda e, hb=hb, k_ap=k_ap: e.dma_start(out=kT[hb][0:dq, :], in_=k_ap), reads=[tag + "kT_d"], writes=["a_kT%d" % hb])
            P.dma("sp", lambda e, hb=hb, v_ap=v_ap: e.dma_start(out=vt[hb][:, :, 0:65], in_=v_ap.rearrange("(kc p) e -> p kc e", p=128)),
                  reads=[tag + "v_d"], writes=["a_v%d" % hb])
            for qc in range(S // QN):
                qb = nq % 2
                nq += 1
                q0 = qc * QN
                P.dma("sp", lambda e, qb=qb, q_ap=q_ap, q0=q0: e.dma_start(out=qT[qb][0:dq, :], in_=q_ap[:, q0:q0 + QN]),
                      reads=[tag + "qT_d"], writes=["a_qT%d" % qb])
                sbs = []
                for kc in range(NKT + 1):
                    if kc < NKT:
                        sb_ = ns % 3
                        ns += 1
                        sbs.append(sb_)
                        P.op("pe", lambda e, sb_=sb_, hb=hb, kc=kc, qb=qb: e.matmul(
                            ps_s[sb_][:], lhsT=kT[hb][:, kc * 128:(kc + 1) * 128], rhs=qT[qb][:], start=True, stop=True),
                            reads=["a_kT%d" % hb, "a_qT%d" % qb], writes=["a_ps%d" % sb_])
                        P.op("act", lambda e, sb_=sb_: e.activation(out=pT[sb_][:], in_=ps_s[sb_][:], func=AF.Exp, scale=scale),
                             reads=["a_ps%d" % sb_], writes=["a_pT%d" % sb_])
                    if kc >= 1:
                        kp = kc - 1
                        sp_ = sbs[kp]
                        P.op("pe", lambda e, sp_=sp_, hb=hb, kp=kp, qb=qb: e.matmul(
                            ps_o[qb][:], lhsT=vt[hb][:, kp, :], rhs=pT[sp_][:], start=(kp == 0), stop=(kp == NKT - 1)),
                            reads=["a_v%d" % hb, "a_pT%d" % sp_], writes=["a_po%d" % qb])
                P.op("dve", lambda e, qb=qb: e.reciprocal(out=rrow[64:65, :], in_=ps_o[qb][64:65, :]), reads=["a_po%d" % qb], writes=["a_rrow"])
                P.op("pe", lambda e: e.matmul(ps_b[:], lhsT=ones[64:65, 0:64], rhs=rrow[64:65, :], start=True, stop=True),
                     reads=["a_ones", "a_rrow"], writes=["a_pb"])
                P.op("act", lambda e: e.copy(out=rb[:], in_=ps_b[:]), reads=["a_pb"], writes=["a_rb"])
                P.op("dve", lambda e, qb=qb: e.tensor_tensor(out=ob[qb][:], in0=ps_o[qb][0:64, :], in1=rb[:], op=ALU.mult),
                     reads=["a_po%d" % qb, "a_rb"], writes=["a_ob%d" % qb])
                P.dma("sp", lambda e, qb=qb, o_ap=o_ap, q0=q0: e.dma_start(out=o_ap[:, q0:q0 + QN], in_=ob[qb][:]),
                      reads=["a_ob%d" % qb], writes=["br"])


def phase_gqa_attn(K, l):
    gqT, gv, br = K.Sc["gqT"], K.Sc["gv"], K.Sc["br"]
    heads = []
    for h in range(8):
        kv = h // 4
        heads.append((gqT[h * 64:(h + 1) * 64, :], gqT[512 + kv * 64:512 + (kv + 1) * 64, :], gv[:, kv, :],
                      br[512 + h * 64:512 + (h + 1) * 64, :]))
    attention(K, heads, 64, 64 ** -0.5, "g")


def phase_mla_prep(K, l):
    P, I, S = K.P, K.I, K.S
    tm = K.Sc["tm"]
    rope_m = I["rope_m"]
    with Phase(K) as ph:
        eps_t = ph.sb("eps_t", [128, 1])
        P.op("dve", lambda e: e.memset(eps_t[:], NORM_EPS), writes=["eps_t"])
        gc, gckeys = bcast_rows(K, ph, "m_gc", I["mla_qc_norm"][l:l + 1, :], 384, 1)
        gkv, gkvkeys = bcast_rows(K, ph, "m_gkv", I["mla_kvc_norm"][l:l + 1, :], 256, 1)
        gq, gqkeys = bcast_rows(K, ph, "m_gq", I["mla_q_norm"][l:l + 1, :], 96, 8)
        gk, gkkeys = bcast_rows(K, ph, "m_gk", I["mla_k_norm"][l:l + 1, :], 96, 8)
        wqf = ph.sb("m_wqf", [128, 3, 768])
        wkf = ph.sb("m_wkf", [128, 2, 1024])
        wq = ph.sb("m_wq", [128, 3, 768], BF16)
        wk = ph.sb("m_wk", [128, 2, 1024], BF16)
        P.dma("sp", lambda e: e.dma_start(out=wqf[:], in_=I["mla_w_uq"][l].rearrange("(k p) n -> p k n", p=128)), writes=["m_wqf"])
        P.dma("sp", lambda e: e.dma_start(out=wkf[:], in_=I["mla_w_ukv"][l].rearrange("(k p) n -> p k n", p=128)), writes=["m_wkf"])
        P.op("pool", lambda e: e.tensor_copy(out=wq[:], in_=wqf[:]), reads=["m_wqf"], writes=["m_wq"])
        P.op("pool", lambda e: e.tensor_copy(out=wk[:], in_=wkf[:]), reads=["m_wkf"], writes=["m_wk"])
        x = ph.sb("m_x", [128, 672])
        rp = ph.sb("m_rp", [128, 32])
        junk = ph.sb("m_junk", [128, 384])
        ss = ph.sb("m_ss", [128, 2])
        cn = ph.sb("m_cn", [128, 640], BF16)
        pt = ph.ps("m_pt", [128, 1024], BF16)
        cnT = ph.sb("m_cnT", [128, 5, 128], BF16)
        pq = [ph.ps("m_pq%d" % i, [128, 512]) for i in range(2)]
        pkv = [ph.ps("m_pkv%d" % i, [128, 512]) for i in range(2)]
        q = ph.sb("m_q", [128, 8, 96])
        kk = ph.sb("m_k", [128, 8, 96])
        sq = ph.sb("m_sq", [128, 8, 96])
        ssq = ph.sb("m_ssq", [128, 8])
        t1 = ph.sb("m_t1", [128, 8, 16])
        t2 = ph.sb("m_t2", [128, 8, 16])
        qb = ph.sb("m_qb", [128, 8, 96], BF16)
        kb = ph.sb("m_kb", [128, 8, 96], BF16)
        v = ph.sb("m_v", [128, 8, 65], BF16)
        ptq = ph.ps("m_ptq", [96, 8, 128], BF16)
        ptk = ph.ps("m_ptk", [96, 8, 128], BF16)
        qT = ph.sb("m_qT", [96, 8, 128], BF16)
        kT = ph.sb("m_kT", [96, 8, 128], BF16)
        P.op("pool", lambda e: e.memset(v[:, :, 64:65], 1.0), writes=["m_v1"])
        for tt in range(S // 128):
            t0 = tt * 128
            P.dma("sp", lambda e, t0=t0: e.dma_start(out=x[:], in_=tm[t0:t0 + 128, 768:1440]), reads=["tm"], writes=["m_x"])
            P.dma("sp", lambda e, t0=t0: e.dma_start(out=rp[:], in_=rope_m[t0:t0 + 128, :]), writes=["m_rp"])
            for i, (c0, n, g_t, gkeys_) in enumerate(((0, 384, gc, gckeys), (384, 256, gkv, gkvkeys))):
                P.op("act", lambda e, c0=c0, n=n, i=i: e.activation(out=junk[:, 0:n], in_=x[:, c0:c0 + n], func=AF.Square,
                                                                    accum_out=ss[:, i:i + 1]), reads=["m_x"], writes=["m_junk", "m_ss%d" % i])
                P.op("act", lambda e, n=n, i=i: e.activation(out=ss[:, i:i + 1], in_=ss[:, i:i + 1], func=AF.Sqrt, scale=1.0 / n,
                                                             bias=eps_t[:, 0:1]), reads=["m_ss%d" % i, "eps_t"], writes=["m_ss%d" % i])
                P.op("dve", lambda e, i=i: e.reciprocal(out=ss[:, i:i + 1], in_=ss[:, i:i + 1]), reads=["m_ss%d" % i], writes=["m_ss%d" % i])
                P.op("dve", lambda e, c0=c0, n=n, i=i, g_t=g_t: e.scalar_tensor_tensor(
                    out=cn[:, c0:c0 + n], in0=x[:, c0:c0 + n], scalar=ss[:, i:i + 1], in1=g_t[:, 0, :], op0=ALU.mult, op1=ALU.mult),
                    reads=["m_x", "m_ss%d" % i] + gkeys_, writes=["m_cn%d" % i])
            for j in range(5):
                P.op("pe", lambda e, j=j: e.transpose(pt[:, j * 128:(j + 1) * 128], cn[:, j * 128:(j + 1) * 128], K.identb[:]),
                     reads=["m_cn0", "m_cn1", "c_identb"], writes=["m_pt"])
            P.op("act", lambda e: e.copy(out=cnT[:], in_=pt[:, 0:640].rearrange("p (j t) -> p j t", j=5)), reads=["m_pt"], writes=["m_cnT"])
            if getattr(K.cfg, "dbg", 0) == 1:
                continue
            for half in range(2):
                for kc in range(3):
                    P.op("pe", lambda e, half=half, kc=kc: e.matmul(pq[half][:, 0:384], lhsT=cnT[:, kc, :], rhs=wq[:, kc, half * 384:(half + 1) * 384],
                                                                    start=(kc == 0), stop=(kc == 2)), reads=["m_cnT", "m_wq"], writes=["m_pq%d" % half])
                for kc in range(2):
                    P.op("pe", lambda e, half=half, kc=kc: e.matmul(pkv[half][:], lhsT=cnT[:, 3 + kc, :], rhs=wk[:, kc, half * 512:(half + 1) * 512],
                                                                    start=(kc == 0), stop=(kc == 1)), reads=["m_cnT", "m_wk"], writes=["m_pkv%d" % half])
            if getattr(K.cfg, "dbg", 0) == 5:
                continue
            for half in range(2):
                P.op("act", lambda e, half=half: e.copy(out=q[:, half * 4:(half + 1) * 4, :], in_=pq[half][:, 0:384].rearrange("p (h d) -> p h d", h=4)),
                     reads=["m_pq%d" % half], writes=["m_q%d" % half])
                pk4 = pkv[half][:].rearrange("p (h d) -> p h d", h=4)
                if getattr(K.cfg, "dbg", 0) == 6:
                    continue
                P.op("dve", lambda e, half=half, pk4=pk4: e.tensor_copy(out=kk[:, half * 4:(half + 1) * 4, 0:64], in_=pk4[:, :, 0:64]),
                     reads=["m_pkv%d" % half], writes=["m_kn%d" % half])
                if getattr(K.cfg, "dbg", 0) == 7:
                    continue
                P.op("dve", lambda e, half=half, pk4=pk4: e.tensor_copy(out=v[:, half * 4:(half + 1) * 4, 0:64], in_=pk4[:, :, 64:128]),
                     reads=["m_pkv%d" % half], writes=["m_v%d" % half])
            if getattr(K.cfg, "dbg", 0) == 2:
                continue
            P.op("pool", lambda e: e.tensor_copy(out=kk[:, :, 64:96], in_=x[:, 640:672].unsqueeze(1).to_broadcast([128, 8, 32])),
                 reads=["m_x"], writes=["m_kpe"])
            if getattr(K.cfg, "dbg", 0) == 3:
                continue
            P.dma("sp", lambda e, t0=t0: e.dma_start(out=K.Sc["mv"][t0:t0 + 128, :, :], in_=v[:]), reads=["m_v0", "m_v1", "m_v1"], writes=["mv"])
            cs = rp[:, 0:16].unsqueeze(1).to_broadcast([128, 8, 16])
            sn = rp[:, 16:32].unsqueeze(1).to_broadcast([128, 8, 16])
            for (t_, keys_, g_t, gkeys_, ob_, okey, pt_, T_, dkey) in (
                    (q, ["m_q0", "m_q1"], gq, gqkeys, qb, "m_qb", ptq, qT, "mqT"),
                    (kk, ["m_kn0", "m_kn1", "m_kpe"], gk, gkkeys, kb, "m_kb", ptk, kT, "mkT")):
                kx = okey + "x"
                P.op("dve", lambda e, t_=t_: e.tensor_tensor(out=sq[:], in0=t_[:], in1=t_[:], op=ALU.mult), reads=keys_, writes=["m_sq"])
                P.op("dve", lambda e: e.tensor_reduce(out=ssq[:], in_=sq[:], axis=AX.X, op=ALU.add), reads=["m_sq"], writes=["m_ssq"])
                P.op("act", lambda e: e.activation(out=ssq[:], in_=ssq[:], func=AF.Sqrt, scale=1.0 / 96, bias=eps_t[:, 0:1]),
                     reads=["m_ssq", "eps_t"], writes=["m_ssq"])
                P.op("dve", lambda e: e.reciprocal(out=ssq[:], in_=ssq[:]), reads=["m_ssq"], writes=["m_ssq"])
                P.op("dve", lambda e, t_=t_: e.tensor_tensor(out=t_[:], in0=t_[:], in1=ssq[:].unsqueeze(2).to_broadcast([128, 8, 96]), op=ALU.mult),
                     reads=keys_ + ["m_ssq"], writes=[kx])
                P.op("pool", lambda e, t_=t_, g_t=g_t: e.tensor_tensor(out=t_[:], in0=t_[:], in1=g_t[:], op=ALU.mult), reads=[kx] + gkeys_, writes=[kx])
                P.op("act", lambda e, t_=t_, ob_=ob_: e.copy(out=ob_[:, :, 0:64], in_=t_[:, :, 0:64]), reads=[kx], writes=[okey + "n"])
                rope3(P, t_[:, :, 64:80], t_[:, :, 80:96], ob_[:, :, 64:80], ob_[:, :, 80:96], cs, sn, t1[:], t2[:], None,
                      [kx, "m_rp"], [okey + "r0", okey + "r1"], okey)
                if getattr(K.cfg, "dbg", 0) == 4:
                    continue
                for h in range(8):
                    P.op("pe", lambda e, h=h, pt_=pt_, ob_=ob_: e.transpose(pt_[:, h, :], ob_[:, h, :], K.identb[:]),
                         reads=[okey + "n", okey + "r0", okey + "r1", "c_identb"], writes=[okey + "pt"])
                P.op("act", lambda e, pt_=pt_, T_=T_: e.copy(out=T_[:], in_=pt_[:]), reads=[okey + "pt"], writes=[okey + "T"])
                P.dma("sp", lambda e, T_=T_, t0=t0, dkey=dkey: e.dma_start(
                    out=K.Sc[dkey].rearrange("(h d) s -> d h s", d=96)[:, :, t0:t0 + 128], in_=T_[:]), reads=[okey + "T"], writes=[dkey])


def phase_mla_attn(K, l):
    mqT, mkT, mv, br = K.Sc["mqT"], K.Sc["mkT"], K.Sc["mv"], K.Sc["br"]
    heads = []
    for h in range(8):
        heads.append((mqT[h * 96:(h + 1) * 96, :], mkT[h * 96:(h + 1) * 96, :], mv[:, h, :], br[1536 + h * 64:1536 + (h + 1) * 64, :]))
    attention(K, heads, 96, 96 ** -0.5, "m")


def load_cast(K, src_ap, stage_ap, dst_ap, skey, dkey, eng="pool"):
    P = K.P
    P.dma("sp", lambda e: e.dma_start(out=stage_ap, in_=src_ap), writes=[skey])
    P.op(eng, lambda e: e.tensor_copy(out=dst_ap, in_=stage_ap), reads=[skey], writes=[dkey])


def phase3a(K, l):
    P, I, S = K.P, K.I, K.S
    br, gt, mT = K.Sc["br"], K.Sc["gt"], K.Sc["mT"]
    NT = min(512, S)
    with Phase(K) as ph:
        wbf = ph.sb("wbf", [128, 4, D])
        wb = ph.sb("wb3", [128, 16, D], BF16)
        for i in range(4):
            load_cast(K, I["w_branch"][l][i * 512:(i + 1) * 512, :].rearrange("(kc p) n -> p kc n", p=128), wbf[:],
                      wb[:, i * 4:(i + 1) * 4, :], "wbf", "wb3_%d" % i)
        wkeys = ["wb3_%d" % i for i in range(4)]
        brT = [ph.sb("brT%d" % i, [128, 16, NT], BF16) for i in range(2)]
        gts = [ph.sb("gts%d" % i, [128, 4, NT], BF16) for i in range(2)]
        tmp = [ph.sb("tmp3_%d" % i, [128, NT]) for i in range(4)]
        ms = [ph.sb("ms%d" % i, [128, NT], BF16) for i in range(2)]
        ps = [ph.ps("p3_%d" % i, [128, 512]) for i in range(8)]
        gtv = gt.rearrange("(i n) s -> n i s", i=4)
        brv = br.rearrange("(k p) s -> p k s", p=128)
        n = 0
        for st in range(S // NT):
            tok0 = st * NT
            bb = st % 2
            P.dma("sp", lambda e, bb=bb, tok0=tok0: e.dma_start(out=brT[bb][:], in_=brv[:, :, tok0:tok0 + NT]), reads=["br"], writes=["brT%d" % bb])
            for nt in range(16):
                gb = n % 2
                P.dma("sp", lambda e, gb=gb, nt=nt, tok0=tok0: e.dma_start(out=gts[gb][:], in_=gtv[nt * 128:(nt + 1) * 128, :, tok0:tok0 + NT]),
                      reads=["gt"], writes=["gts%d" % gb])
                for i in range(4):
                    pi = (n % 2) * 4 + i
                    for kc in range(4):
                        P.op("pe", lambda e, pi=pi, i=i, kc=kc, nt=nt, bb=bb: e.matmul(
                            ps[pi][:, 0:NT], lhsT=wb[:, i * 4 + kc, nt * 128:(nt + 1) * 128], rhs=brT[bb][:, i * 4 + kc, :],
                            start=(kc == 0), stop=(kc == 3)), reads=wkeys + ["brT%d" % bb], writes=["p3_%d" % pi])
                    P.op("dve", lambda e, pi=pi, i=i, gb=gb: e.tensor_tensor(out=tmp[i][:], in0=ps[pi][:, 0:NT], in1=gts[gb][:, i, :], op=ALU.mult),
                         reads=["p3_%d" % pi, "gts%d" % gb], writes=["tmp3_%d" % i])
                P.op("pool", lambda e: e.tensor_tensor(out=tmp[0][:], in0=tmp[0][:], in1=tmp[1][:], op=ALU.add),
                     reads=["tmp3_0", "tmp3_1"], writes=["tmp3_0"])
                P.op("pool", lambda e: e.tensor_tensor(out=tmp[2][:], in0=tmp[2][:], in1=tmp[3][:], op=ALU.add),
                     reads=["tmp3_2", "tmp3_3"], writes=["tmp3_2"])
                P.op("pool", lambda e, gb=gb: e.tensor_tensor(out=ms[gb][:], in0=tmp[0][:], in1=tmp[2][:], op=ALU.add),
                     reads=["tmp3_0", "tmp3_2"], writes=["ms%d" % gb])
                P.dma("sp", lambda e, gb=gb, nt=nt, tok0=tok0: e.dma_start(out=mT[nt * 128:(nt + 1) * 128, tok0:tok0 + NT], in_=ms[gb][:]),
                      reads=["ms%d" % gb], writes=["mT"])
                n += 1


def phase3b(K, l):
    P, I, S = K.P, K.I, K.S
    mT, modv = K.Sc["mT"], K.Sc["modv"]
    xd = K.xres
    NTT = S // 128
    MT = min(512, S)
    with Phase(K) as ph:
        eps_t = ph.sb("eps3", [128, 1])
        P.op("dve", lambda e: e.memset(eps_t[:], NORM_EPS), writes=["eps3"])
        wof = ph.sb("wof", [128, 4, D])
        wo = ph.sb("wo", [128, 16, D], BF16)
        for i in range(4):
            load_cast(K, I["w_out"][l][i * 512:(i + 1) * 512, :].rearrange("(kc p) n -> p kc n", p=128), wof[:],
                      wo[:, i * 4:(i + 1) * 4, :], "wof", "wo_%d" % i)
        wkeys = ["wo_%d" % i for i in range(4)]
        wr = ph.sb("wr", [128, 16, NE])
        P.dma("sp", lambda e: e.dma_start(out=wr[:], in_=I["w_router"][l].rearrange("(k p) n -> p k n", p=128)), writes=["wr"])
        G1 = load_bcast_row(K, ph, "G1_b", modv[l * 6 + 2:l * 6 + 3, :], D)
        B2 = load_bcast_row(K, ph, "B2_b", modv[l * 6 + 3:l * 6 + 4, :], D)
        A2 = load_bcast_row(K, ph, "A2_b", modv[l * 6 + 4:l * 6 + 5, :], D)
        mTt = [ph.sb("mTt%d" % i, [128, 16, MT], BF16) for i in range(2)]
        xt = [ph.sb("x3t%d" % i, [128, D]) for i in range(2)]
        tmp = [ph.sb("tmp3b%d" % i, [128, 512]) for i in range(2)]
        junk = ph.sb("junk3", [128, D], BF16)
        h2b = [ph.sb("h2b%d" % i, [128, D], BF16) for i in range(2)]
        ss = ph.sb("ss3", [128, 1])
        rstd = ph.sb("rstd3", [128, 1])
        h2T = ph.sb("h2T", [128, 16, 128])
        lg = ph.sb("lg", [128, NE])
        mx = ph.sb("mx3", [128, 1])
        sm = ph.sb("sm3", [128, 1])
        aff = ph.sb("aff_all", [128, NTT, NE])
        affT = [ph.sb("affT_s%d" % i, [NE, 128]) for i in range(2)]
        po = [ph.ps("p3o%d" % i, [128, 512]) for i in range(2)]
        ptr = [ph.ps("p3t%d" % i, [128, 512]) for i in range(2)]
        pr = ph.ps("p3r", [128, 512])
        pat = ph.ps("p3at", [128, 512])
        mTv = mT.rearrange("(k p) s -> p k s", p=128)
        n = 0
        for st in range(S // MT):
            mb = st % 2
            P.dma("sp", lambda e, mb=mb, st=st: e.dma_start(out=mTt[mb][:], in_=mTv[:, :, st * MT:(st + 1) * MT]), reads=["mT"], writes=["mTt%d" % mb])
            for t in range(MT // 128):
                tt = st * (MT // 128) + t
                t0 = tt * 128
                xb = tt % 2
                P.dma("sp", lambda e, xb=xb, t0=t0: e.dma_start(out=xt[xb][:], in_=xd[t0:t0 + 128, :]), reads=["xres"], writes=["x3t%d" % xb])
                for c4 in range(4):
                    pi = n % 2
                    n += 1
                    for kc in range(16):
                        P.op("pe", lambda e, pi=pi, kc=kc, mb=mb, t=t, c4=c4: e.matmul(
                            po[pi][:], lhsT=mTt[mb][:, kc, t * 128:(t + 1) * 128], rhs=wo[:, kc, c4 * 512:(c4 + 1) * 512],
                            start=(kc == 0), stop=(kc == 15)), reads=wkeys + ["mTt%d" % mb], writes=["p3o%d" % pi])
                    P.op("dve", lambda e, pi=pi, c4=c4: e.tensor_tensor(out=tmp[pi][:], in0=po[pi][:], in1=G1[:, c4 * 512:(c4 + 1) * 512], op=ALU.mult),
                         reads=["p3o%d" % pi, "G1_b"], writes=["tmp3b%d" % pi])
                    P.op("pool", lambda e, pi=pi, c4=c4, xb=xb: e.tensor_tensor(out=xt[xb][:, c4 * 512:(c4 + 1) * 512], in0=xt[xb][:, c4 * 512:(c4 + 1) * 512],
                                                                                in1=tmp[pi][:], op=ALU.add),
                         reads=["tmp3b%d" % pi, "x3t%d" % xb], writes=["x3t%d" % xb])
                P.dma("sp", lambda e, xb=xb, t0=t0: e.dma_start(out=xd[t0:t0 + 128, :], in_=xt[xb][:]), reads=["x3t%d" % xb], writes=["xres"])
                P.op("act", lambda e, xb=xb: e.activation(out=junk[:], in_=xt[xb][:], func=AF.Square, accum_out=ss[:, 0:1]),
                     reads=["x3t%d" % xb], writes=["junk3", "ss3"])
                P.op("act", lambda e: e.activation(out=rstd[:], in_=ss[:], func=AF.Sqrt, scale=1.0 / D, bias=eps_t[:, 0:1]),
                     reads=["ss3", "eps3"], writes=["rstd3"])
                P.op("dve", lambda e: e.reciprocal(out=rstd[:], in_=rstd[:]), reads=["rstd3"], writes=["rstd3"])
                P.op("dve", lambda e, xb=xb: e.scalar_tensor_tensor(out=xt[xb][:], in0=xt[xb][:], scalar=rstd[:, 0:1], in1=A2[:], op0=ALU.mult, op1=ALU.mult),
                     reads=["x3t%d" % xb, "rstd3", "A2_b"], writes=["x3t%d" % xb])
                P.op("pool", lambda e, xb=xb: e.tensor_tensor(out=xt[xb][:], in0=xt[xb][:], in1=B2[:], op=ALU.add),
                     reads=["x3t%d" % xb, "B2_b"], writes=["x3t%d" % xb])
                P.op("act", lambda e, xb=xb: e.copy(out=h2b[xb][:], in_=xt[xb][:]), reads=["x3t%d" % xb], writes=["h2b%d" % xb])
                P.dma("sp", lambda e, xb=xb, t0=t0: e.dma_start(out=K.Sc["h2"][t0:t0 + 128, :], in_=h2b[xb][:]), reads=["h2b%d" % xb], writes=["h2"])
                for g4 in range(4):
                    tb = g4 % 2
                    for j in range(4):
                        kc = g4 * 4 + j
                        P.op("pe", lambda e, tb=tb, j=j, kc=kc, xb=xb: e.transpose(ptr[tb][:, j * 128:(j + 1) * 128], xt[xb][:, kc * 128:(kc + 1) * 128], K.ident[:]),
                             reads=["x3t%d" % xb, "c_ident"], writes=["p3t%d" % tb])
                    P.op("act", lambda e, tb=tb, g4=g4: e.copy(out=h2T[:, g4 * 4:(g4 + 1) * 4, :], in_=ptr[tb][:].rearrange("p (j t) -> p j t", j=4)),
                         reads=["p3t%d" % tb], writes=["h2T"])
                for kc in range(16):
                    P.op("pe", lambda e, kc=kc: e.matmul(pr[:, 0:NE], lhsT=h2T[:, kc, :], rhs=wr[:, kc, :], start=(kc == 0), stop=(kc == 15)),
                         reads=["h2T", "wr"], writes=["p3r"])
                P.op("dve", lambda e: e.tensor_reduce(out=mx[:], in_=pr[:, 0:NE], axis=AX.X, op=ALU.max), reads=["p3r"], writes=["mx3"])
                P.op("dve", lambda e: e.tensor_scalar(out=mx[:], in0=mx[:], scalar1=-1.0, scalar2=None, op0=ALU.mult), reads=["mx3"], writes=["mx3"])
                P.op("act", lambda e: e.activation(out=lg[:], in_=pr[:, 0:NE], func=AF.Exp, bias=mx[:, 0:1], accum_out=sm[:, 0:1]),
                     reads=["p3r", "mx3"], writes=["lg", "sm3"])
                P.op("dve", lambda e: e.reciprocal(out=sm[:], in_=sm[:]), reads=["sm3"], writes=["sm3"])
                P.op("dve", lambda e, tt=tt: e.tensor_scalar(out=aff[:, tt, :], in0=lg[:], scalar1=sm[:, 0:1], scalar2=None, op0=ALU.mult),
                     reads=["lg", "sm3"], writes=["aff%d" % tt])
                P.op("pe", lambda e, tt=tt: e.transpose(pat[0:NE, 0:128], aff[:, tt, :], K.ident[:]), reads=["aff%d" % tt, "c_ident"], writes=["p3at"])
                P.op("act", lambda e, xb=xb: e.copy(out=affT[xb][:], in_=pat[0:NE, 0:128]), reads=["p3at"], writes=["affT%d" % xb])
                P.dma("sp", lambda e, xb=xb, t0=t0: e.dma_start(out=K.Sc["affT"][:, t0:t0 + 128], in_=affT[xb][:]), reads=["affT%d" % xb], writes=["affT_d"])
        akeys = ["aff%d" % tt for tt in range(NTT)]
        P.dma("sp", lambda e: e.dma_start(out=K.Sc["aff"].rearrange("(t p) n -> p t n", p=128), in_=aff[:]), reads=akeys, writes=["aff_d"])


def phase_moe(K, l):
    P, I, S = K.P, K.I, K.S
    NTT = S // 128
    CAP = S // 8
    NSL = min(128, CAP)
    NST = CAP // NSL
    h2, affd, affTd, ybuf, rankTd = K.Sc["h2"], K.Sc["aff"], K.Sc["affT"], K.Sc["ybuf"], K.Sc["rankT"]
    cast_i = [0]

    def cast(dst, src, rk, wk):
        eng = ("pool", "act")[cast_i[0] % 2]
        cast_i[0] += 1
        if eng == "pool":
            P.op("pool", lambda e: e.tensor_copy(out=dst, in_=src), reads=[rk], writes=[wk])
        else:
            P.op("act", lambda e: e.copy(out=dst, in_=src), reads=[rk], writes=[wk])

    with Phase(K) as ph:
        aff = ph.sb("m_aff", [128, NTT, NE])
        rank = ph.sb("e_rank", [128, NTT, NE])
        affb = ph.sb("m_affb", [128, S])
        junk = ph.sb("m_junk", [128, S], BF16)
        rkT = [ph.sb("m_rkT%d" % i, [NE, 128]) for i in range(2)]
        P.dma("sp", lambda e: e.dma_start(out=aff[:], in_=affd.rearrange("(t p) n -> p t n", p=128)), reads=["aff_d"], writes=["m_aff"])

        def rank_gen(ex):
            P.dma("sp", lambda e: e.dma_start(out=affb[:], in_=affTd[ex:ex + 1, :].to_broadcast([128, S])), reads=["affT_d"], writes=["m_affb"])
            for tt in range(NTT):
                P.op("dve", lambda e, tt=tt: e.tensor_scalar(
                    out=junk[:], in0=affb[:], scalar1=aff[:, tt, ex:ex + 1], scalar2=0.0, op0=ALU.is_gt, op1=ALU.add,
                    accum_out=rank[:, tt, ex:ex + 1]), reads=["m_affb", "m_aff"], writes=["m_junk", "e_rank%d" % ex])
                yield

        def pull(g, n):
            for _ in range(n):
                try:
                    next(g)
                except StopIteration:
                    return

        h2h = ph.sb("e_h2h", [128, NTT, 512], BF16)
        Pm = ph.sb("e_Pm", [128, NTT, CAP], BF16)
        xgT = ph.sb("e_xgT", [128, 16, CAP], BF16)
        hidT = ph.sb("e_hidT", [128, 8, CAP], BF16)
        wst = [ph.sb("e_wst%d" % i, [128, 4096]) for i in range(3)]
        wbf = [ph.sb("e_wbf%d" % i, [128, 4096], BF16) for i in range(4)]
        s1 = [ph.sb("e_s1%d" % i, [128, CAP]) for i in range(2)]
        ystg = [ph.sb("e_ystg%d" % i, [NSL, 512], BF16) for i in range(2)]
        pg = [ph.ps("e_pg%d" % i, [128, 512]) for i in range(2)]
        p1 = [ph.ps("e_p1%d" % i, [128, 512]) for i in range(2)]
        p3 = [ph.ps("e_p3%d" % i, [128, 512]) for i in range(2)]
        py = [ph.ps("e_py%d" % i, [128, 512]) for i in range(2)]
        h2v = h2.rearrange("(t p) d -> p t d", p=128)
        nws = [0]
        nwb = [0]

        wl = []
        for ex_ in range(NE):
            for fq in range(4):
                wl.append((I["moe_w1"][l, ex_][:, fq * 256:(fq + 1) * 256], 16, 256))
                wl.append((I["moe_w3"][l, ex_][:, fq * 256:(fq + 1) * 256], 16, 256))
            for dc in range(4):
                wl.append((I["moe_w2"][l, ex_][:, dc * 512:(dc + 1) * 512], 8, 512))
        issued = [0]
        wviews = {}

        def ensure(j):
            while issued[0] <= min(j, len(wl) - 1):
                i = issued[0]
                issued[0] += 1
                src_ap, nk, ncol = wl[i]
                si, bi = i % 3, i % 4
                sv = wst[si][:, 0:nk * ncol].rearrange("p (k n) -> p k n", k=nk)
                bv = wbf[bi][:, 0:nk * ncol].rearrange("p (k n) -> p k n", k=nk)
                P.dma("sp", lambda e, sv=sv, src_ap=src_ap: e.dma_start(out=sv, in_=src_ap.rearrange("(k p) n -> p k n", p=128)),
                      writes=["e_wst%d" % si])
                cast(bv, sv, "e_wst%d" % si, "e_wbf%d" % bi)
                wviews[i] = (bv, "e_wbf%d" % bi)

        wpos = [0]

        def load_w(src_ap, nk, ncol):
            i = wpos[0]
            wpos[0] += 1
            ensure(i + 2)
            return wviews[i]

        g0 = rank_gen(0)
        pull(g0, NTT + 1)
        for ex in range(NE):
            for tt in range(NTT):
                P.op("dve", lambda e, tt=tt, ex=ex: e.tensor_scalar(out=Pm[:, tt, :], in0=K.io[:, 0:CAP], scalar1=rank[:, tt, ex:ex + 1], scalar2=None,
                                                                    op0=ALU.is_equal), reads=["e_rank%d" % ex, "c_io"], writes=["e_Pm%d" % tt])
            pkeys = ["e_Pm%d" % tt for tt in range(NTT)]
            rg = rank_gen(ex + 1) if ex + 1 < NE else iter(())
            per = max(1, (NTT + 27) // 28)
            pull(rg, 2 * per)
            ng = 0
            ensure(wpos[0] + 1)
            for dh in range(4):
                P.dma("sp", lambda e, dh=dh: e.dma_start(out=h2h[:], in_=h2v[:, :, dh * 512:(dh + 1) * 512]), reads=["h2"], writes=["e_h2h"])
                for dt in range(4):
                    gi = ng % 2
                    ng += 1
                    for tt in range(NTT):
                        P.op("pe", lambda e, gi=gi, tt=tt, dt=dt: e.matmul(pg[gi][:, 0:CAP], lhsT=h2h[:, tt, dt * 128:(dt + 1) * 128], rhs=Pm[:, tt, :],
                                                                           start=(tt == 0), stop=(tt == NTT - 1)), reads=["e_h2h"] + pkeys, writes=["e_pg%d" % gi])
                    P.op("act", lambda e, gi=gi, dh=dh, dt=dt: e.copy(out=xgT[:, dh * 4 + dt, :], in_=pg[gi][:, 0:CAP]), reads=["e_pg%d" % gi], writes=["e_xgT"])
                    pull(rg, per)
            nf = 0
            for fq in range(4):
                w1v, w1k = load_w(I["moe_w1"][l, ex][:, fq * 256:(fq + 1) * 256], 16, 256)
                w3v, w3k = load_w(I["moe_w3"][l, ex][:, fq * 256:(fq + 1) * 256], 16, 256)
                for j in range(2):
                    fi = nf % 2
                    nf += 1
                    for kc in range(16):
                        P.op("pe", lambda e, fi=fi, kc=kc, j=j, w1v=w1v: e.matmul(p1[fi][:, 0:CAP], lhsT=w1v[:, kc, j * 128:(j + 1) * 128], rhs=xgT[:, kc, :],
                                                                                 start=(kc == 0), stop=(kc == 15)), reads=[w1k, "e_xgT"], writes=["e_p1%d" % fi])
                    for kc in range(16):
                        P.op("pe", lambda e, fi=fi, kc=kc, j=j, w3v=w3v: e.matmul(p3[fi][:, 0:CAP], lhsT=w3v[:, kc, j * 128:(j + 1) * 128], rhs=xgT[:, kc, :],
                                                                                 start=(kc == 0), stop=(kc == 15)), reads=[w3k, "e_xgT"], writes=["e_p3%d" % fi])
                    P.op("act", lambda e, fi=fi: e.activation(out=s1[fi][:], in_=p1[fi][:, 0:CAP], func=AF.Silu), reads=["e_p1%d" % fi], writes=["e_s1%d" % fi])
                    P.op("dve", lambda e, fi=fi, fq=fq, j=j: e.tensor_tensor(out=hidT[:, fq * 2 + j, :], in0=p3[fi][:, 0:CAP], in1=s1[fi][:], op=ALU.mult),
                         reads=["e_p3%d" % fi, "e_s1%d" % fi], writes=["e_hidT"])
                    pull(rg, per)
            ny = 0
            for dc in range(4):
                w2v, w2k = load_w(I["moe_w2"][l, ex][:, dc * 512:(dc + 1) * 512], 8, 512)
                for st in range(NST):
                    yi = ny % 2
                    ny += 1
                    for fc in range(8):
                        P.op("pe", lambda e, yi=yi, fc=fc, st=st, w2v=w2v: e.matmul(py[yi][0:NSL, :], lhsT=hidT[:, fc, st * NSL:(st + 1) * NSL], rhs=w2v[:, fc, :],
                                                                                   start=(fc == 0), stop=(fc == 7)), reads=[w2k, "e_hidT"], writes=["e_py%d" % yi])
                    P.op("act", lambda e, yi=yi: e.copy(out=ystg[yi][:], in_=py[yi][0:NSL, :]), reads=["e_py%d" % yi], writes=["e_ystg%d" % yi])
                    P.dma("sp", lambda e, yi=yi, ex=ex, st=st, dc=dc: e.dma_start(out=ybuf[ex][st * NSL:(st + 1) * NSL, dc * 512:(dc + 1) * 512], in_=ystg[yi][:]),
                          reads=["e_ystg%d" % yi], writes=["ybuf"])
            pull(rg, NTT + 1)
        for tt in range(NTT):
            ri = tt % 2
            P.op("pe", lambda e, tt=tt: e.transpose(pg[0][0:NE, 0:128], rank[:, tt, :], K.ident[:]),
                 reads=["e_rank%d" % ex_ for ex_ in range(NE)] + ["c_ident"], writes=["e_pg0"])
            P.op("act", lambda e, ri=ri: e.copy(out=rkT[ri][:], in_=pg[0][0:NE, 0:128]), reads=["e_pg0"], writes=["m_rkT%d" % ri])
            P.dma("sp", lambda e, ri=ri, tt=tt: e.dma_start(out=rankTd[:, tt * 128:(tt + 1) * 128], in_=rkT[ri][:]), reads=["m_rkT%d" % ri], writes=["rankT_d"])
    with Phase(K) as ph:
        TB = min(256, S)
        NTB = TB // 128
        G2 = load_bcast_row(K, ph, "G2_b", K.Sc["modv"][l * 6 + 5:l * 6 + 6, :], D)
        sid = ph.sb("s_sid", [128, NST])
        for st in range(NST):
            P.op("dve", lambda e, st=st: e.tensor_scalar(out=sid[:, st:st + 1], in0=K.iop[:, 0:1], scalar1=float(st * NSL), scalar2=None, op0=ALU.add),
                 reads=["c_iop"], writes=["s_sid"])
        rb = [ph.sb("s_rb%d" % i, [128, TB]) for i in range(2)]
        ab = [ph.sb("s_ab%d" % i, [128, TB]) for i in range(2)]
        pgt = [ph.sb("s_pgt%d" % i, [128, TB], BF16) for i in range(3)]
        yt = [ph.sb("s_yt%d" % i, [128, D], BF16) for i in range(3)]
        xt = [ph.sb("s_xt%d" % i, [128, D]) for i in range(2)]
        tmp = [ph.sb("s_tmp%d" % i, [128, 512]) for i in range(2)]
        pa = [ph.ps("s_pa%d" % i, [128, 512]) for i in range(NTB * 4)]
        nb = 0
        for blk in range(S // TB):
            tok0 = blk * TB
            for ex in range(NE):
                bi = ex % 2
                P.dma("sp", lambda e, bi=bi, ex=ex, tok0=tok0: e.dma_start(out=rb[bi][0:NSL, :], in_=rankTd[ex:ex + 1, tok0:tok0 + TB].to_broadcast([NSL, TB])),
                      reads=["rankT_d"], writes=["s_rb%d" % bi])
                P.dma("sp", lambda e, bi=bi, ex=ex, tok0=tok0: e.dma_start(out=ab[bi][0:NSL, :], in_=affTd[ex:ex + 1, tok0:tok0 + TB].to_broadcast([NSL, TB])),
                      reads=["affT_d"], writes=["s_ab%d" % bi])
                for st in range(NST):
                    ci = nb % 3
                    nb += 1
                    first = (ex == 0 and st == 0)
                    last = (ex == NE - 1 and st == NST - 1)
                    P.op("dve", lambda e, ci=ci, bi=bi, st=st: e.scalar_tensor_tensor(out=pgt[ci][0:NSL, :], in0=rb[bi][0:NSL, :], scalar=sid[0:NSL, st:st + 1],
                                                                                    in1=ab[bi][0:NSL, :], op0=ALU.is_equal, op1=ALU.mult),
                         reads=["s_rb%d" % bi, "s_ab%d" % bi, "s_sid"], writes=["s_pgt%d" % ci])
                    P.dma("sp", lambda e, ci=ci, ex=ex, st=st: e.dma_start(out=yt[ci][0:NSL, :], in_=ybuf[ex][st * NSL:(st + 1) * NSL, :]),
                          reads=["ybuf"], writes=["s_yt%d" % ci])
                    for tb in range(NTB):
                        for dc in range(4):
                            P.op("pe", lambda e, ci=ci, tb=tb, dc=dc, first=first, last=last: e.matmul(
                                pa[tb * 4 + dc][:], lhsT=pgt[ci][0:NSL, tb * 128:(tb + 1) * 128], rhs=yt[ci][0:NSL, dc * 512:(dc + 1) * 512],
                                start=first, stop=last), reads=["s_pgt%d" % ci, "s_yt%d" % ci], writes=["s_pa%d" % (tb * 4 + dc)])
            for tb in range(NTB):
                t0 = tok0 + tb * 128
                xb = tb % 2
                P.dma("sp", lambda e, xb=xb, t0=t0: e.dma_start(out=xt[xb][:], in_=K.xres[t0:t0 + 128, :]), reads=["xres"], writes=["s_xt%d" % xb])
                for dc in range(4):
                    ti = dc % 2
                    P.op("dve", lambda e, ti=ti, tb=tb, dc=dc: e.tensor_tensor(out=tmp[ti][:], in0=pa[tb * 4 + dc][:], in1=G2[:, dc * 512:(dc + 1) * 512], op=ALU.mult),
                         reads=["s_pa%d" % (tb * 4 + dc), "G2_b"], writes=["s_tmp%d" % ti])
                    P.op("pool", lambda e, ti=ti, xb=xb, dc=dc: e.tensor_tensor(out=xt[xb][:, dc * 512:(dc + 1) * 512], in0=xt[xb][:, dc * 512:(dc + 1) * 512],
                                                                                in1=tmp[ti][:], op=ALU.add), reads=["s_tmp%d" % ti, "s_xt%d" % xb], writes=["s_xt%d" % xb])
                P.dma("sp", lambda e, xb=xb, t0=t0: e.dma_start(out=K.xres[t0:t0 + 128, :], in_=xt[xb][:]), reads=["s_xt%d" % xb], writes=["xres"])


def phase_rwkv_prep(K, l):
    P, I, S = K.P, K.I, K.S
    fm = K.Sc["fm"]
    TT = min(512, S)
    NCH = TT // CH
    R0 = 1536
    with Phase(K) as ph:
        mu = ph.sb("r_mu", [128, 15, 3])
        muv = I["rwkv_mu"][l]
        for m_ in range(2):
            for rt in range(15):
                P.dma("sp", lambda e, m_=m_, rt=rt: e.dma_start(out=mu[:, rt, m_:m_ + 1], in_=muv[m_:m_ + 1, rt * 128:(rt + 1) * 128].rearrange("o p -> p o")),
                      writes=["r_mu"])
        P.op("dve", lambda e: e.tensor_tensor(out=mu[:, :, 2:3], in0=mu[:, :, 0:1], in1=mu[:, :, 1:2], op=ALU.add), reads=["r_mu"], writes=["r_mu"])
        P.op("dve", lambda e: e.tensor_scalar(out=mu[:, :, 2:3], in0=mu[:, :, 2:3], scalar1=-1.0, scalar2=1.0, op0=ALU.mult, op1=ALU.add),
             reads=["r_mu"], writes=["r_mu"])
        pc = ph.sb("r_pc", [128, 4, 8])
        srcs = [I["rwkv_w0"][l][0:1, :], I["rwkv_w0"][l][1:2, :], I["rwkv_a0"][l][0:1, :], I["rwkv_a0"][l][1:2, :],
                I["rwkv_k_k"][l:l + 1, :], I["rwkv_k_a"][l:l + 1, :], None, I["rwkv_r_k"][l:l + 1, :]]
        for ci, src in enumerate(srcs):
            if src is None:
                continue
            for ct in range(4):
                P.dma("sp", lambda e, ci=ci, ct=ct, src=src: e.dma_start(out=pc[:, ct, ci:ci + 1], in_=src[:, ct * 128:(ct + 1) * 128].rearrange("o p -> p o")),
                      writes=["r_pc"])
        P.op("dve", lambda e: e.tensor_scalar(out=pc[:, :, 6:7], in0=pc[:, :, 5:6], scalar1=-1.0, scalar2=1.0, op0=ALU.mult, op1=ALU.add),
             reads=["r_pc"], writes=["r_pc"])
        wst = ph.sb("r_wst", [128, 512])
        w2p = [ph.sb("r_w2p%d" % d, [128, 512], BF16) for d in range(2)]
        a2p = [ph.sb("r_a2p%d" % d, [128, 512], BF16) for d in range(2)]
        g2b = ph.sb("r_g2b", [128, 512], BF16)
        for name, dst in (("rwkv_w2", w2p), ("rwkv_a2", a2p)):
            P.dma("sp", lambda e, name=name: e.dma_start(out=wst[:], in_=I[name][l]), writes=["r_wst"])
            for d in range(2):
                P.op("dve", lambda e, d=d, dst=dst: e.memset(dst[d][:], 0.0), writes=["r_lw%s%d" % (name, d)])
                P.op("dve", lambda e, d=d, dst=dst: e.tensor_copy(out=dst[d][d * 64:(d + 1) * 64, :], in_=wst[d * 64:(d + 1) * 64, :]),
                     reads=["r_wst"], writes=["r_lw%s%d" % (name, d)])
        P.dma("sp", lambda e: e.dma_start(out=wst[:], in_=I["rwkv_g2"][l]), writes=["r_wst"])
        P.op("dve", lambda e: e.tensor_copy(out=g2b[:], in_=wst[:]), reads=["r_wst"], writes=["r_g2b"])
        lwk = ["r_lwrwkv_w2%d" % d for d in range(2)] + ["r_lwrwkv_a2%d" % d for d in range(2)] + ["r_g2b"]
        bones = ph.sb("r_bones", [128, 128])
        P.op("dve", lambda e: e.memset(bones[:], 0.0), writes=["r_bones"])
        P.op("dve", lambda e: e.memset(bones[0:64, 0:64], 1.0), writes=["r_bones"])
        P.op("dve", lambda e: e.memset(bones[64:128, 64:128], 1.0), writes=["r_bones"])
        ones = ph.sb("r_ones", [128, CH])
        P.op("dve", lambda e: e.memset(ones[:], 1.0), writes=["r_ones"])
        tiny = ph.sb("r_tiny", [128, 1])
        P.op("dve", lambda e: e.memset(tiny[:], 1e-24), writes=["r_tiny"])
        xr = [ph.sb("r_xr%d" % i, [128, TT + 2]) for i in range(2)]
        z = [ph.sb("r_z%d" % i, [128, TT]) for i in range(15)]
        twb = ph.sb("r_twb", [128, TT], BF16)
        adb = ph.sb("r_adb", [128, TT], BF16)
        sgb = ph.sb("r_sgb", [128, TT], BF16)
        ld = [ph.sb("r_ld%d" % d, [128, TT]) for d in range(2)]
        av = [ph.sb("r_a%d" % d, [128, TT]) for d in range(2)]
        kd = [ph.sb("r_kd%d" % d, [128, TT]) for d in range(2)]
        bb = [ph.sb("r_bb%d" % d, [128, TT]) for d in range(2)]
        gt_ = ph.sb("r_g", [128, TT])
        kq = ph.sb("r_kq", [128, TT])
        kkt = ph.sb("r_kk", [128, TT])
        t1 = ph.sb("r_t1", [128, TT])
        t2 = ph.sb("r_t2", [128, TT])
        lam = ph.sb("r_lam", [128, TT])
        Lt = ph.sb("r_L", [128, TT])
        E = [ph.sb("r_E%d" % i, [128, TT]) for i in range(4)]
        outs = [ph.sb("r_o%d" % i, [128, TT]) for i in range(6)]
        gc = ph.sb("r_gc", [128, NCH])
        tro = [ph.sb("r_tro%d" % i, [128, 128]) for i in range(2)]
        pm = [ph.ps("r_pm%d" % i, [128, 512]) for i in range(4)]
        ptr = [ph.ps("r_ptr%d" % i, [128, 512]) for i in range(2)]
        npm = [0]
        ntr = [0]

        def transpose_store(src, skey, dst_dram, t0):
            for tb in range(TT // 128):
                i = ntr[0] % 2
                ntr[0] += 1
                P.op("pe", lambda e, i=i, tb=tb: e.transpose(ptr[i][:, 0:128], src[:, tb * 128:(tb + 1) * 128], K.ident[:]),
                     reads=[skey, "c_ident"], writes=["r_ptr%d" % i])
                P.op("act", lambda e, i=i: e.copy(out=tro[i][:], in_=ptr[i][:, 0:128]), reads=["r_ptr%d" % i], writes=["r_tro%d" % i])
                P.dma("sp", lambda e, i=i, tb=tb: e.dma_start(out=dst_dram[t0 + tb * 128:t0 + (tb + 1) * 128, :], in_=tro[i][:]),
                      reads=["r_tro%d" % i], writes=["r_scr"])

        for st in range(S // TT):
            t0 = st * TT
            lo, hi = max(t0 - 1, 0), min(t0 + TT + 1, S)
            for rt in range(15):
                b = rt % 2
                k = "r_xr%d" % b
                P.op("pool", lambda e, b=b: e.memset(xr[b][:, 0:1], 0.0), writes=[k])
                P.op("pool", lambda e, b=b: e.memset(xr[b][:, TT + 1:TT + 2], 0.0), writes=[k])
                P.dma("sp", lambda e, b=b, rt=rt, lo=lo, hi=hi, t0=t0: e.dma_start(
                    out=xr[b][:, lo - (t0 - 1):hi - (t0 - 1)], in_=fm[R0 + rt * 128:R0 + (rt + 1) * 128, lo:hi]), reads=["fm"], writes=[k])
                zk = "r_z%d" % rt
                P.op("dve", lambda e, b=b, rt=rt: e.tensor_scalar(out=z[rt][:], in0=xr[b][:, 1:TT + 1], scalar1=mu[:, rt, 2:3], scalar2=None, op0=ALU.mult),
                     reads=[k, "r_mu"], writes=[zk])
                P.op("dve", lambda e, b=b, rt=rt: e.scalar_tensor_tensor(out=z[rt][:], in0=xr[b][:, 0:TT], scalar=mu[:, rt, 0:1], in1=z[rt][:],
                                                                        op0=ALU.mult, op1=ALU.add), reads=[k, zk], writes=[zk])
                P.op("dve", lambda e, b=b, rt=rt: e.scalar_tensor_tensor(out=z[rt][:], in0=xr[b][:, 2:TT + 2], scalar=mu[:, rt, 1:2], in1=z[rt][:],
                                                                        op0=ALU.mult, op1=ALU.add), reads=[k, zk], writes=[zk])
            P.op("act", lambda e: e.activation(out=twb[:], in_=z[12][:], func=AF.Tanh), reads=["r_z12"], writes=["r_twb"])
            P.op("act", lambda e: e.copy(out=adb[:], in_=z[13][:]), reads=["r_z13"], writes=["r_adb"])
            P.op("act", lambda e: e.activation(out=sgb[:], in_=z[14][:], func=AF.Sigmoid), reads=["r_z14"], writes=["r_sgb"])
            for ct in range(4):
                zr, zk_, zv = z[ct], z[4 + ct], z[8 + ct]
                kr, kk_, kv = "r_z%d" % ct, "r_z%d" % (4 + ct), "r_z%d" % (8 + ct)
                cs = slice(ct * 128, (ct + 1) * 128)
                for d in range(2):
                    pi = npm[0] % 4
                    npm[0] += 1
                    P.op("pe", lambda e, pi=pi, d=d, cs=cs: e.matmul(pm[pi][:, 0:TT], lhsT=w2p[d][:, cs], rhs=twb[:], start=True, stop=True),
                         reads=lwk + ["r_twb"], writes=["r_pm%d" % pi])
                    P.op("act", lambda e, pi=pi, d=d, ct=ct: e.activation(out=ld[d][:], in_=pm[pi][:, 0:TT], func=AF.Sigmoid, bias=pc[:, ct, d:d + 1]),
                         reads=["r_pm%d" % pi, "r_pc"], writes=["r_ld%d" % d])
                    P.op("dve", lambda e, d=d: e.tensor_scalar(out=ld[d][:], in0=ld[d][:], scalar1=-0.6065306597126334, scalar2=None, op0=ALU.mult),
                         reads=["r_ld%d" % d], writes=["r_ld%d" % d])
                    pi = npm[0] % 4
                    npm[0] += 1
                    P.op("pe", lambda e, pi=pi, d=d, cs=cs: e.matmul(pm[pi][:, 0:TT], lhsT=a2p[d][:, cs], rhs=adb[:], start=True, stop=True),
                         reads=lwk + ["r_adb"], writes=["r_pm%d" % pi])
                    P.op("act", lambda e, pi=pi, d=d, ct=ct: e.activation(out=av[d][:], in_=pm[pi][:, 0:TT], func=AF.Sigmoid, bias=pc[:, ct, 2 + d:3 + d]),
                         reads=["r_pm%d" % pi, "r_pc"], writes=["r_a%d" % d])
                pi = npm[0] % 4
                npm[0] += 1
                P.op("pe", lambda e, pi=pi, cs=cs: e.matmul(pm[pi][:, 0:TT], lhsT=g2b[:, cs], rhs=sgb[:], start=True, stop=True),
                     reads=lwk + ["r_sgb"], writes=["r_pm%d" % pi])
                P.op("act", lambda e, pi=pi: e.copy(out=gt_[:], in_=pm[pi][:, 0:TT]), reads=["r_pm%d" % pi], writes=["r_g"])
                P.dma("sp", lambda e, cs=cs, t0=t0: e.dma_start(out=K.Sc["rg"][cs, t0:t0 + TT], in_=gt_[:]), reads=["r_g"], writes=["r_scr"])
                P.op("dve", lambda e, ct=ct, zk_=zk_: e.tensor_scalar(out=kq[:], in0=zk_[:], scalar1=pc[:, ct, 4:5], scalar2=None, op0=ALU.mult),
                     reads=[kk_, "r_pc"], writes=["r_kq"])
                P.op("pool", lambda e: e.tensor_tensor(out=t1[:], in0=kq[:], in1=kq[:], op=ALU.mult), reads=["r_kq"], writes=["r_t1"])
                pi = npm[0] % 4
                npm[0] += 1
                P.op("pe", lambda e, pi=pi: e.matmul(pm[pi][:, 0:TT], lhsT=bones[:], rhs=t1[:], start=True, stop=True),
                     reads=["r_bones", "r_t1"], writes=["r_pm%d" % pi])
                P.op("act", lambda e, pi=pi: e.activation(out=t2[:], in_=pm[pi][:, 0:TT], func=AF.Sqrt, bias=tiny[:, 0:1]),
                     reads=["r_pm%d" % pi, "r_tiny"], writes=["r_t2"])
                P.op("dve", lambda e: e.reciprocal(out=t2[:], in_=t2[:]), reads=["r_t2"], writes=["r_t2"])
                P.op("dve", lambda e: e.tensor_tensor(out=kkt[:], in0=kq[:], in1=t2[:], op=ALU.mult), reads=["r_kq", "r_t2"], writes=["r_kk"])
                for d in range(2):
                    P.op("dve", lambda e, d=d, ct=ct: e.tensor_scalar(out=t1[:], in0=av[d][:], scalar1=pc[:, ct, 5:6], scalar2=pc[:, ct, 6:7],
                                                                    op0=ALU.mult, op1=ALU.add), reads=["r_a%d" % d, "r_pc"], writes=["r_t1"])
                    P.op("dve", lambda e, d=d, zk_=zk_: e.tensor_tensor(out=kd[d][:], in0=zk_[:], in1=t1[:], op=ALU.mult), reads=[kk_, "r_t1"], writes=["r_kd%d" % d])
                    P.op("pool", lambda e, d=d: e.tensor_tensor(out=bb[d][:], in0=av[d][:], in1=kkt[:], op=ALU.mult), reads=["r_a%d" % d, "r_kk"], writes=["r_bb%d" % d])
                P.op("pool", lambda e: e.tensor_tensor(out=t1[:], in0=kd[0][:], in1=kd[1][:], op=ALU.add), reads=["r_kd0", "r_kd1", "r_t1"], writes=["r_t1"])
                P.op("dve", lambda e, ct=ct, zr=zr: e.scalar_tensor_tensor(out=t1[:], in0=t1[:], scalar=pc[:, ct, 7:8], in1=zr[:], op0=ALU.mult, op1=ALU.mult),
                     reads=["r_t1", kr, "r_pc"], writes=["r_t1"])
                pi = npm[0] % 4
                npm[0] += 1
                P.op("pe", lambda e, pi=pi: e.matmul(pm[pi][:, 0:TT], lhsT=bones[:], rhs=t1[:], start=True, stop=True),
                     reads=["r_bones", "r_t1"], writes=["r_pm%d" % pi])
                P.op("dve", lambda e, pi=pi, zv=zv: e.tensor_tensor(out=t2[:], in0=pm[pi][:, 0:TT], in1=zv[:], op=ALU.mult), reads=["r_pm%d" % pi, kv], writes=["r_t2"])
                P.dma("sp", lambda e, cs=cs, t0=t0: e.dma_start(out=K.Sc["rbon"][cs, t0:t0 + TT], in_=t2[:]), reads=["r_t2"], writes=["r_scr"])
                transpose_store(zv, kv, K.Sc["rv"][:, cs], t0)
                for d in range(2):
                    for c in range(NCH):
                        P.op("dve", lambda e, c=c, d=d: e.tensor_tensor_scan(out=lam[:, c * CH:(c + 1) * CH], data0=ones[:, 0:CH], data1=ld[d][:, c * CH:(c + 1) * CH],
                                                                             initial=0.0, op0=ALU.mult, op1=ALU.add), reads=["r_ld%d" % d, "r_ones"], writes=["r_lam"])
                    lam3 = lam[:].rearrange("p (c t) -> p c t", t=CH)
                    lamC = lam3[:, :, CH - 1:CH].to_broadcast([128, NCH, CH])
                    L3 = Lt[:].rearrange("p (c t) -> p c t", t=CH)
                    if d == 0:
                        P.op("pool", lambda e: e.tensor_copy(out=Lt[:], in_=lam[:]), reads=["r_lam"], writes=["r_L"])
                    else:
                        P.op("dve", lambda e, L3=L3, lamC=lamC, lam3=lam3: e.tensor_tensor(out=L3, in0=lamC, in1=lam3, op=ALU.subtract), reads=["r_lam"], writes=["r_L"])
                        P.op("dve", lambda e, d=d: e.tensor_tensor(out=Lt[:], in0=Lt[:], in1=ld[d][:], op=ALU.add), reads=["r_L", "r_ld%d" % d], writes=["r_L"])
                    P.op("act", lambda e: e.activation(out=E[0][:], in_=Lt[:], func=AF.Exp, scale=-1.0), reads=["r_L"], writes=["r_E0"])
                    P.op("act", lambda e: e.activation(out=E[2][:], in_=Lt[:], func=AF.Exp), reads=["r_L"], writes=["r_E2"])
                    P.op("dve", lambda e, d=d: e.tensor_tensor(out=t1[:], in0=Lt[:], in1=ld[d][:], op=ALU.subtract), reads=["r_L", "r_ld%d" % d, "r_t1"], writes=["r_t1"])
                    P.op("act", lambda e: e.activation(out=E[1][:], in_=t1[:], func=AF.Exp), reads=["r_t1"], writes=["r_E1"])
                    P.op("dve", lambda e, L3=L3, lamC=lamC: e.tensor_tensor(out=t2[:].rearrange("p (c t) -> p c t", t=CH), in0=lamC, in1=L3, op=ALU.subtract),
                         reads=["r_L", "r_lam", "r_t2"], writes=["r_t2"])
                    P.op("act", lambda e: e.activation(out=E[3][:], in_=t2[:], func=AF.Exp), reads=["r_t2"], writes=["r_E3"])
                    P.op("act", lambda e, lam3=lam3: e.activation(out=gc[:], in_=lam3[:, :, CH - 1], func=AF.Exp), reads=["r_lam"], writes=["r_gc"])
                    P.dma("sp", lambda e, d=d, cs=cs, st=st: e.dma_start(out=K.Sc["rgc"][d][cs, st * NCH:(st + 1) * NCH], in_=gc[:]), reads=["r_gc"], writes=["r_scr"])
                    prods = [(kd[d], "r_kd%d" % d, 0, "dve"), (bb[d], "r_bb%d" % d, 0, "pool"), (kkt, "r_kk", 1, "dve"), (zr, kr, 2, "pool"),
                             (kd[d], "r_kd%d" % d, 3, "dve"), (bb[d], "r_bb%d" % d, 3, "pool")]
                    for oi, (src, skey, ei, eng) in enumerate(prods):
                        P.op(eng, lambda e, oi=oi, src=src, ei=ei: e.tensor_tensor(out=outs[oi][:], in0=src[:], in1=E[ei][:], op=ALU.mult),
                             reads=[skey, "r_E%d" % ei], writes=["r_o%d" % oi])
                    for oi in range(4):
                        P.dma("sp", lambda e, oi=oi, d=d, cs=cs, t0=t0: e.dma_start(out=K.Sc["rf"][d][oi][cs, t0:t0 + TT], in_=outs[oi][:]),
                              reads=["r_o%d" % oi], writes=["r_scr"])
                    transpose_store(outs[4], "r_o4", K.Sc["rt"][d][0][:, cs], t0)
                    transpose_store(outs[5], "r_o5", K.Sc["rt"][d][1][:, cs], t0)


def phase_rwkv_scan(K, l):
    P, S = K.P, K.S
    NC = S // CH
    with Phase(K) as ph:
        msk = {}
        for name, op in (("su", ALU.is_gt), ("iu", ALU.is_ge), ("sl", ALU.is_lt), ("il", ALU.is_le), ("eye", ALU.is_equal)):
            m = ph.sb("k_" + name, [CH, CH])
            P.op("dve", lambda e, m=m, op=op: e.tensor_scalar(out=m[:], in0=K.io[0:CH, 0:CH], scalar1=K.iop[0:CH, 0:1], scalar2=None, op0=op),
                 reads=["c_io", "c_iop"], writes=["k_msk"])
            msk[name] = m
        bc = lambda m: m[:].unsqueeze(1).to_broadcast([CH, 8, CH])
        spad = getattr(K.cfg, "spad", 0)
        PP = 128 if spad else CH

        class PadT:
            def __init__(self, t, flat):
                self.t, self.flat = t, flat

            def __getitem__(self, idx):
                v = self.t[0:CH, 0:8 * CH] if self.flat else self.t[0:CH, 0:8, :]
                return v[idx]

            def L(self, h):
                if self.flat:
                    return self.t[0:PP, h * CH:(h + 2) * CH] if spad else self.t[0:CH, h * CH:(h + 1) * CH]
                return self.t[0:PP, h:h + 2, :] if spad else self.t[0:CH, h, :]

            def R(self, h):
                if self.flat:
                    return self.t[0:PP, h * CH:(h + 1) * CH]
                return self.t[0:PP, h, :]

        def padsb(name, flat=False):
            t = ph.sb(name, [PP, 9 * CH] if flat else [PP, 9, CH])
            P.op("pool", lambda e: e.memset(t[:], 0.0), writes=[name])
            return PadT(t, flat)

        A = [ph.ps("k_A%d" % i, [PP, 8, CH]) for i in range(5)]
        Bp = [ph.ps("k_B%d" % i, [PP, 8, CH]) for i in range(3)]
        ST = [padsb("k_ST%d" % d) for d in range(2)]
        fmt = [[padsb("k_f%d_%d" % (d, i)) for i in range(4)] for d in range(2)]
        tmt = [[padsb("k_t%d_%d" % (d, i), flat=True) for i in range(3)] for d in range(2)]
        gcs = [ph.sb("k_gc%d" % d, [CH, 8]) for d in range(2)]
        Mk = padsb("k_Mk")
        Ak = padsb("k_Ak")
        Abn = padsb("k_Abn")
        Q = [padsb("k_Q%d" % i) for i in range(2)]
        QT = [padsb("k_QT%d" % i) for i in range(2)]
        Tc = [padsb("k_Tc%d" % i) for i in range(2)]
        W = padsb("k_W")
        Un = padsb("k_Un")
        Y = [ph.sb("k_Y%d" % i, [CH, 8 * CH]) for i in range(2)]
        tmpS = ph.sb("k_tmpS", [CH, 8, CH])

        def mm8(out_ps, okey, tl, tr, rkeys):
            for h in range(8):
                P.op("pe", lambda e, h=h: e.matmul(out_ps[0:PP, h, :], lhsT=tl.L(h), rhs=tr.R(h), start=True, stop=True), reads=rkeys, writes=[okey])

        for step in range(NC):
            for d in range(2):
                c = step if d == 0 else NC - 1 - step
                cs = slice(c * CH, (c + 1) * CH)
                mS, mI, mST = (msk["su"], msk["iu"], msk["sl"]) if d == 0 else (msk["sl"], msk["il"], msk["su"])
                f = fmt[d]
                t = tmt[d]
                fk = ["k_f%d_%d" % (d, i) for i in range(4)]
                tk = ["k_t%d_%d" % (d, i) for i in range(3)]
                for i in range(4):
                    P.dma("sp", lambda e, i=i, d=d, cs=cs, f=f: e.dma_start(out=f[i][:], in_=K.Sc["rf"][d][i].rearrange("(h j) s -> j h s", j=CH)[:, :, cs]),
                          reads=["r_scr"], writes=[fk[i]])
                for i in range(2):
                    P.dma("sp", lambda e, i=i, d=d, cs=cs, t=t: e.dma_start(out=t[i][:], in_=K.Sc["rt"][d][i][cs, :]), reads=["r_scr"], writes=[tk[i]])
                P.dma("sp", lambda e, cs=cs, t=t: e.dma_start(out=t[2][:], in_=K.Sc["rv"][cs, :]), reads=["r_scr"], writes=[tk[2]])
                P.dma("sp", lambda e, d=d, c=c: e.dma_start(out=gcs[d][:], in_=K.Sc["rgc"][d].rearrange("(h j) c -> j h c", j=CH)[:, :, c],
                                                                allow_slow_non_contiguous=True), reads=["r_scr"], writes=["k_gc%d" % d])
                Kt, Bt, Qt, Rt = f
                Kh, Bh, V = t
                hs = lambda h: slice(h * CH, (h + 1) * CH)
                mm8(A[0], "k_A0", Kt, Qt, [fk[0], fk[2]])
                mm8(A[1], "k_A1", Bt, Qt, [fk[1], fk[2]])
                mm8(A[2], "k_A2", Qt, Bt, [fk[1], fk[2]])
                mm8(A[3], "k_A3", Kt, Rt, [fk[0], fk[3]])
                mm8(A[4], "k_A4", Bt, Rt, [fk[1], fk[3]])
                P.op("dve", lambda e, mS=mS: e.tensor_tensor(out=Mk[:], in0=A[0][0:CH], in1=bc(mS), op=ALU.mult), reads=["k_A0", "k_msk"], writes=["k_Mk"])
                P.op("dve", lambda e, mS=mS: e.tensor_tensor(out=Q[0][:], in0=A[1][0:CH], in1=bc(mS), op=ALU.mult), reads=["k_A1", "k_msk"], writes=["k_Q0"])
                P.op("dve", lambda e, mST=mST: e.tensor_tensor(out=QT[0][:], in0=A[2][0:CH], in1=bc(mST), op=ALU.mult), reads=["k_A2", "k_msk"], writes=["k_QT0"])
                P.op("dve", lambda e, mI=mI: e.tensor_tensor(out=Ak[:], in0=A[3][0:CH], in1=bc(mI), op=ALU.mult), reads=["k_A3", "k_msk"], writes=["k_Ak"])
                P.op("dve", lambda e, mI=mI: e.tensor_tensor(out=Abn[:], in0=A[4][0:CH], in1=bc(mI), op=ALU.mult),
                     reads=["k_A4", "k_msk"], writes=["k_Abn"])
                P.op("pool", lambda e: e.tensor_tensor(out=Tc[0][:], in0=bc(msk["eye"]), in1=Q[0][:], op=ALU.subtract), reads=["k_Q0", "k_msk"], writes=["k_Tc0"])
                qi, ti = 0, 0
                for lev in range(1, 6):
                    qn = 1 - qi
                    if lev < 5:
                        mm8(A[0], "k_A0", QT[qi], Q[qi], ["k_Q%d" % qi, "k_QT%d" % qi])
                    mm8(A[1], "k_A1", Q[qi], QT[qi], ["k_Q%d" % qi, "k_QT%d" % qi])
                    if lev < 5:
                        P.op("act", lambda e, qn=qn: e.copy(out=Q[qn][:], in_=A[0][0:CH]), reads=["k_A0"], writes=["k_Q%d" % qn])
                    P.op("dve", lambda e, qn=qn: e.tensor_copy(out=QT[qn][:], in_=A[1][0:CH]), reads=["k_A1"], writes=["k_QT%d" % qn])
                    tn = 1 - ti
                    mm8(A[2], "k_A2", QT[qn], Tc[ti], ["k_QT%d" % qn, "k_Tc%d" % ti])
                    P.op("dve", lambda e, tn=tn, ti=ti: e.tensor_tensor(out=Tc[tn][:], in0=A[2][0:CH], in1=Tc[ti][:], op=ALU.add),
                         reads=["k_A2", "k_Tc%d" % ti], writes=["k_Tc%d" % tn])
                    qi, ti = qn, tn
                Tf, Tfk = Tc[ti], "k_Tc%d" % ti
                sk = "k_ST%d" % d
                for h in range(8):
                    P.op("pe", lambda e, h=h, d=d, Qt=Qt: e.matmul(Bp[0][0:PP, h, :], lhsT=Qt.L(h), rhs=ST[d].R(h), start=True, stop=False),
                         reads=[fk[2], sk], writes=["k_B0"])
                    P.op("pe", lambda e, h=h, V=V: e.matmul(Bp[0][0:PP, h, :], lhsT=Mk.L(h), rhs=V.R(h), start=False, stop=True),
                         reads=["k_Mk", tk[2]], writes=["k_B0"])
                P.op("act", lambda e: e.copy(out=W[:], in_=Bp[0][0:CH]), reads=["k_B0"], writes=["k_W"])
                mm8(Bp[0], "k_B0", Tf, W, [Tfk, "k_W"])
                P.op("act", lambda e: e.activation(out=Un[:], in_=Bp[0][0:CH], func=AF.Copy, scale=-1.0), reads=["k_B0"], writes=["k_Un"])
                for h in range(8):
                    P.op("pe", lambda e, h=h, Kh=Kh, V=V: e.matmul(Bp[1][0:PP, h, :], lhsT=Kh.L(h), rhs=V.R(h), start=True, stop=False),
                         reads=[tk[0], tk[2]], writes=["k_B1"])
                    P.op("pe", lambda e, h=h, Bh=Bh: e.matmul(Bp[1][0:PP, h, :], lhsT=Bh.L(h), rhs=Un.R(h), start=False, stop=True),
                         reads=[tk[1], "k_Un"], writes=["k_B1"])
                for h in range(8):
                    P.op("pe", lambda e, h=h, d=d, Rt=Rt: e.matmul(Bp[2][0:PP, h, :], lhsT=Rt.L(h), rhs=ST[d].R(h), start=True, stop=False),
                         reads=[fk[3], sk], writes=["k_B2"])
                    P.op("pe", lambda e, h=h, V=V: e.matmul(Bp[2][0:PP, h, :], lhsT=Ak.L(h), rhs=V.R(h), start=False, stop=False),
                         reads=["k_Ak", tk[2]], writes=["k_B2"])
                    P.op("pe", lambda e, h=h: e.matmul(Bp[2][0:PP, h, :], lhsT=Abn.L(h), rhs=Un.R(h), start=False, stop=True),
                         reads=["k_Abn", "k_Un"], writes=["k_B2"])
                yi = (step * 2 + d) % 2
                P.op("act", lambda e, yi=yi: e.copy(out=Y[yi][:], in_=Bp[2][0:CH].rearrange("p h i -> p (h i)")), reads=["k_B2"], writes=["k_Y%d" % yi])
                P.dma("sp", lambda e, yi=yi, d=d, cs=cs: e.dma_start(out=K.Sc["ry"][d][cs, :], in_=Y[yi][:]), reads=["k_Y%d" % yi], writes=["r_y"])
                P.op("dve", lambda e, d=d: e.tensor_tensor(out=tmpS[:], in0=ST[d][:], in1=gcs[d][:].unsqueeze(2).to_broadcast([CH, 8, CH]), op=ALU.mult),
                     reads=[sk, "k_gc%d" % d], writes=["k_tmpS"])
                P.op("dve", lambda e, d=d: e.tensor_tensor(out=ST[d][:], in0=tmpS[:], in1=Bp[1][0:CH], op=ALU.add), reads=["k_tmpS", "k_B1"], writes=[sk])


def phase_rwkv_post(K, l):
    P, I, S = K.P, K.I, K.S
    TT = min(512, S)
    with Phase(K) as ph:
        eps_t = ph.sb("q_eps", [128, 1])
        P.op("dve", lambda e: e.memset(eps_t[:], GN_EPS), writes=["q_eps"])
        lnp = ph.sb("q_lnp", [128, 4, 2])
        for ci, name in enumerate(("rwkv_ln_w", "rwkv_ln_b")):
            for ct in range(4):
                P.dma("sp", lambda e, ci=ci, ct=ct, name=name: e.dma_start(out=lnp[:, ct, ci:ci + 1],
                                                                         in_=I[name][l:l + 1, ct * 128:(ct + 1) * 128].rearrange("o p -> p o")), writes=["q_lnp"])
        y0 = [ph.sb("q_y0%d" % i, [128, 512]) for i in range(2)]
        y1 = [ph.sb("q_y1%d" % i, [128, 512]) for i in range(2)]
        sq = ph.sb("q_sq", [128, 512])
        mean = ph.sb("q_mean", [128, 8])
        var = ph.sb("q_var", [128, 8])
        bon = [ph.sb("q_bon%d" % i, [128, TT]) for i in range(2)]
        gg = [ph.sb("q_g%d" % i, [128, TT]) for i in range(2)]
        o1 = [ph.sb("q_o1%d" % i, [128, TT]) for i in range(2)]
        ob = [ph.sb("q_ob%d" % i, [128, TT], BF16) for i in range(2)]
        pT = [ph.ps("q_pT%d" % i, [128, 512]) for i in range(4)]
        for st in range(S // TT):
            t0 = st * TT
            for tb in range(TT // 128):
                b = tb % 2
                r0 = t0 + tb * 128
                P.dma("sp", lambda e, b=b, r0=r0: e.dma_start(out=y0[b][:], in_=K.Sc["ry"][0][r0:r0 + 128, :]), reads=["r_y"], writes=["q_y0%d" % b])
                P.dma("sp", lambda e, b=b, r0=r0: e.dma_start(out=y1[b][:], in_=K.Sc["ry"][1][r0:r0 + 128, :]), reads=["r_y"], writes=["q_y1%d" % b])
                yk = "q_y0%d" % b
                y3 = y0[b][:].rearrange("p (h i) -> p h i", h=8)
                P.op("pool", lambda e, b=b: e.tensor_tensor(out=y0[b][:], in0=y0[b][:], in1=y1[b][:], op=ALU.add), reads=[yk, "q_y1%d" % b], writes=[yk])
                P.op("dve", lambda e, y3=y3: e.tensor_reduce(out=mean[:], in_=y3, axis=AX.X, op=ALU.add), reads=[yk], writes=["q_mean"])
                P.op("dve", lambda e: e.tensor_scalar(out=mean[:], in0=mean[:], scalar1=1.0 / 64, scalar2=None, op0=ALU.mult), reads=["q_mean"], writes=["q_mean"])
                P.op("dve", lambda e, y3=y3: e.tensor_tensor(out=y3, in0=y3, in1=mean[:].unsqueeze(2).to_broadcast([128, 8, 64]), op=ALU.subtract),
                     reads=[yk, "q_mean"], writes=[yk])
                P.op("pool", lambda e, b=b: e.tensor_tensor(out=sq[:], in0=y0[b][:], in1=y0[b][:], op=ALU.mult), reads=[yk], writes=["q_sq"])
                P.op("dve", lambda e: e.tensor_reduce(out=var[:], in_=sq[:].rearrange("p (h i) -> p h i", h=8), axis=AX.X, op=ALU.add), reads=["q_sq"], writes=["q_var"])
                P.op("act", lambda e: e.activation(out=var[:], in_=var[:], func=AF.Sqrt, scale=1.0 / 64, bias=eps_t[:, 0:1]), reads=["q_var", "q_eps"], writes=["q_var"])
                P.op("dve", lambda e: e.reciprocal(out=var[:], in_=var[:]), reads=["q_var"], writes=["q_var"])
                P.op("dve", lambda e, y3=y3: e.tensor_tensor(out=y3, in0=y3, in1=var[:].unsqueeze(2).to_broadcast([128, 8, 64]), op=ALU.mult),
                     reads=[yk, "q_var"], writes=[yk])
                for ct in range(4):
                    P.op("pe", lambda e, ct=ct, b=b, tb=tb: e.transpose(pT[ct][:, tb * 128:(tb + 1) * 128], y0[b][:, ct * 128:(ct + 1) * 128], K.ident[:]),
                         reads=[yk, "c_ident"], writes=["q_pT%d" % ct])
            for ct in range(4):
                b = ct % 2
                cs = slice(ct * 128, (ct + 1) * 128)
                P.dma("sp", lambda e, b=b, cs=cs, t0=t0: e.dma_start(out=bon[b][:], in_=K.Sc["rbon"][cs, t0:t0 + TT]), reads=["r_scr"], writes=["q_bon%d" % b])
                P.dma("sp", lambda e, b=b, cs=cs, t0=t0: e.dma_start(out=gg[b][:], in_=K.Sc["rg"][cs, t0:t0 + TT]), reads=["r_scr"], writes=["q_g%d" % b])
                P.op("dve", lambda e, b=b, ct=ct: e.tensor_scalar(out=o1[b][:], in0=pT[ct][:, 0:TT], scalar1=lnp[:, ct, 0:1], scalar2=lnp[:, ct, 1:2],
                                                                op0=ALU.mult, op1=ALU.add), reads=["q_pT%d" % ct, "q_lnp"], writes=["q_o1%d" % b])
                P.op("pool", lambda e, b=b: e.tensor_tensor(out=o1[b][:], in0=o1[b][:], in1=bon[b][:], op=ALU.add), reads=["q_o1%d" % b, "q_bon%d" % b], writes=["q_o1%d" % b])
                P.op("dve", lambda e, b=b: e.tensor_tensor(out=ob[b][:], in0=o1[b][:], in1=gg[b][:], op=ALU.mult), reads=["q_o1%d" % b, "q_g%d" % b], writes=["q_ob%d" % b])
                P.dma("sp", lambda e, b=b, ct=ct, t0=t0: e.dma_start(out=K.Sc["br"][1024 + ct * 128:1024 + (ct + 1) * 128, t0:t0 + TT], in_=ob[b][:]),
                      reads=["q_ob%d" % b], writes=["br"])


def rope_tables(S):
    t = np.arange(S)
    row = (t // 64).astype(np.float32)
    col = (t % 64).astype(np.float32)
    outs = []
    for n_pairs in (32, 16):
        n_axis = n_pairs // 2
        freqs = np.power(np.float32(10000.0), -np.arange(n_axis, dtype=np.float32) / n_axis).astype(np.float32)
        ang = np.concatenate([row[:, None] * freqs, col[:, None] * freqs], axis=-1).astype(np.float32)
        outs.append(np.concatenate([np.cos(ang), np.sin(ang)], axis=-1).astype(np.float32))
    return outs[0], outs[1]


_NC_CACHE = {}


def kernel(**inputs):
    x = np.ascontiguousarray(np.asarray(inputs["x"], dtype=np.float32))
    B, S, _ = x.shape
    L = int(np.asarray(inputs["w_in"]).shape[0])
    key = (S, L)
    if key not in _NC_CACHE:
        _NC_CACHE[key] = build(Cfg(S=S, L=L))
    nc = _NC_CACHE[key]
    f32 = lambda a: np.ascontiguousarray(np.asarray(a, dtype=np.float32))
    shared = {}
    for name in ("w_mod", "norm1_g", "norm2_g", "w_in", "conv_w", "gqa_q_norm", "gqa_k_norm", "rwkv_mu", "rwkv_w0", "rwkv_a0",
                 "rwkv_g2", "rwkv_k_k", "rwkv_k_a", "rwkv_ln_w", "rwkv_ln_b", "mla_qc_norm", "mla_kvc_norm", "mla_w_uq",
                 "mla_w_ukv", "mla_q_norm", "mla_k_norm", "w_out", "w_router", "moe_w1", "moe_w3", "moe_w2"):
        shared[name] = f32(inputs[name])
    shared["mod_table"] = f32(inputs["mod_table"]).reshape(L, 6 * D)
    shared["rwkv_w2"] = f32(inputs["rwkv_w2"]).reshape(L, 128, 512)
    shared["rwkv_a2"] = f32(inputs["rwkv_a2"]).reshape(L, 128, 512)
    shared["rwkv_r_k"] = f32(inputs["rwkv_r_k"]).reshape(L, 512)
    shared["w_branch"] = f32(inputs["w_branch"]).reshape(L, 4 * 512, D)
    rg, rm = rope_tables(S)
    shared["rope_g"], shared["rope_m"] = rg, rm
    c = f32(inputs["c"])
    in_maps = []
    for b in range(B):
        m = dict(shared)
        m["x"] = x[b]
        m["c"] = c[b]
        in_maps.append(m)
    res = run_bass_kernel_spmd(nc, in_maps, core_ids=list(range(B)))
    return np.stack([np.asarray(r["out"], dtype=np.float32) for r in res.results], axis=0)
```

```python
import contextlib
import numpy as np
import concourse.bass as bass
import concourse.mybir as mybir
from concourse.bass_utils import run_bass_kernel_spmd

F32 = mybir.dt.float32
BF16 = mybir.dt.bfloat16
ALU = mybir.AluOpType
AF = mybir.ActivationFunctionType
AX = mybir.AxisListType

D = 2048
NKC = D // 128
NORM_EPS = 1e-6
GN_EPS = 64e-5
IN_WIDTH = 13088
C_CONV, C_GQA, C_RWKV, C_MLA, C_GATE = 0, 1536, 2304, 4224, 4896
NE = 16
EH = 1024
CH = 64

NDMA_SEMS = 12


class Prog:
    ENGS = ("pe", "act", "dve", "pool", "sp")

    def __init__(self, nc):
        self.nc = nc
        self.ops = {e: [] for e in self.ENGS}
        self.cnt = {e: 0 for e in self.ENGS}
        self.seen = {e: {} for e in self.ENGS}
        self.state = {}
        self.dma_n = {e: 0 for e in self.ENGS}
        self.dma_exp = {}
        self.pending = {e: [] for e in self.ENGS}
        self.nops = 0

    def _deps(self, reads, writes):
        toks = []
        for k in reads:
            st = self.state.get(k)
            if st and st[0] is not None:
                toks.append(st[0])
        for k in writes:
            st = self.state.get(k)
            if st:
                if st[0] is not None:
                    toks.append(st[0])
                toks.extend(st[1])
        return toks

    def _commit(self, tok, reads, writes):
        for k in reads:
            st = self.state.setdefault(k, [None, []])
            st[1].append(tok)
            if len(st[1]) > 64:
                st[1] = st[1][-64:] if False else self._compress(st[1])
        for k in writes:
            self.state[k] = [tok, []]

    @staticmethod
    def _compress(toks):
        best = {}
        for t in toks:
            sid = t[:3]
            if sid not in best or best[sid][3] < t[3]:
                best[sid] = t
        return list(best.values())

    def _waits(self, eng, toks, same_engine_ok=False):
        need = {}
        for t in toks:
            kind, e, idx, val = t
            if kind == "c" and e == eng and same_engine_ok:
                continue
            sid = (kind, e, idx)
            if need.get(sid, 0) < val:
                need[sid] = val
        out = []
        for sid, val in need.items():
            if self.seen[eng].get(sid, 0) >= val:
                continue
            self.seen[eng][sid] = val
            out.append((sid, val))
        return out

    def op(self, eng, fn, reads=(), writes=()):
        toks = self._deps(reads, writes) + self.pending[eng]
        self.pending[eng] = []
        waits = self._waits(eng, toks, same_engine_ok=(eng == "pe"))
        self.cnt[eng] += 1
        tok = ("c", eng, 0, self.cnt[eng])
        self.ops[eng].append((waits, fn, ("c", eng, 0)))
        self._commit(tok, reads, writes)
        self.nops += 1

    def dma(self, eng, fn, reads=(), writes=()):
        toks = self._deps(reads, writes) + self.pending[eng]
        self.pending[eng] = []
        i = self.dma_n[eng]
        self.dma_n[eng] += 1
        slot = i % NDMA_SEMS
        gen = i // NDMA_SEMS
        if gen > 0:
            toks = toks + [("d", eng, slot, 16 * gen)]
        waits = self._waits(eng, toks)
        tok = ("d", eng, slot, 16 * (gen + 1))
        self.dma_exp[(eng, slot)] = 16 * (gen + 1)
        self.ops[eng].append((waits, fn, ("d", eng, slot)))
        self._commit(tok, reads, writes)
        self.nops += 1

    def barrier(self):
        toks = [("c", e, 0, self.cnt[e]) for e in self.ENGS if self.cnt[e] > 0]
        toks += [("d", e, s, v) for (e, s), v in self.dma_exp.items()]
        for e in self.ENGS:
            self.pending[e] = list(toks)
        self.state = {}

    def emit(self):
        nc = self.nc
        self.barrier()
        fin_waits = self._waits("sp", self.pending["sp"])
        with contextlib.ExitStack() as es:
            sems = {}
            for e in self.ENGS:
                sems[("c", e, 0)] = es.enter_context(nc.semaphore("c_" + e))
            for e in ("sp", "act", "pool"):
                for s in range(NDMA_SEMS):
                    sems[("d", e, s)] = es.enter_context(nc.semaphore("d_%s_%d" % (e, s)))
            block = es.enter_context(nc.Block())

            def run(engname, engobj):
                for waits, fn, inc in self.ops[engname]:
                    for sid, val in waits:
                        engobj.wait_ge(sems[sid], val)
                    ins = fn(engobj)
                    ins.then_inc(sems[inc], 16 if inc[0] == "d" else 1)
                if engname == "sp":
                    for sid, val in fin_waits:
                        engobj.wait_ge(sems[sid], val)

            @block.sync
            def _(e):
                run("sp", e)

            @block.tensor
            def _(e):
                run("pe", e)

            @block.scalar
            def _(e):
                run("act", e)

            @block.vector
            def _(e):
                run("dve", e)

            @block.gpsimd
            def _(e):
                run("pool", e)


class Cfg:
    def __init__(self, S=4096, L=4, phases=None, ext=()):
        self.S = S
        self.L = L
        self.phases = phases
        self.ext = set(ext)
        self.cap = 2 * S // NE


class Ctx:
    pass


def build(cfg):
    nc = bass.Bass("TRN2", target_bir_lowering=False)
    K = Ctx()
    K.nc, K.cfg = nc, cfg
    K.P = Prog(nc)
    S, L = cfg.S, cfg.L
    K.S, K.L = S, L

    def din(name, shape, dt=F32):
        return nc.dram_tensor(name, list(shape), dt, kind="ExternalInput").ap()

    def dscr(name, shape, dt=F32):
        kind = "Internal"
        if name in cfg.ext:
            kind = "ExternalOutput"
        if ("in:" + name) in cfg.ext:
            kind = "ExternalInput"
        return nc.dram_tensor(name, list(shape), dt, kind=kind).ap()

    shapes = {
        "x": [S, D], "c": [D], "w_mod": [D, 6 * D], "mod_table": [L, 6 * D], "norm1_g": [L, D], "norm2_g": [L, D],
        "w_in": [L, D, IN_WIDTH], "conv_w": [L, 3, 512], "gqa_q_norm": [L, 64], "gqa_k_norm": [L, 64],
        "rwkv_mu": [L, 2, 1920], "rwkv_w0": [L, 2, 512], "rwkv_w2": [L, 128, 512], "rwkv_a0": [L, 2, 512],
        "rwkv_a2": [L, 128, 512], "rwkv_g2": [L, 128, 512], "rwkv_k_k": [L, 512], "rwkv_k_a": [L, 512],
        "rwkv_r_k": [L, 512], "rwkv_ln_w": [L, 512], "rwkv_ln_b": [L, 512], "mla_qc_norm": [L, 384],
        "mla_kvc_norm": [L, 256], "mla_w_uq": [L, 384, 768], "mla_w_ukv": [L, 256, 1024], "mla_q_norm": [L, 96],
        "mla_k_norm": [L, 96], "w_branch": [L, 4 * 512, D], "w_out": [L, D, D], "w_router": [L, D, NE],
        "moe_w1": [L, NE, D, EH], "moe_w3": [L, NE, D, EH], "moe_w2": [L, NE, EH, D],
        "rope_g": [S, 64], "rope_m": [S, 32],
    }

    class LazyIn(dict):
        def __missing__(self, name):
            v = din(name, shapes[name])
            self[name] = v
            return v
    I = LazyIn()
    K.I = I
    K.out = nc.dram_tensor("out", [S, D], F32, kind="ExternalOutput").ap()

    Sc = {}
    Sc["modv"] = dscr("modv", [L * 6, D])
    Sc["fm"] = dscr("fm", [1536 + 1920, S])
    Sc["gt"] = dscr("gt", [4 * D, S], BF16)
    Sc["tm"] = dscr("tm", [S, 768 + 672])
    Sc["br"] = dscr("br", [4 * 512, S], BF16)
    Sc["gqT"] = dscr("gqT", [640, S], BF16)
    Sc["gv"] = dscr("gv", [S, 2, 65], BF16)
    Sc["mT"] = dscr("mT", [D, S], BF16)
    Sc["h2"] = dscr("h2", [S, D], BF16)
    Sc["aff"] = dscr("aff", [S, NE])
    Sc["affT"] = dscr("affT", [NE, S])
    Sc["rank"] = dscr("rank", [S, NE])
    Sc["rankT"] = dscr("rankT", [NE, S])
    Sc["ybuf"] = dscr("ybuf", [NE, S // 8, D], BF16)
    Sc["rg"] = dscr("rg", [512, S])
    Sc["rbon"] = dscr("rbon", [512, S])
    Sc["rv"] = dscr("rv", [S, 512])
    Sc["rgc"] = [dscr("rgc%d" % d_, [512, S // CH]) for d_ in range(2)]
    Sc["rf"] = [[dscr("rf%d_%d" % (d_, i_), [512, S]) for i_ in range(4)] for d_ in range(2)]
    Sc["rt"] = [[dscr("rt%d_%d" % (d_, i_), [S, 512]) for i_ in range(2)] for d_ in range(2)]
    Sc["ry"] = [dscr("ry%d" % d_, [S, 512]) for d_ in range(2)]
    Sc["mqT"] = dscr("mqT", [8 * 96, S], BF16)
    Sc["mkT"] = dscr("mkT", [8 * 96, S], BF16)
    Sc["mv"] = dscr("mv", [S, 8, 65], BF16)
    K.Sc = Sc
    K.dscr = dscr

    K.xres = K.out
    with contextlib.ExitStack() as glob:
        K.glob = glob
        setup_consts(K)
        ph = cfg.phases
        if ph is None or "copyx" in ph:
            xin = K.I["x"]
            for t in range(0, S, 512):
                n = min(512, S - t)
                K.P.dma("sp", lambda e, t=t, n=n: e.dma_start(out=K.out[t:t + n, :], in_=xin[t:t + n, :]), writes=["xres"])
            K.P.barrier()
        if ph is None or "p0" in ph:
            phase0(K)
        for l in range(L):
            if ph is None or "p1" in ph:
                phase1(K, l)
            if ph is None or "conv" in ph:
                phase_conv(K, l)
            if ph is None or "gqa" in ph:
                phase_gqa_prep(K, l)
            if ph is None or "gqa" in ph or "gqa_attn" in ph:
                phase_gqa_attn(K, l)
            if ph is None or "mla" in ph or "mla_prep" in ph:
                phase_mla_prep(K, l)
            if ph is None or "mla" in ph or "mla_attn" in ph:
                phase_mla_attn(K, l)
            if ph is None or "rwkv" in ph or "rwkv_prep" in ph:
                phase_rwkv_prep(K, l)
            if ph is None or "rwkv" in ph or "rwkv_scan" in ph:
                phase_rwkv_scan(K, l)
            if ph is None or "rwkv" in ph or "rwkv_post" in ph:
                phase_rwkv_post(K, l)
            if ph is None or "p3a" in ph:
                phase3a(K, l)
            if ph is None or "p3b" in ph:
                phase3b(K, l)
            if ph is None or "moe" in ph:
                phase_moe(K, l)
        K.P.emit()
    return nc


class Phase:
    def __init__(self, K):
        self.K = K
        self.es = contextlib.ExitStack()

    def __enter__(self):
        self.es.__enter__()
        return self

    def __exit__(self, *a):
        self.K.P.barrier()
        return self.es.__exit__(*a)

    _uid = [0]

    def sb(self, name, shape, dt=F32):
        Phase._uid[0] += 1
        return self.es.enter_context(self.K.nc.sbuf_tensor("%s_u%d" % (name, Phase._uid[0]), list(shape), dt))

    def ps(self, name, shape, dt=F32):
        Phase._uid[0] += 1
        return self.es.enter_context(self.K.nc.psum_tensor("%s_u%d" % (name, Phase._uid[0]), list(shape), dt))


def setup_consts(K):
    nc, P = K.nc, K.P
    g = K.glob

    def sb(name, shape, dt=F32):
        return g.enter_context(nc.sbuf_tensor(name, list(shape), dt))
    io = sb("c_io", [128, 512])
    iop = sb("c_iop", [128, 1])
    K.ident = sb("c_ident", [128, 128])
    K.identb = sb("c_identb", [128, 128], BF16)
    P.op("pool", lambda e: e.iota(io[:], pattern=[[1, 512]], base=0, channel_multiplier=0,
                                  allow_small_or_imprecise_dtypes=True), writes=["c_io"])
    P.op("pool", lambda e: e.iota(iop[:], pattern=[[0, 1]], base=0, channel_multiplier=1,
                                  allow_small_or_imprecise_dtypes=True), writes=["c_iop"])
    P.op("dve", lambda e: e.tensor_scalar(out=K.ident[:], in0=io[:, 0:128], scalar1=iop[:, 0:1], scalar2=None,
                                          op0=ALU.is_equal), reads=["c_io", "c_iop"], writes=["c_ident"])
    P.op("dve", lambda e: e.tensor_copy(out=K.identb[:], in_=K.ident[:]), reads=["c_ident"], writes=["c_identb"])
    K.io, K.iop = io, iop


def phase0(K):
    nc, P, I, L = K.nc, K.P, K.I, K.L
    with Phase(K) as ph:
        cT = ph.sb("cT", [128, NKC])
        sc = ph.sb("sc", [128, NKC])
        modrow = ph.sb("modrow", [1, 6 * D])
        P.dma("sp", lambda e: e.dma_start(out=cT[:], in_=I["c"].rearrange("(k p) -> p k", p=128),
                                          allow_slow_non_contiguous=True), writes=["cT"])
        P.op("act", lambda e: e.activation(out=sc[:], in_=cT[:], func=AF.Silu), reads=["cT"], writes=["sc"])
        wm = [ph.sb("wm%d" % i, [128, NKC, 512]) for i in range(2)]
        pm = [ph.ps("pm%d" % i, [1, 512]) for i in range(2)]
        wv = I["w_mod"].rearrange("(k p) n -> p k n", p=128)
        for ci in range(6 * D // 512):
            b = ci % 2
            P.dma("sp", lambda e, b=b, ci=ci: e.dma_start(out=wm[b][:], in_=wv[:, :, ci * 512:(ci + 1) * 512]),
                  writes=["wm%d" % b])
            for kc in range(NKC):
                P.op("pe", lambda e, b=b, kc=kc: e.matmul(pm[b][:], lhsT=sc[:, kc:kc + 1], rhs=wm[b][:, kc, :],
                                                          start=(kc == 0), stop=(kc == NKC - 1)),
                     reads=["sc", "wm%d" % b], writes=["pm%d" % b])
            P.op("act", lambda e, b=b, ci=ci: e.copy(out=modrow[:, ci * 512:(ci + 1) * 512], in_=pm[b][:]),
                 reads=["pm%d" % b], writes=["modrow"])
        tb = ph.sb("tb", [1, 6 * D])
        g12 = ph.sb("g12", [1, 2 * D])
        for l in range(L):
            P.dma("sp", lambda e, l=l: e.dma_start(out=tb[:], in_=I["mod_table"][l:l + 1, :]), writes=["tb"])
            P.dma("sp", lambda e, l=l: e.dma_start(out=g12[:, 0:D], in_=I["norm1_g"][l:l + 1, :]), writes=["g1"])
            P.dma("sp", lambda e, l=l: e.dma_start(out=g12[:, D:2 * D], in_=I["norm2_g"][l:l + 1, :]), writes=["g2"])
            P.op("dve", lambda e: e.tensor_tensor(out=tb[:], in0=tb[:], in1=modrow[:], op=ALU.add),
                 reads=["tb", "modrow"], writes=["tb"])
            for sub in range(2):
                o = sub * 3 * D
                P.op("dve", lambda e, o=o, sub=sub: e.scalar_tensor_tensor(
                    out=tb[:, o + D:o + 2 * D], in0=tb[:, o + D:o + 2 * D], scalar=1.0, in1=g12[:, sub * D:(sub + 1) * D],
                    op0=ALU.add, op1=ALU.mult), reads=["tb", "g1", "g2"], writes=["tb"])
            P.dma("sp", lambda e, l=l: e.dma_start(out=K.Sc["modv"][l * 6:(l + 1) * 6, :].rearrange("a d -> (a d)").unsqueeze(0),
                                                   in_=tb[:]), reads=["tb"], writes=["modv"])


def rms_rstd(P, ph, eng_sq, x_ap, junk_ap, ss, rstd, n, eps, keys_r, tag):
    P.op("act", lambda e: e.activation(out=junk_ap, in_=x_ap, func=AF.Square, accum_out=ss[:, 0:1]),
         reads=keys_r, writes=[tag + "_junk", tag + "_ss"])
    P.op("act", lambda e: e.activation(out=rstd[:, 0:1], in_=ss[:, 0:1], func=AF.Sqrt, scale=1.0 / n, bias=K_EPS[eps][:, 0:1]),
         reads=[tag + "_ss"], writes=[tag + "_rstd"])
    P.op("dve", lambda e: e.reciprocal(out=rstd[:, 0:1], in_=rstd[:, 0:1]), reads=[tag + "_rstd"], writes=[tag + "_rstd"])


K_EPS = {}

IN_CHUNKS = []


def _mk_chunks():
    out = []
    for c0 in range(C_CONV, C_GQA, 512):
        out.append((c0, 512, "fm", c0 - C_CONV))
    out.append((C_GQA, 512, "tm", 0))
    out.append((C_GQA + 512, 256, "tm", 512))
    c0 = C_RWKV
    while c0 < C_MLA:
        n = min(512, C_MLA - c0)
        out.append((c0, n, "fm", 1536 + c0 - C_RWKV))
        c0 += n
    out.append((C_MLA, 512, "tm", 768))
    out.append((C_MLA + 512, 160, "tm", 768 + 512))
    for c0 in range(C_GATE, IN_WIDTH, 512):
        out.append((c0, 512, "gt", c0 - C_GATE))
    return out


IN_CHUNKS = _mk_chunks()


def load_bcast_row(K, ph, name, row_ap, n):
    t = ph.sb(name, [128, n])
    K.P.dma("sp", lambda e: e.dma_start(out=t[:], in_=row_ap.to_broadcast([128, n])), reads=["modv"], writes=[name])
    return t


def norm_to_hT(K, ph, l, sub, x_dram, tok0, ntok, hT, A_b, B_b, bufs, h_keep=None):
    P = K.P
    xt, hb, junk, ss, rstd, pt = bufs
    for tt in range(ntok // 128):
        t0 = tok0 + tt * 128
        b = tt % 2
        xk = "xt%d" % (b if xt[0] is not xt[1] else 0)
        P.dma("sp", lambda e, b=b, t0=t0: e.dma_start(out=xt[b][:], in_=x_dram[t0:t0 + 128, :]),
              reads=["xdram"], writes=[xk])
        jk = junk if junk is not None else hb[b]
        P.op("act", lambda e, b=b, jk=jk: e.activation(out=jk[:], in_=xt[b][:], func=AF.Square, accum_out=ss[:, 0:1]),
             reads=[xk], writes=["junk" if junk is not None else "hb%d" % b, "ss"])
        P.op("act", lambda e: e.activation(out=rstd[:, 0:1], in_=ss[:, 0:1], func=AF.Sqrt, scale=1.0 / D,
                                           bias=K.eps_norm[:, 0:1]), reads=["ss"], writes=["rstd"])
        P.op("dve", lambda e: e.reciprocal(out=rstd[:, 0:1], in_=rstd[:, 0:1]), reads=["rstd"], writes=["rstd"])
        P.op("dve", lambda e, b=b: e.scalar_tensor_tensor(out=xt[b][:], in0=xt[b][:], scalar=rstd[:, 0:1], in1=A_b[:],
                                                          op0=ALU.mult, op1=ALU.mult),
             reads=[xk, "rstd", "A_b"], writes=[xk])
        P.op("pool", lambda e, b=b: e.tensor_tensor(out=hb[b][:], in0=xt[b][:], in1=B_b[:], op=ALU.add),
             reads=[xk, "B_b"], writes=["hb%d" % b])
        if h_keep is not None:
            h_keep(tt, t0, xt[b], hb[b], xk, "hb%d" % b)
        for g4 in range(NKC // 4):
            pb = g4 % 2
            for j in range(4):
                kc = g4 * 4 + j
                P.op("pe", lambda e, b=b, pb=pb, j=j, kc=kc: e.transpose(pt[pb][:, j * 128:(j + 1) * 128],
                                                                         hb[b][:, kc * 128:(kc + 1) * 128], K.identb[:]),
                     reads=["hb%d" % b, "c_identb"], writes=["pt%d" % pb])
            P.op("act", lambda e, pb=pb, g4=g4, tt=tt: e.copy(
                out=hT[:, g4 * 4:(g4 + 1) * 4, tt * 128:(tt + 1) * 128],
                in_=pt[pb][:].rearrange("p (j t) -> p j t", j=4)),
                reads=["pt%d" % pb], writes=["hT"])


def phase1(K, l):
    nc, P, I, S = K.nc, K.P, K.I, K.S
    TS = min(1024, S)
    NT = min(512, TS)
    xd = K.out if (l > 0 or K.cfg.phases is None or "copyx" in K.cfg.phases) else I["x"]
    with Phase(K) as ph:
        K.eps_norm = ph.sb("eps_norm", [128, 1])
        P.op("dve", lambda e: e.memset(K.eps_norm[:], NORM_EPS), writes=["eps_norm"])
        A_b = load_bcast_row(K, ph, "A_b", K.Sc["modv"][l * 6 + 1:l * 6 + 2, :], D)
        B_b = load_bcast_row(K, ph, "B_b", K.Sc["modv"][l * 6 + 0:l * 6 + 1, :], D)
        hT = ph.sb("hT", [128, NKC, TS], BF16)
        xt1 = ph.sb("xt0", [128, D])
        xt = [xt1, xt1]
        hb = [ph.sb("hb%d" % i, [128, D], BF16) for i in range(2)]
        junk = None
        ss = ph.sb("ss", [128, 1])
        rstd = ph.sb("rstd", [128, 1])
        pt = [ph.ps("pt%d" % i, [128, 512], BF16) for i in range(2)]
        wf = [ph.sb("wf%d" % i, [128, NKC, 512]) for i in range(3)]
        wb = [ph.sb("wb%d" % i, [128, NKC, 512], BF16) for i in range(2)]
        stg = [ph.sb("stg%d" % i, [128, 512]) for i in range(4)]
        stgb = [ph.sb("stgb%d" % i, [128, 512], BF16) for i in range(2)]
        pm = [ph.ps("pm%d" % i, [128, 512]) for i in range(4)]
        wv = I["w_in"][l].rearrange("(k p) n -> p k n", p=128)
        nmm = 0
        work = [(st, ci) for st in range(S // TS) for ci in range(len(IN_CHUNKS))]

        def issue_dma(wi):
            c0, ncol, _, _ = IN_CHUNKS[work[wi][1]]
            f = wi % 3
            P.dma("sp", lambda e, f=f, c0=c0, ncol=ncol: e.dma_start(out=wf[f][:, :, 0:ncol], in_=wv[:, :, c0:c0 + ncol]),
                  writes=["wf%d" % f])

        def issue_cast(wi):
            c0, ncol, _, _ = IN_CHUNKS[work[wi][1]]
            f, b = wi % 3, wi % 2
            P.op("pool", lambda e, b=b, f=f, ncol=ncol: e.tensor_copy(out=wb[b][:, :, 0:ncol], in_=wf[f][:, :, 0:ncol]),
                 reads=["wf%d" % f], writes=["wb%d" % b])

        issue_dma(0)
        if len(work) > 1:
            issue_dma(1)
        issue_cast(0)
        for wi, (st, ci) in enumerate(work):
            tok0 = st * TS
            if ci == 0:
                norm_to_hT(K, ph, l, 0, xd, tok0, TS, hT, A_b, B_b, (xt, hb, junk, ss, rstd, pt))
            if wi + 2 < len(work):
                issue_dma(wi + 2)
            if wi + 1 < len(work):
                issue_cast(wi + 1)
            if True:
                (c0, ncol, kind, doff) = IN_CHUNKS[ci]
                b = wi % 2
                if kind in ("fm", "gt"):
                    for j in range(ncol // 128):
                        for th in range(TS // NT):
                            pi = nmm % 4
                            nmm += 1
                            for kc in range(NKC):
                                P.op("pe", lambda e, pi=pi, b=b, kc=kc, j=j, th=th: e.matmul(
                                    pm[pi][:, 0:NT], lhsT=wb[b][:, kc, j * 128:(j + 1) * 128], rhs=hT[:, kc, th * NT:(th + 1) * NT],
                                    start=(kc == 0), stop=(kc == NKC - 1)),
                                    reads=["wb%d" % b, "hT"], writes=["pm%d" % pi])
                            r0 = doff + j * 128
                            t0 = tok0 + th * NT
                            if kind == "fm":
                                P.op("dve", lambda e, pi=pi: e.tensor_copy(out=stg[pi][:, 0:NT], in_=pm[pi][:, 0:NT]),
                                     reads=["pm%d" % pi], writes=["stg%d" % pi])
                                P.dma("sp", lambda e, pi=pi, r0=r0, t0=t0: e.dma_start(
                                    out=K.Sc["fm"][r0:r0 + 128, t0:t0 + NT], in_=stg[pi][:, 0:NT]),
                                    reads=["stg%d" % pi], writes=["fm"])
                            else:
                                sb_i = pi % 2
                                P.op("act", lambda e, pi=pi, sb_i=sb_i: e.activation(out=stgb[sb_i][:, 0:NT], in_=pm[pi][:, 0:NT], func=AF.Sigmoid),
                                     reads=["pm%d" % pi], writes=["stgb%d" % sb_i])
                                P.dma("sp", lambda e, sb_i=sb_i, r0=r0, t0=t0: e.dma_start(
                                    out=K.Sc["gt"][r0:r0 + 128, t0:t0 + NT], in_=stgb[sb_i][:, 0:NT]),
                                    reads=["stgb%d" % sb_i], writes=["gt"])
                else:
                    for tt in range(TS // 128):
                        pi = nmm % 4
                        nmm += 1
                        for kc in range(NKC):
                            P.op("pe", lambda e, pi=pi, b=b, kc=kc, tt=tt, ncol=ncol: e.matmul(
                                pm[pi][:, 0:ncol], lhsT=hT[:, kc, tt * 128:(tt + 1) * 128], rhs=wb[b][:, kc, 0:ncol],
                                start=(kc == 0), stop=(kc == NKC - 1)),
                                reads=["wb%d" % b, "hT"], writes=["pm%d" % pi])
                        t0 = tok0 + tt * 128
                        P.op("dve", lambda e, pi=pi, ncol=ncol: e.tensor_copy(out=stg[pi][:, 0:ncol], in_=pm[pi][:, 0:ncol]),
                             reads=["pm%d" % pi], writes=["stg%d" % pi])
                        P.dma("sp", lambda e, pi=pi, t0=t0, doff=doff, ncol=ncol: e.dma_start(
                            out=K.Sc["tm"][t0:t0 + 128, doff:doff + ncol], in_=stg[pi][:, 0:ncol]),
                            reads=["stg%d" % pi], writes=["tm"])


def phase_conv(K, l):
    P, I, S = K.P, K.I, K.S
    fm, br = K.Sc["fm"], K.Sc["br"]
    with Phase(K) as ph:
        cw = ph.sb("cw", [128, 4, 3])
        cwv = I["conv_w"][l]
        for ct in range(4):
            P.dma("sp", lambda e, ct=ct: e.dma_start(out=cw[:, ct, :], in_=cwv[:, ct * 128:(ct + 1) * 128].rearrange("k p -> p k"),
                                                     allow_slow_non_contiguous=True), writes=["cw"])
        bg = ph.sb("bg", [128, S])
        cg = ph.sb("cg", [128, S])
        zp = ph.sb("zp", [128, S + 2])
        y = ph.sb("y", [128, S])
        ob = ph.sb("ob", [128, S], BF16)
        for ct in range(4):
            k = "cv_"
            P.dma("sp", lambda e, ct=ct: e.dma_start(out=bg[:], in_=fm[ct * 128:(ct + 1) * 128, :]), reads=["fm"], writes=[k + "bg"])
            P.dma("sp", lambda e, ct=ct: e.dma_start(out=cg[:], in_=fm[512 + ct * 128:512 + (ct + 1) * 128, :]), reads=["fm"], writes=[k + "cg"])
            P.dma("sp", lambda e, ct=ct: e.dma_start(out=zp[:, 1:S + 1], in_=fm[1024 + ct * 128:1024 + (ct + 1) * 128, :]), reads=["fm"], writes=[k + "zp"])
            P.op("pool", lambda e: e.memset(zp[:, 0:1], 0.0), writes=[k + "z0"])
            P.op("pool", lambda e: e.memset(zp[:, S + 1:S + 2], 0.0), writes=[k + "z1"])
            P.op("dve", lambda e: e.tensor_tensor(out=zp[:, 1:S + 1], in0=zp[:, 1:S + 1], in1=cg[:], op=ALU.mult),
                 reads=[k + "zp", k + "cg"], writes=[k + "zp"])
            P.op("dve", lambda e, ct=ct: e.tensor_scalar(out=y[:], in0=zp[:, 0:S], scalar1=cw[:, ct, 0:1], scalar2=None, op0=ALU.mult),
                 reads=[k + "zp", k + "z0", "cw"], writes=[k + "y"])
            P.op("dve", lambda e, ct=ct: e.scalar_tensor_tensor(out=y[:], in0=zp[:, 1:S + 1], scalar=cw[:, ct, 1:2], in1=y[:],
                                                                op0=ALU.mult, op1=ALU.add), reads=[k + "zp", k + "y"], writes=[k + "y"])
            P.op("dve", lambda e, ct=ct: e.scalar_tensor_tensor(out=y[:], in0=zp[:, 2:S + 2], scalar=cw[:, ct, 2:3], in1=y[:],
                                                                op0=ALU.mult, op1=ALU.add), reads=[k + "zp", k + "z1", k + "y"], writes=[k + "y"])
            P.op("pool", lambda e: e.tensor_tensor(out=ob[:], in0=y[:], in1=bg[:], op=ALU.mult), reads=[k + "y", k + "bg"], writes=[k + "ob"])
            P.dma("sp", lambda e, ct=ct: e.dma_start(out=br[ct * 128:(ct + 1) * 128, :], in_=ob[:]), reads=[k + "ob"], writes=["br"])


def bcast_rows(K, ph, name, row_ap, n, reps):
    t = ph.sb(name, [128, reps, n])
    for r in range(reps):
        K.P.dma("sp", lambda e, r=r: e.dma_start(out=t[:, r, :], in_=row_ap.to_broadcast([128, n])), writes=[name + str(r)])
    return t, [name + str(r) for r in range(reps)]


def head_rms(P, x3, nh, hd, sq3, ssq, eps_t, keys_in, tag):
    P.op("dve", lambda e: e.tensor_tensor(out=sq3, in0=x3, in1=x3, op=ALU.mult), reads=keys_in, writes=[tag + "sq"])
    P.op("dve", lambda e: e.tensor_reduce(out=ssq, in_=sq3, axis=AX.X, op=ALU.add), reads=[tag + "sq"], writes=[tag + "ssq"])
    P.op("act", lambda e: e.activation(out=ssq, in_=ssq, func=AF.Sqrt, scale=1.0 / hd, bias=eps_t[:, 0:1]),
         reads=[tag + "ssq"], writes=[tag + "ssq"])
    P.op("dve", lambda e: e.reciprocal(out=ssq, in_=ssq), reads=[tag + "ssq"], writes=[tag + "ssq"])
    P.op("dve", lambda e: e.tensor_tensor(out=x3, in0=x3, in1=ssq.unsqueeze(2).to_broadcast([128, nh, hd]), op=ALU.mult),
         reads=keys_in + [tag + "ssq"], writes=keys_in)


def rope3(P, x1, x2, o1, o2, cs, sn, t1, t2, shape, keys_in, keys_out, tag):
    P.op("dve", lambda e: e.tensor_tensor(out=t1, in0=x1, in1=cs, op=ALU.mult), reads=keys_in, writes=[tag + "t1"])
    P.op("pool", lambda e: e.tensor_tensor(out=t2, in0=x2, in1=sn, op=ALU.mult), reads=keys_in, writes=[tag + "t2"])
    P.op("dve", lambda e: e.tensor_tensor(out=o1, in0=t1, in1=t2, op=ALU.subtract), reads=[tag + "t1", tag + "t2"], writes=[keys_out[0]])
    P.op("dve", lambda e: e.tensor_tensor(out=t1, in0=x1, in1=sn, op=ALU.mult), reads=keys_in + [keys_out[0]], writes=[tag + "t1"])
    P.op("pool", lambda e: e.tensor_tensor(out=t2, in0=x2, in1=cs, op=ALU.mult), reads=keys_in + [keys_out[0]], writes=[tag + "t2"])
    P.op("dve", lambda e: e.tensor_tensor(out=o2, in0=t1, in1=t2, op=ALU.add), reads=[tag + "t1", tag + "t2"], writes=[keys_out[1]])


def phase_gqa_prep(K, l):
    P, I, S = K.P, K.I, K.S
    tm = K.Sc["tm"]
    rope_g = I["rope_g"]
    with Phase(K) as ph:
        eps_t = ph.sb("eps_t", [128, 1])
        P.op("dve", lambda e: e.memset(eps_t[:], NORM_EPS), writes=["eps_t"])
        gb = ph.sb("gb", [128, 10, 64])
        gkeys = []
        for r in range(10):
            src = I["gqa_q_norm"][l:l + 1, :] if r < 8 else I["gqa_k_norm"][l:l + 1, :]
            P.dma("sp", lambda e, r=r, src=src: e.dma_start(out=gb[:, r, :], in_=src.to_broadcast([128, 64])), writes=["gb%d" % r])
            gkeys.append("gb%d" % r)
        for tt in range(S // 128):
            b = tt % 2
            t0 = tt * 128
            if tt < 2:
                K_ = {}
                K_["x"] = ph.sb("gx%d" % b, [128, 768])
                K_["rp"] = ph.sb("grp%d" % b, [128, 64])
                K_["sq"] = ph.sb("gsq%d" % b, [128, 640])
                K_["ssq"] = ph.sb("gssq%d" % b, [128, 10])
                K_["t1"] = ph.sb("gt1%d" % b, [128, 320])
                K_["t2"] = ph.sb("gt2%d" % b, [128, 320])
                K_["qk"] = ph.sb("gqk%d" % b, [128, 640], BF16)
                K_["v"] = ph.sb("gv%d" % b, [128, 2, 65], BF16)
                K_["pt"] = ph.ps("gpt%d" % b, [128, 1024], BF16)
                K_["qkT"] = ph.sb("gqkT%d" % b, [128, 640], BF16)
                if tt == 0:
                    bufs = [K_, None]
                else:
                    bufs[1] = K_
            B = bufs[b]
            k = "g%d_" % b
            x, rp = B["x"], B["rp"]
            P.dma("sp", lambda e, x=x, t0=t0: e.dma_start(out=x[:], in_=tm[t0:t0 + 128, 0:768]), reads=["tm"], writes=[k + "x"])
            P.dma("sp", lambda e, rp=rp, t0=t0: e.dma_start(out=rp[:], in_=rope_g[t0:t0 + 128, :]), writes=[k + "rp"])
            x3 = x[:, 0:640].rearrange("p (h d) -> p h d", h=10)
            head_rms(P, x3, 10, 64, B["sq"][:].rearrange("p (h d) -> p h d", h=10), B["ssq"][:], eps_t, [k + "x"], k)
            P.op("pool", lambda e, x3=x3: e.tensor_tensor(out=x3, in0=x3, in1=gb[:], op=ALU.mult), reads=[k + "x"] + gkeys, writes=[k + "x"])
            x4 = x[:, 0:640].rearrange("p (h two d) -> p h two d", h=10, two=2)
            qk4 = B["qk"][:].rearrange("p (h two d) -> p h two d", h=10, two=2)
            cs = rp[:, 0:32].unsqueeze(1).to_broadcast([128, 10, 32])
            sn = rp[:, 32:64].unsqueeze(1).to_broadcast([128, 10, 32])
            t1 = B["t1"][:].rearrange("p (h d) -> p h d", h=10)
            t2 = B["t2"][:].rearrange("p (h d) -> p h d", h=10)
            rope3(P, x4[:, :, 0, :], x4[:, :, 1, :], qk4[:, :, 0, :], qk4[:, :, 1, :], cs, sn, t1, t2, None,
                  [k + "x", k + "rp"], [k + "qk0", k + "qk1"], k)
            v = B["v"]
            P.op("pool", lambda e, v=v: e.memset(v[:, :, 64:65], 1.0), writes=[k + "v1"])
            P.op("act", lambda e, v=v, x=x: e.copy(out=v[:, :, 0:64], in_=x[:, 640:768].rearrange("p (h d) -> p h d", h=2)),
                 reads=[k + "x"], writes=[k + "v"])
            P.dma("sp", lambda e, v=v, t0=t0: e.dma_start(out=K.Sc["gv"][t0:t0 + 128, :, :], in_=v[:]), reads=[k + "v", k + "v1"], writes=["gv"])
            pt, qkT = B["pt"], B["qkT"]
            for j in range(5):
                P.op("pe", lambda e, pt=pt, j=j, qk=B["qk"]: e.transpose(pt[:, j * 128:(j + 1) * 128], qk[:, j * 128:(j + 1) * 128], K.identb[:]),
                     reads=[k + "qk0", k + "qk1", "c_identb"], writes=[k + "pt"])
            P.op("act", lambda e, pt=pt, qkT=qkT: e.copy(out=qkT[:], in_=pt[:, 0:640]), reads=[k + "pt"], writes=[k + "qkT"])
            P.dma("sp", lambda e, qkT=qkT, t0=t0: e.dma_start(
                out=K.Sc["gqT"].rearrange("(j p) s -> p j s", p=128)[:, :, t0:t0 + 128],
                in_=qkT[:].rearrange("p (j t) -> p j t", j=5)), reads=[k + "qkT"], writes=["gqT"])


def attention(K, heads, dq, scale, tag):
    P, S = K.P, K.S
    QN = min(512, S)
    NKT = S // 128
    with Phase(K) as ph:
        ones = ph.sb("a_ones", [128, 64])
        P.op("dve", lambda e: e.memset(ones[:], 1.0), writes=["a_ones"])
        pad = getattr(K.cfg, "pad", 1)
        PK = 128 if pad else dq
        PM = 128 if pad else 65
        kT = [ph.sb("a_kT%d" % i, [PK, S], BF16) for i in range(2)]
        vt = [ph.sb("a_v%d" % i, [128, NKT, PM], BF16) for i in range(2)]
        qT = [ph.sb("a_qT%d" % i, [PK, QN], BF16) for i in range(2)]
        if pad:
            for i in range(2):
                P.op("pool", lambda e, i=i: e.memset(kT[i][:], 0.0), writes=["a_kT%d" % i])
                P.op("pool", lambda e, i=i: e.memset(vt[i][:], 0.0), writes=["a_v%d" % i])
                P.op("pool", lambda e, i=i: e.memset(qT[i][:], 0.0), writes=["a_qT%d" % i])
        pT = [ph.sb("a_pT%d" % i, [128, QN], BF16) for i in range(3)]
        rrow = ph.sb("a_rrow", [128, QN])
        rb = ph.sb("a_rb", [64, QN])
        ob = [ph.sb("a_ob%d" % i, [64, QN], BF16) for i in range(2)]
        ps_s = [ph.ps("a_ps%d" % i, [128, QN]) for i in range(3)]
        ps_o = [ph.ps("a_po%d" % i, [PM, QN]) for i in range(2)]
        ps_b = ph.ps("a_pb", [64, QN])
        nq = 0
        ns = 0
        for hi, (q_ap, k_ap, v_ap, o_ap) in enumerate(heads):
            hb = hi % 2
            P.dma("sp", lambda e, hb=hb, k_ap=k_ap: e.dma_start(out=kT[hb][0:dq, :], in_=k_ap), reads=[tag + "kT_d"], writes=["a_kT%d" % hb])
            P.dma("sp", lambda e, hb=hb, v_ap=v_ap: e.dma_start(out=vt[hb][:, :, 0:65], in_=v_ap.rearrange("(kc p) e -> p kc e", p=128)),
                  reads=[tag + "v_d"], writes=["a_v%d" % hb])
            for qc in range(S // QN):
                qb = nq % 2
                nq += 1
                q0 = qc * QN
                P.dma("sp", lambda e, qb=qb, q_ap=q_ap, q0=q0: e.dma_start(out=qT[qb][0:dq, :], in_=q_ap[:, q0:q0 + QN]),
                      reads=[tag + "qT_d"], writes=["a_qT%d" % qb])
                sbs = []
                for kc in range(NKT + 1):
                    if kc < NKT:
                        sb_ = ns % 3
                        ns += 1
                        sbs.append(sb_)
                        P.op("pe", lambda e, sb_=sb_, hb=hb, kc=kc, qb=qb: e.matmul(
                            ps_s[sb_][:], lhsT=kT[hb][:, kc * 128:(kc + 1) * 128], rhs=qT[qb][:], start=True, stop=True),
                            reads=["a_kT%d" % hb, "a_qT%d" % qb], writes=["a_ps%d" % sb_])
                        P.op("act", lambda e, sb_=sb_: e.activation(out=pT[sb_][:], in_=ps_s[sb_][:], func=AF.Exp, scale=scale),
                             reads=["a_ps%d" % sb_], writes=["a_pT%d" % sb_])
                    if kc >= 1:
                        kp = kc - 1
                        sp_ = sbs[kp]
                        P.op("pe", lambda e, sp_=sp_, hb=hb, kp=kp, qb=qb: e.matmul(
                            ps_o[qb][:], lhsT=vt[hb][:, kp, :], rhs=pT[sp_][:], start=(kp == 0), stop=(kp == NKT - 1)),
                            reads=["a_v%d" % hb, "a_pT%d" % sp_], writes=["a_po%d" % qb])
                P.op("dve", lambda e, qb=qb: e.reciprocal(out=rrow[64:65, :], in_=ps_o[qb][64:65, :]), reads=["a_po%d" % qb], writes=["a_rrow"])
                P.op("pe", lambda e: e.matmul(ps_b[:], lhsT=ones[64:65, 0:64], rhs=rrow[64:65, :], start=True, stop=True),
                     reads=["a_ones", "a_rrow"], writes=["a_pb"])
                P.op("act", lambda e: e.copy(out=rb[:], in_=ps_b[:]), reads=["a_pb"], writes=["a_rb"])
                P.op("dve", lambda e, qb=qb: e.tensor_tensor(out=ob[qb][:], in0=ps_o[qb][0:64, :], in1=rb[:], op=ALU.mult),
                     reads=["a_po%d" % qb, "a_rb"], writes=["a_ob%d" % qb])
                P.dma("sp", lambda e, qb=qb, o_ap=o_ap, q0=q0: e.dma_start(out=o_ap[:, q0:q0 + QN], in_=ob[qb][:]),
                      reads=["a_ob%d" % qb], writes=["br"])


def phase_gqa_attn(K, l):
    gqT, gv, br = K.Sc["gqT"], K.Sc["gv"], K.Sc["br"]
    heads = []
    for h in range(8):
        kv = h // 4
        heads.append((gqT[h * 64:(h + 1) * 64, :], gqT[512 + kv * 64:512 + (kv + 1) * 64, :], gv[:, kv, :],
                      br[512 + h * 64:512 + (h + 1) * 64, :]))
    attention(K, heads, 64, 64 ** -0.5, "g")


def phase_mla_prep(K, l):
    P, I, S = K.P, K.I, K.S
    tm = K.Sc["tm"]
    rope_m = I["rope_m"]
    with Phase(K) as ph:
        eps_t = ph.sb("eps_t", [128, 1])
        P.op("dve", lambda e: e.memset(eps_t[:], NORM_EPS), writes=["eps_t"])
        gc, gckeys = bcast_rows(K, ph, "m_gc", I["mla_qc_norm"][l:l + 1, :], 384, 1)
        gkv, gkvkeys = bcast_rows(K, ph, "m_gkv", I["mla_kvc_norm"][l:l + 1, :], 256, 1)
        gq, gqkeys = bcast_rows(K, ph, "m_gq", I["mla_q_norm"][l:l + 1, :], 96, 8)
        gk, gkkeys = bcast_rows(K, ph, "m_gk", I["mla_k_norm"][l:l + 1, :], 96, 8)
        wqf = ph.sb("m_wqf", [128, 3, 768])
        wkf = ph.sb("m_wkf", [128, 2, 1024])
        wq = ph.sb("m_wq", [128, 3, 768], BF16)
        wk = ph.sb("m_wk", [128, 2, 1024], BF16)
        P.dma("sp", lambda e: e.dma_start(out=wqf[:], in_=I["mla_w_uq"][l].rearrange("(k p) n -> p k n", p=128)), writes=["m_wqf"])
        P.dma("sp", lambda e: e.dma_start(out=wkf[:], in_=I["mla_w_ukv"][l].rearrange("(k p) n -> p k n", p=128)), writes=["m_wkf"])
        P.op("pool", lambda e: e.tensor_copy(out=wq[:], in_=wqf[:]), reads=["m_wqf"], writes=["m_wq"])
        P.op("pool", lambda e: e.tensor_copy(out=wk[:], in_=wkf[:]), reads=["m_wkf"], writes=["m_wk"])
        x = ph.sb("m_x", [128, 672])
        rp = ph.sb("m_rp", [128, 32])
        junk = ph.sb("m_junk", [128, 384])
        ss = ph.sb("m_ss", [128, 2])
        cn = ph.sb("m_cn", [128, 640], BF16)
        pt = ph.ps("m_pt", [128, 1024], BF16)
        cnT = ph.sb("m_cnT", [128, 5, 128], BF16)
        pq = [ph.ps("m_pq%d" % i, [128, 512]) for i in range(2)]
        pkv = [ph.ps("m_pkv%d" % i, [128, 512]) for i in range(2)]
        q = ph.sb("m_q", [128, 8, 96])
        kk = ph.sb("m_k", [128, 8, 96])
        sq = ph.sb("m_sq", [128, 8, 96])
        ssq = ph.sb("m_ssq", [128, 8])
        t1 = ph.sb("m_t1", [128, 8, 16])
        t2 = ph.sb("m_t2", [128, 8, 16])
        qb = ph.sb("m_qb", [128, 8, 96], BF16)
        kb = ph.sb("m_kb", [128, 8, 96], BF16)
        v = ph.sb("m_v", [128, 8, 65], BF16)
        ptq = ph.ps("m_ptq", [96, 8, 128], BF16)
        ptk = ph.ps("m_ptk", [96, 8, 128], BF16)
        qT = ph.sb("m_qT", [96, 8, 128], BF16)
        kT = ph.sb("m_kT", [96, 8, 128], BF16)
        P.op("pool", lambda e: e.memset(v[:, :, 64:65], 1.0), writes=["m_v1"])
        for tt in range(S // 128):
            t0 = tt * 128
            P.dma("sp", lambda e, t0=t0: e.dma_start(out=x[:], in_=tm[t0:t0 + 128, 768:1440]), reads=["tm"], writes=["m_x"])
            P.dma("sp", lambda e, t0=t0: e.dma_start(out=rp[:], in_=rope_m[t0:t0 + 128, :]), writes=["m_rp"])
            for i, (c0, n, g_t, gkeys_) in enumerate(((0, 384, gc, gckeys), (384, 256, gkv, gkvkeys))):
                P.op("act", lambda e, c0=c0, n=n, i=i: e.activation(out=junk[:, 0:n], in_=x[:, c0:c0 + n], func=AF.Square,
                                                                    accum_out=ss[:, i:i + 1]), reads=["m_x"], writes=["m_junk", "m_ss%d" % i])
                P.op("act", lambda e, n=n, i=i: e.activation(out=ss[:, i:i + 1], in_=ss[:, i:i + 1], func=AF.Sqrt, scale=1.0 / n,
                                                             bias=eps_t[:, 0:1]), reads=["m_ss%d" % i, "eps_t"], writes=["m_ss%d" % i])
                P.op("dve", lambda e, i=i: e.reciprocal(out=ss[:, i:i + 1], in_=ss[:, i:i + 1]), reads=["m_ss%d" % i], writes=["m_ss%d" % i])
                P.op("dve", lambda e, c0=c0, n=n, i=i, g_t=g_t: e.scalar_tensor_tensor(
                    out=cn[:, c0:c0 + n], in0=x[:, c0:c0 + n], scalar=ss[:, i:i + 1], in1=g_t[:, 0, :], op0=ALU.mult, op1=ALU.mult),
                    reads=["m_x", "m_ss%d" % i] + gkeys_, writes=["m_cn%d" % i])
            for j in range(5):
                P.op("pe", lambda e, j=j: e.transpose(pt[:, j * 128:(j + 1) * 128], cn[:, j * 128:(j + 1) * 128], K.identb[:]),
                     reads=["m_cn0", "m_cn1", "c_identb"], writes=["m_pt"])
            P.op("act", lambda e: e.copy(out=cnT[:], in_=pt[:, 0:640].rearrange("p (j t) -> p j t", j=5)), reads=["m_pt"], writes=["m_cnT"])
            if getattr(K.cfg, "dbg", 0) == 1:
                continue
            for half in range(2):
                for kc in range(3):
                    P.op("pe", lambda e, half=half, kc=kc: e.matmul(pq[half][:, 0:384], lhsT=cnT[:, kc, :], rhs=wq[:, kc, half * 384:(half + 1) * 384],
                                                                    start=(kc == 0), stop=(kc == 2)), reads=["m_cnT", "m_wq"], writes=["m_pq%d" % half])
                for kc in range(2):
                    P.op("pe", lambda e, half=half, kc=kc: e.matmul(pkv[half][:], lhsT=cnT[:, 3 + kc, :], rhs=wk[:, kc, half * 512:(half + 1) * 512],
                                                                    start=(kc == 0), stop=(kc == 1)), reads=["m_cnT", "m_wk"], writes=["m_pkv%d" % half])
            if getattr(K.cfg, "dbg", 0) == 5:
                continue
            for half in range(2):
                P.op("act", lambda e, half=half: e.copy(out=q[:, half * 4:(half + 1) * 4, :], in_=pq[half][:, 0:384].rearrange("p (h d) -> p h d", h=4)),
                     reads=["m_pq%d" % half], writes=["m_q%d" % half])
                pk4 = pkv[half][:].rearrange("p (h d) -> p h d", h=4)
                if getattr(K.cfg, "dbg", 0) == 6:
                    continue
                P.op("dve", lambda e, half=half, pk4=pk4: e.tensor_copy(out=kk[:, half * 4:(half + 1) * 4, 0:64], in_=pk4[:, :, 0:64]),
                     reads=["m_pkv%d" % half], writes=["m_kn%d" % half])
                if getattr(K.cfg, "dbg", 0) == 7:
                    continue
                P.op("dve", lambda e, half=half, pk4=pk4: e.tensor_copy(out=v[:, half * 4:(half + 1) * 4, 0:64], in_=pk4[:, :, 64:128]),
                     reads=["m_pkv%d" % half], writes=["m_v%d" % half])
            if getattr(K.cfg, "dbg", 0) == 2:
                continue
            P.op("pool", lambda e: e.tensor_copy(out=kk[:, :, 64:96], in_=x[:, 640:672].unsqueeze(1).to_broadcast([128, 8, 32])),
                 reads=["m_x"], writes=["m_kpe"])
            if getattr(K.cfg, "dbg", 0) == 3:
                continue
            P.dma("sp", lambda e, t0=t0: e.dma_start(out=K.Sc["mv"][t0:t0 + 128, :, :], in_=v[:]), reads=["m_v0", "m_v1", "m_v1"], writes=["mv"])
            cs = rp[:, 0:16].unsqueeze(1).to_broadcast([128, 8, 16])
            sn = rp[:, 16:32].unsqueeze(1).to_broadcast([128, 8, 16])
            for (t_, keys_, g_t, gkeys_, ob_, okey, pt_, T_, dkey) in (
                    (q, ["m_q0", "m_q1"], gq, gqkeys, qb, "m_qb", ptq, qT, "mqT"),
                    (kk, ["m_kn0", "m_kn1", "m_kpe"], gk, gkkeys, kb, "m_kb", ptk, kT, "mkT")):
                kx = okey + "x"
                P.op("dve", lambda e, t_=t_: e.tensor_tensor(out=sq[:], in0=t_[:], in1=t_[:], op=ALU.mult), reads=keys_, writes=["m_sq"])
                P.op("dve", lambda e: e.tensor_reduce(out=ssq[:], in_=sq[:], axis=AX.X, op=ALU.add), reads=["m_sq"], writes=["m_ssq"])
                P.op("act", lambda e: e.activation(out=ssq[:], in_=ssq[:], func=AF.Sqrt, scale=1.0 / 96, bias=eps_t[:, 0:1]),
                     reads=["m_ssq", "eps_t"], writes=["m_ssq"])
                P.op("dve", lambda e: e.reciprocal(out=ssq[:], in_=ssq[:]), reads=["m_ssq"], writes=["m_ssq"])
                P.op("dve", lambda e, t_=t_: e.tensor_tensor(out=t_[:], in0=t_[:], in1=ssq[:].unsqueeze(2).to_broadcast([128, 8, 96]), op=ALU.mult),
                     reads=keys_ + ["m_ssq"], writes=[kx])
                P.op("pool", lambda e, t_=t_, g_t=g_t: e.tensor_tensor(out=t_[:], in0=t_[:], in1=g_t[:], op=ALU.mult), reads=[kx] + gkeys_, writes=[kx])
                P.op("act", lambda e, t_=t_, ob_=ob_: e.copy(out=ob_[:, :, 0:64], in_=t_[:, :, 0:64]), reads=[kx], writes=[okey + "n"])
                rope3(P, t_[:, :, 64:80], t_[:, :, 80:96], ob_[:, :, 64:80], ob_[:, :, 80:96], cs, sn, t1[:], t2[:], None,
                      [kx, "m_rp"], [okey + "r0", okey + "r1"], okey)
                if getattr(K.cfg, "dbg", 0) == 4:
                    continue
                for h in range(8):
                    P.op("pe", lambda e, h=h, pt_=pt_, ob_=ob_: e.transpose(pt_[:, h, :], ob_[:, h, :], K.identb[:]),
                         reads=[okey + "n", okey + "r0", okey + "r1", "c_identb"], writes=[okey + "pt"])
                P.op("act", lambda e, pt_=pt_, T_=T_: e.copy(out=T_[:], in_=pt_[:]), reads=[okey + "pt"], writes=[okey + "T"])
                P.dma("sp", lambda e, T_=T_, t0=t0, dkey=dkey: e.dma_start(
                    out=K.Sc[dkey].rearrange("(h d) s -> d h s", d=96)[:, :, t0:t0 + 128], in_=T_[:]), reads=[okey + "T"], writes=[dkey])


def phase_mla_attn(K, l):
    mqT, mkT, mv, br = K.Sc["mqT"], K.Sc["mkT"], K.Sc["mv"], K.Sc["br"]
    heads = []
    for h in range(8):
        heads.append((mqT[h * 96:(h + 1) * 96, :], mkT[h * 96:(h + 1) * 96, :], mv[:, h, :], br[1536 + h * 64:1536 + (h + 1) * 64, :]))
    attention(K, heads, 96, 96 ** -0.5, "m")


def load_cast(K, src_ap, stage_ap, dst_ap, skey, dkey, eng="pool"):
    P = K.P
    P.dma("sp", lambda e: e.dma_start(out=stage_ap, in_=src_ap), writes=[skey])
    P.op(eng, lambda e: e.tensor_copy(out=dst_ap, in_=stage_ap), reads=[skey], writes=[dkey])


def phase3a(K, l):
    P, I, S = K.P, K.I, K.S
    br, gt, mT = K.Sc["br"], K.Sc["gt"], K.Sc["mT"]
    NT = min(512, S)
    with Phase(K) as ph:
        wbf = ph.sb("wbf", [128, 4, D])
        wb = ph.sb("wb3", [128, 16, D], BF16)
        for i in range(4):
            load_cast(K, I["w_branch"][l][i * 512:(i + 1) * 512, :].rearrange("(kc p) n -> p kc n", p=128), wbf[:],
                      wb[:, i * 4:(i + 1) * 4, :], "wbf", "wb3_%d" % i)
        wkeys = ["wb3_%d" % i for i in range(4)]
        brT = [ph.sb("brT%d" % i, [128, 16, NT], BF16) for i in range(2)]
        gts = [ph.sb("gts%d" % i, [128, 4, NT], BF16) for i in range(2)]
        tmp = [ph.sb("tmp3_%d" % i, [128, NT]) for i in range(8)]
        ms = [ph.sb("ms%d" % i, [128, NT], BF16) for i in range(2)]
        ps = [ph.ps("p3_%d" % i, [128, 512]) for i in range(8)]
        gtv = gt.rearrange("(i n) s -> n i s", i=4)
        brv = br.rearrange("(k p) s -> p k s", p=128)
        n = 0
        for st in range(S // NT):
            tok0 = st * NT
            bb = st % 2
            P.dma("sp", lambda e, bb=bb, tok0=tok0: e.dma_start(out=brT[bb][:], in_=brv[:, :, tok0:tok0 + NT]), reads=["br"], writes=["brT%d" % bb])
            for nt in range(16):
                gb = n % 2
                P.dma("sp", lambda e, gb=gb, nt=nt, tok0=tok0: e.dma_start(out=gts[gb][:], in_=gtv[nt * 128:(nt + 1) * 128, :, tok0:tok0 + NT]),
                      reads=["gt"], writes=["gts%d" % gb])
                for i in range(4):
                    pi = (n % 2) * 4 + i
                    for kc in range(4):
                        P.op("pe", lambda e, pi=pi, i=i, kc=kc, nt=nt, bb=bb: e.matmul(
                            ps[pi][:, 0:NT], lhsT=wb[:, i * 4 + kc, nt * 128:(nt + 1) * 128], rhs=brT[bb][:, i * 4 + kc, :],
                            start=(kc == 0), stop=(kc == 3)), reads=wkeys + ["brT%d" % bb], writes=["p3_%d" % pi])
                    ti = (n % 2) * 4 + i
                    P.op("dve", lambda e, pi=pi, i=i, gb=gb, ti=ti: e.tensor_tensor(out=tmp[ti][:], in0=ps[pi][:, 0:NT], in1=gts[gb][:, i, :], op=ALU.mult),
                         reads=["p3_%d" % pi, "gts%d" % gb], writes=["tmp3_%d" % ti])
                tb_ = (n % 2) * 4
                P.op("pool", lambda e, tb_=tb_: e.tensor_tensor(out=tmp[tb_][:], in0=tmp[tb_][:], in1=tmp[tb_ + 1][:], op=ALU.add),
                     reads=["tmp3_%d" % tb_, "tmp3_%d" % (tb_ + 1)], writes=["tmp3_%d" % tb_])
                P.op("pool", lambda e, tb_=tb_: e.tensor_tensor(out=tmp[tb_ + 2][:], in0=tmp[tb_ + 2][:], in1=tmp[tb_ + 3][:], op=ALU.add),
                     reads=["tmp3_%d" % (tb_ + 2), "tmp3_%d" % (tb_ + 3)], writes=["tmp3_%d" % (tb_ + 2)])
                P.op("dve", lambda e, gb=gb, tb_=tb_: e.tensor_tensor(out=ms[gb][:], in0=tmp[tb_][:], in1=tmp[tb_ + 2][:], op=ALU.add),
                     reads=["tmp3_%d" % tb_, "tmp3_%d" % (tb_ + 2)], writes=["ms%d" % gb])
                P.dma("sp", lambda e, gb=gb, nt=nt, tok0=tok0: e.dma_start(out=mT[nt * 128:(nt + 1) * 128, tok0:tok0 + NT], in_=ms[gb][:]),
                      reads=["ms%d" % gb], writes=["mT"])
                n += 1


def phase3b(K, l):
    P, I, S = K.P, K.I, K.S
    mT, modv = K.Sc["mT"], K.Sc["modv"]
    xd = K.xres
    NTT = S // 128
    MT = min(512, S)
    with Phase(K) as ph:
        eps_t = ph.sb("eps3", [128, 1])
        P.op("dve", lambda e: e.memset(eps_t[:], NORM_EPS), writes=["eps3"])
        wof = ph.sb("wof", [128, 4, D])
        wo = ph.sb("wo", [128, 16, D], BF16)
        for i in range(4):
            load_cast(K, I["w_out"][l][i * 512:(i + 1) * 512, :].rearrange("(kc p) n -> p kc n", p=128), wof[:],
                      wo[:, i * 4:(i + 1) * 4, :], "wof", "wo_%d" % i)
        wkeys = ["wo_%d" % i for i in range(4)]
        wr = ph.sb("wr", [128, 16, NE])
        P.dma("sp", lambda e: e.dma_start(out=wr[:], in_=I["w_router"][l].rearrange("(k p) n -> p k n", p=128)), writes=["wr"])
        G1 = load_bcast_row(K, ph, "G1_b", modv[l * 6 + 2:l * 6 + 3, :], D)
        B2 = load_bcast_row(K, ph, "B2_b", modv[l * 6 + 3:l * 6 + 4, :], D)
        A2 = load_bcast_row(K, ph, "A2_b", modv[l * 6 + 4:l * 6 + 5, :], D)
        mTt = [ph.sb("mTt%d" % i, [128, 16, MT], BF16) for i in range(2)]
        xt = [ph.sb("x3t%d" % i, [128, D]) for i in range(2)]
        tmp = [ph.sb("tmp3b%d" % i, [128, 512]) for i in range(2)]
        junk = ph.sb("junk3", [128, D], BF16)
        h2b = [ph.sb("h2b%d" % i, [128, D], BF16) for i in range(2)]
        ss = ph.sb("ss3", [128, 1])
        rstd = ph.sb("rstd3", [128, 1])
        h2T = ph.sb("h2T", [128, 16, 128])
        lg = ph.sb("lg", [128, NE])
        mx = ph.sb("mx3", [128, 1])
        sm = ph.sb("sm3", [128, 1])
        aff = ph.sb("aff_all", [128, NTT, NE])
        affT = [ph.sb("affT_s%d" % i, [NE, 128]) for i in range(2)]
        po = [ph.ps("p3o%d" % i, [128, 512]) for i in range(2)]
        ptr = [ph.ps("p3t%d" % i, [128, 512]) for i in range(2)]
        pr = ph.ps("p3r", [128, 512])
        pat = ph.ps("p3at", [128, 512])
        mTv = mT.rearrange("(k p) s -> p k s", p=128)
        n = 0
        for st in range(S // MT):
            mb = st % 2
            P.dma("sp", lambda e, mb=mb, st=st: e.dma_start(out=mTt[mb][:], in_=mTv[:, :, st * MT:(st + 1) * MT]), reads=["mT"], writes=["mTt%d" % mb])
            for t in range(MT // 128):
                tt = st * (MT // 128) + t
                t0 = tt * 128
                xb = tt % 2
                P.dma("sp", lambda e, xb=xb, t0=t0: e.dma_start(out=xt[xb][:], in_=xd[t0:t0 + 128, :]), reads=["xres"], writes=["x3t%d" % xb])
                for c4 in range(4):
                    pi = n % 2
                    n += 1
                    for kc in range(16):
                        P.op("pe", lambda e, pi=pi, kc=kc, mb=mb, t=t, c4=c4: e.matmul(
                            po[pi][:], lhsT=mTt[mb][:, kc, t * 128:(t + 1) * 128], rhs=wo[:, kc, c4 * 512:(c4 + 1) * 512],
                            start=(kc == 0), stop=(kc == 15)), reads=wkeys + ["mTt%d" % mb], writes=["p3o%d" % pi])
                    P.op("dve", lambda e, pi=pi, c4=c4: e.tensor_tensor(out=tmp[pi][:], in0=po[pi][:], in1=G1[:, c4 * 512:(c4 + 1) * 512], op=ALU.mult),
                         reads=["p3o%d" % pi, "G1_b"], writes=["tmp3b%d" % pi])
                    P.op("pool", lambda e, pi=pi, c4=c4, xb=xb: e.tensor_tensor(out=xt[xb][:, c4 * 512:(c4 + 1) * 512], in0=xt[xb][:, c4 * 512:(c4 + 1) * 512],
                                                                                in1=tmp[pi][:], op=ALU.add),
                         reads=["tmp3b%d" % pi, "x3t%d" % xb], writes=["x3t%d" % xb])
                P.dma("sp", lambda e, xb=xb, t0=t0: e.dma_start(out=xd[t0:t0 + 128, :], in_=xt[xb][:]), reads=["x3t%d" % xb], writes=["xres"])
                P.op("act", lambda e, xb=xb: e.activation(out=junk[:], in_=xt[xb][:], func=AF.Square, accum_out=ss[:, 0:1]),
                     reads=["x3t%d" % xb], writes=["junk3", "ss3"])
                P.op("act", lambda e: e.activation(out=rstd[:], in_=ss[:], func=AF.Sqrt, scale=1.0 / D, bias=eps_t[:, 0:1]),
                     reads=["ss3", "eps3"], writes=["rstd3"])
                P.op("dve", lambda e: e.reciprocal(out=rstd[:], in_=rstd[:]), reads=["rstd3"], writes=["rstd3"])
                P.op("dve", lambda e, xb=xb: e.scalar_tensor_tensor(out=xt[xb][:], in0=xt[xb][:], scalar=rstd[:, 0:1], in1=A2[:], op0=ALU.mult, op1=ALU.mult),
                     reads=["x3t%d" % xb, "rstd3", "A2_b"], writes=["x3t%d" % xb])
                P.op("pool", lambda e, xb=xb: e.tensor_tensor(out=xt[xb][:], in0=xt[xb][:], in1=B2[:], op=ALU.add),
                     reads=["x3t%d" % xb, "B2_b"], writes=["x3t%d" % xb])
                P.op("act", lambda e, xb=xb: e.copy(out=h2b[xb][:], in_=xt[xb][:]), reads=["x3t%d" % xb], writes=["h2b%d" % xb])
                P.dma("sp", lambda e, xb=xb, t0=t0: e.dma_start(out=K.Sc["h2"][t0:t0 + 128, :], in_=h2b[xb][:]), reads=["h2b%d" % xb], writes=["h2"])
                for g4 in range(4):
                    tb = g4 % 2
                    for j in range(4):
                        kc = g4 * 4 + j
                        P.op("pe", lambda e, tb=tb, j=j, kc=kc, xb=xb: e.transpose(ptr[tb][:, j * 128:(j + 1) * 128], xt[xb][:, kc * 128:(kc + 1) * 128], K.ident[:]),
                             reads=["x3t%d" % xb, "c_ident"], writes=["p3t%d" % tb])
                    P.op("act", lambda e, tb=tb, g4=g4: e.copy(out=h2T[:, g4 * 4:(g4 + 1) * 4, :], in_=ptr[tb][:].rearrange("p (j t) -> p j t", j=4)),
                         reads=["p3t%d" % tb], writes=["h2T"])
                for kc in range(16):
                    P.op("pe", lambda e, kc=kc: e.matmul(pr[:, 0:NE], lhsT=h2T[:, kc, :], rhs=wr[:, kc, :], start=(kc == 0), stop=(kc == 15)),
                         reads=["h2T", "wr"], writes=["p3r"])
                P.op("dve", lambda e: e.tensor_reduce(out=mx[:], in_=pr[:, 0:NE], axis=AX.X, op=ALU.max), reads=["p3r"], writes=["mx3"])
                P.op("dve", lambda e: e.tensor_scalar(out=mx[:], in0=mx[:], scalar1=-1.0, scalar2=None, op0=ALU.mult), reads=["mx3"], writes=["mx3"])
                P.op("act", lambda e: e.activation(out=lg[:], in_=pr[:, 0:NE], func=AF.Exp, bias=mx[:, 0:1], accum_out=sm[:, 0:1]),
                     reads=["p3r", "mx3"], writes=["lg", "sm3"])
                P.op("dve", lambda e: e.reciprocal(out=sm[:], in_=sm[:]), reads=["sm3"], writes=["sm3"])
                P.op("dve", lambda e, tt=tt: e.tensor_scalar(out=aff[:, tt, :], in0=lg[:], scalar1=sm[:, 0:1], scalar2=None, op0=ALU.mult),
                     reads=["lg", "sm3"], writes=["aff%d" % tt])
                P.op("pe", lambda e, tt=tt: e.transpose(pat[0:NE, 0:128], aff[:, tt, :], K.ident[:]), reads=["aff%d" % tt, "c_ident"], writes=["p3at"])
                P.op("act", lambda e, xb=xb: e.copy(out=affT[xb][:], in_=pat[0:NE, 0:128]), reads=["p3at"], writes=["affT%d" % xb])
                P.dma("sp", lambda e, xb=xb, t0=t0: e.dma_start(out=K.Sc["affT"][:, t0:t0 + 128], in_=affT[xb][:]), reads=["affT%d" % xb], writes=["affT_d"])
        akeys = ["aff%d" % tt for tt in range(NTT)]
        P.dma("sp", lambda e: e.dma_start(out=K.Sc["aff"].rearrange("(t p) n -> p t n", p=128), in_=aff[:]), reads=akeys, writes=["aff_d"])


def phase_moe(K, l):
    P, I, S = K.P, K.I, K.S
    NTT = S // 128
    CAP = S // 8
    NSL = min(128, CAP)
    NST = CAP // NSL
    h2, affd, affTd, ybuf, rankTd = K.Sc["h2"], K.Sc["aff"], K.Sc["affT"], K.Sc["ybuf"], K.Sc["rankT"]
    cast_i = [0]

    def cast(dst, src, rk, wk):
        eng = ("pool", "act")[cast_i[0] % 2]
        cast_i[0] += 1
        if eng == "pool":
            P.op("pool", lambda e: e.tensor_copy(out=dst, in_=src), reads=[rk], writes=[wk])
        else:
            P.op("act", lambda e: e.copy(out=dst, in_=src), reads=[rk], writes=[wk])

    with Phase(K) as ph:
        aff = ph.sb("m_aff", [128, NTT, NE])
        rank = ph.sb("e_rank", [128, NTT, NE])
        affb = ph.sb("m_affb", [128, S])
        junk = ph.sb("m_junk", [128, S], BF16)
        rkT = [ph.sb("m_rkT%d" % i, [NE, 128]) for i in range(2)]
        P.dma("sp", lambda e: e.dma_start(out=aff[:], in_=affd.rearrange("(t p) n -> p t n", p=128)), reads=["aff_d"], writes=["m_aff"])

        def rank_gen(ex):
            P.dma("sp", lambda e: e.dma_start(out=affb[:], in_=affTd[ex:ex + 1, :].to_broadcast([128, S])), reads=["affT_d"], writes=["m_affb"])
            for tt in range(NTT):
                P.op("dve", lambda e, tt=tt: e.tensor_scalar(
                    out=junk[:], in0=affb[:], scalar1=aff[:, tt, ex:ex + 1], scalar2=0.0, op0=ALU.is_gt, op1=ALU.add,
                    accum_out=rank[:, tt, ex:ex + 1]), reads=["m_affb", "m_aff"], writes=["m_junk", "e_rank%d" % ex])
                yield

        def pull(g, n):
            for _ in range(n):
                try:
                    next(g)
                except StopIteration:
                    return

        h2h = ph.sb("e_h2h", [128, NTT, 512], BF16)
        Pm = ph.sb("e_Pm", [128, NTT, CAP], BF16)
        xgT = ph.sb("e_xgT", [128, 16, CAP], BF16)
        hidT = ph.sb("e_hidT", [128, 8, CAP], BF16)
        wst = [ph.sb("e_wst%d" % i, [128, 4096]) for i in range(3)]
        wbf = [ph.sb("e_wbf%d" % i, [128, 4096], BF16) for i in range(4)]
        s1 = [ph.sb("e_s1%d" % i, [128, CAP]) for i in range(2)]
        ystg = [ph.sb("e_ystg%d" % i, [NSL, 512], BF16) for i in range(2)]
        pg = [ph.ps("e_pg%d" % i, [128, 512]) for i in range(2)]
        p1 = [ph.ps("e_p1%d" % i, [128, 512]) for i in range(2)]
        p3 = [ph.ps("e_p3%d" % i, [128, 512]) for i in range(2)]
        py = [ph.ps("e_py%d" % i, [128, 512]) for i in range(2)]
        h2v = h2.rearrange("(t p) d -> p t d", p=128)
        nws = [0]
        nwb = [0]

        wl = []
        for ex_ in range(NE):
            for fq in range(4):
                wl.append((I["moe_w1"][l, ex_][:, fq * 256:(fq + 1) * 256], 16, 256))
                wl.append((I["moe_w3"][l, ex_][:, fq * 256:(fq + 1) * 256], 16, 256))
            for dc in range(4):
                wl.append((I["moe_w2"][l, ex_][:, dc * 512:(dc + 1) * 512], 8, 512))
        issued = [0]
        wviews = {}

        def ensure(j):
            while issued[0] <= min(j, len(wl) - 1):
                i = issued[0]
                issued[0] += 1
                src_ap, nk, ncol = wl[i]
                si, bi = i % 3, i % 4
                sv = wst[si][:, 0:nk * ncol].rearrange("p (k n) -> p k n", k=nk)
                bv = wbf[bi][:, 0:nk * ncol].rearrange("p (k n) -> p k n", k=nk)
                P.dma("sp", lambda e, sv=sv, src_ap=src_ap: e.dma_start(out=sv, in_=src_ap.rearrange("(k p) n -> p k n", p=128)),
                      writes=["e_wst%d" % si])
                cast(bv, sv, "e_wst%d" % si, "e_wbf%d" % bi)
                wviews[i] = (bv, "e_wbf%d" % bi)

        wpos = [0]

        def load_w(src_ap, nk, ncol):
            i = wpos[0]
            wpos[0] += 1
            ensure(i + 2)
            return wviews[i]

        g0 = rank_gen(0)
        pull(g0, NTT + 1)
        for ex in range(NE):
            for tt in range(NTT):
                P.op("dve", lambda e, tt=tt, ex=ex: e.tensor_scalar(out=Pm[:, tt, :], in0=K.io[:, 0:CAP], scalar1=rank[:, tt, ex:ex + 1], scalar2=None,
                                                                    op0=ALU.is_equal), reads=["e_rank%d" % ex, "c_io"], writes=["e_Pm%d" % tt])
            pkeys = ["e_Pm%d" % tt for tt in range(NTT)]
            rg = rank_gen(ex + 1) if ex + 1 < NE else iter(())
            per = max(1, (NTT + 27) // 28)
            pull(rg, 2 * per)
            ng = 0
            ensure(wpos[0] + 1)
            for dh in range(4):
                P.dma("sp", lambda e, dh=dh: e.dma_start(out=h2h[:], in_=h2v[:, :, dh * 512:(dh + 1) * 512]), reads=["h2"], writes=["e_h2h"])
                for dt in range(4):
                    gi = ng % 2
                    ng += 1
                    for tt in range(NTT):
                        P.op("pe", lambda e, gi=gi, tt=tt, dt=dt: e.matmul(pg[gi][:, 0:CAP], lhsT=h2h[:, tt, dt * 128:(dt + 1) * 128], rhs=Pm[:, tt, :],
                                                                           start=(tt == 0), stop=(tt == NTT - 1)), reads=["e_h2h"] + pkeys, writes=["e_pg%d" % gi])
                    P.op("act", lambda e, gi=gi, dh=dh, dt=dt: e.copy(out=xgT[:, dh * 4 + dt, :], in_=pg[gi][:, 0:CAP]), reads=["e_pg%d" % gi], writes=["e_xgT"])
                    pull(rg, per)
            nf = 0
            for fq in range(4):
                w1v, w1k = load_w(I["moe_w1"][l, ex][:, fq * 256:(fq + 1) * 256], 16, 256)
                w3v, w3k = load_w(I["moe_w3"][l, ex][:, fq * 256:(fq + 1) * 256], 16, 256)
                for j in range(2):
                    fi = nf % 2
                    nf += 1
                    for kc in range(16):
                        P.op("pe", lambda e, fi=fi, kc=kc, j=j, w1v=w1v: e.matmul(p1[fi][:, 0:CAP], lhsT=w1v[:, kc, j * 128:(j + 1) * 128], rhs=xgT[:, kc, :],
                                                                                 start=(kc == 0), stop=(kc == 15)), reads=[w1k, "e_xgT"], writes=["e_p1%d" % fi])
                    for kc in range(16):
                        P.op("pe", lambda e, fi=fi, kc=kc, j=j, w3v=w3v: e.matmul(p3[fi][:, 0:CAP], lhsT=w3v[:, kc, j * 128:(j + 1) * 128], rhs=xgT[:, kc, :],
                                                                                 start=(kc == 0), stop=(kc == 15)), reads=[w3k, "e_xgT"], writes=["e_p3%d" % fi])
                    P.op("act", lambda e, fi=fi: e.activation(out=s1[fi][:], in_=p1[fi][:, 0:CAP], func=AF.Silu), reads=["e_p1%d" % fi], writes=["e_s1%d" % fi])
                    P.op("dve", lambda e, fi=fi, fq=fq, j=j: e.tensor_tensor(out=hidT[:, fq * 2 + j, :], in0=p3[fi][:, 0:CAP], in1=s1[fi][:], op=ALU.mult),
                         reads=["e_p3%d" % fi, "e_s1%d" % fi], writes=["e_hidT"])
                    pull(rg, per)
            ny = 0
            for dc in range(4):
                w2v, w2k = load_w(I["moe_w2"][l, ex][:, dc * 512:(dc + 1) * 512], 8, 512)
                for st in range(NST):
                    yi = ny % 2
                    ny += 1
                    for fc in range(8):
                        P.op("pe", lambda e, yi=yi, fc=fc, st=st, w2v=w2v: e.matmul(py[yi][0:NSL, :], lhsT=hidT[:, fc, st * NSL:(st + 1) * NSL], rhs=w2v[:, fc, :],
                                                                                   start=(fc == 0), stop=(fc == 7)), reads=[w2k, "e_hidT"], writes=["e_py%d" % yi])
                    P.op("act", lambda e, yi=yi: e.copy(out=ystg[yi][:], in_=py[yi][0:NSL, :]), reads=["e_py%d" % yi], writes=["e_ystg%d" % yi])
                    P.dma("sp", lambda e, yi=yi, ex=ex, st=st, dc=dc: e.dma_start(out=ybuf[ex][st * NSL:(st + 1) * NSL, dc * 512:(dc + 1) * 512], in_=ystg[yi][:]),
                          reads=["e_ystg%d" % yi], writes=["ybuf"])
            pull(rg, NTT + 1)
        for tt in range(NTT):
            ri = tt % 2
            P.op("pe", lambda e, tt=tt: e.transpose(pg[0][0:NE, 0:128], rank[:, tt, :], K.ident[:]),
                 reads=["e_rank%d" % ex_ for ex_ in range(NE)] + ["c_ident"], writes=["e_pg0"])
            P.op("act", lambda e, ri=ri: e.copy(out=rkT[ri][:], in_=pg[0][0:NE, 0:128]), reads=["e_pg0"], writes=["m_rkT%d" % ri])
            P.dma("sp", lambda e, ri=ri, tt=tt: e.dma_start(out=rankTd[:, tt * 128:(tt + 1) * 128], in_=rkT[ri][:]), reads=["m_rkT%d" % ri], writes=["rankT_d"])
    with Phase(K) as ph:
        TB = min(256, S)
        NTB = TB // 128
        G2 = load_bcast_row(K, ph, "G2_b", K.Sc["modv"][l * 6 + 5:l * 6 + 6, :], D)
        sid = ph.sb("s_sid", [128, NST])
        for st in range(NST):
            P.op("dve", lambda e, st=st: e.tensor_scalar(out=sid[:, st:st + 1], in0=K.iop[:, 0:1], scalar1=float(st * NSL), scalar2=None, op0=ALU.add),
                 reads=["c_iop"], writes=["s_sid"])
        rb = [ph.sb("s_rb%d" % i, [128, TB]) for i in range(2)]
        ab = [ph.sb("s_ab%d" % i, [128, TB]) for i in range(2)]
        pgt = [ph.sb("s_pgt%d" % i, [128, TB], BF16) for i in range(3)]
        yt = [ph.sb("s_yt%d" % i, [128, D], BF16) for i in range(3)]
        xt = [ph.sb("s_xt%d" % i, [128, D]) for i in range(2)]
        tmp = [ph.sb("s_tmp%d" % i, [128, 512]) for i in range(2)]
        pa = [ph.ps("s_pa%d" % i, [128, 512]) for i in range(NTB * 4)]
        nb = 0
        for blk in range(S // TB):
            tok0 = blk * TB
            for ex in range(NE):
                bi = ex % 2
                P.dma("sp", lambda e, bi=bi, ex=ex, tok0=tok0: e.dma_start(out=rb[bi][0:NSL, :], in_=rankTd[ex:ex + 1, tok0:tok0 + TB].to_broadcast([NSL, TB])),
                      reads=["rankT_d"], writes=["s_rb%d" % bi])
                P.dma("sp", lambda e, bi=bi, ex=ex, tok0=tok0: e.dma_start(out=ab[bi][0:NSL, :], in_=affTd[ex:ex + 1, tok0:tok0 + TB].to_broadcast([NSL, TB])),
                      reads=["affT_d"], writes=["s_ab%d" % bi])
                for st in range(NST):
                    ci = nb % 3
                    nb += 1
                    first = (ex == 0 and st == 0)
                    last = (ex == NE - 1 and st == NST - 1)
                    P.op("dve", lambda e, ci=ci, bi=bi, st=st: e.scalar_tensor_tensor(out=pgt[ci][0:NSL, :], in0=rb[bi][0:NSL, :], scalar=sid[0:NSL, st:st + 1],
                                                                                    in1=ab[bi][0:NSL, :], op0=ALU.is_equal, op1=ALU.mult),
                         reads=["s_rb%d" % bi, "s_ab%d" % bi, "s_sid"], writes=["s_pgt%d" % ci])
                    P.dma("sp", lambda e, ci=ci, ex=ex, st=st: e.dma_start(out=yt[ci][0:NSL, :], in_=ybuf[ex][st * NSL:(st + 1) * NSL, :]),
                          reads=["ybuf"], writes=["s_yt%d" % ci])
                    for tb in range(NTB):
                        for dc in range(4):
                            P.op("pe", lambda e, ci=ci, tb=tb, dc=dc, first=first, last=last: e.matmul(
                                pa[tb * 4 + dc][:], lhsT=pgt[ci][0:NSL, tb * 128:(tb + 1) * 128], rhs=yt[ci][0:NSL, dc * 512:(dc + 1) * 512],
                                start=first, stop=last), reads=["s_pgt%d" % ci, "s_yt%d" % ci], writes=["s_pa%d" % (tb * 4 + dc)])
            for tb in range(NTB):
                t0 = tok0 + tb * 128
                xb = tb % 2
                P.dma("sp", lambda e, xb=xb, t0=t0: e.dma_start(out=xt[xb][:], in_=K.xres[t0:t0 + 128, :]), reads=["xres"], writes=["s_xt%d" % xb])
                for dc in range(4):
                    ti = dc % 2
                    P.op("dve", lambda e, ti=ti, tb=tb, dc=dc: e.tensor_tensor(out=tmp[ti][:], in0=pa[tb * 4 + dc][:], in1=G2[:, dc * 512:(dc + 1) * 512], op=ALU.mult),
                         reads=["s_pa%d" % (tb * 4 + dc), "G2_b"], writes=["s_tmp%d" % ti])
                    P.op("pool", lambda e, ti=ti, xb=xb, dc=dc: e.tensor_tensor(out=xt[xb][:, dc * 512:(dc + 1) * 512], in0=xt[xb][:, dc * 512:(dc + 1) * 512],
                                                                                in1=tmp[ti][:], op=ALU.add), reads=["s_tmp%d" % ti, "s_xt%d" % xb], writes=["s_xt%d" % xb])
                P.dma("sp", lambda e, xb=xb, t0=t0: e.dma_start(out=K.xres[t0:t0 + 128, :], in_=xt[xb][:]), reads=["s_xt%d" % xb], writes=["xres"])


def phase_rwkv_prep(K, l):
    P, I, S = K.P, K.I, K.S
    fm = K.Sc["fm"]
    TT = min(512, S)
    NCH = TT // CH
    R0 = 1536
    with Phase(K) as ph:
        mu = ph.sb("r_mu", [128, 15, 3])
        muv = I["rwkv_mu"][l]
        for m_ in range(2):
            for rt in range(15):
                P.dma("sp", lambda e, m_=m_, rt=rt: e.dma_start(out=mu[:, rt, m_:m_ + 1], in_=muv[m_:m_ + 1, rt * 128:(rt + 1) * 128].rearrange("o p -> p o")),
                      writes=["r_mu"])
        P.op("dve", lambda e: e.tensor_tensor(out=mu[:, :, 2:3], in0=mu[:, :, 0:1], in1=mu[:, :, 1:2], op=ALU.add), reads=["r_mu"], writes=["r_mu"])
        P.op("dve", lambda e: e.tensor_scalar(out=mu[:, :, 2:3], in0=mu[:, :, 2:3], scalar1=-1.0, scalar2=1.0, op0=ALU.mult, op1=ALU.add),
             reads=["r_mu"], writes=["r_mu"])
        pc = ph.sb("r_pc", [128, 4, 8])
        srcs = [I["rwkv_w0"][l][0:1, :], I["rwkv_w0"][l][1:2, :], I["rwkv_a0"][l][0:1, :], I["rwkv_a0"][l][1:2, :],
                I["rwkv_k_k"][l:l + 1, :], I["rwkv_k_a"][l:l + 1, :], None, I["rwkv_r_k"][l:l + 1, :]]
        for ci, src in enumerate(srcs):
            if src is None:
                continue
            for ct in range(4):
                P.dma("sp", lambda e, ci=ci, ct=ct, src=src: e.dma_start(out=pc[:, ct, ci:ci + 1], in_=src[:, ct * 128:(ct + 1) * 128].rearrange("o p -> p o")),
                      writes=["r_pc"])
        P.op("dve", lambda e: e.tensor_scalar(out=pc[:, :, 6:7], in0=pc[:, :, 5:6], scalar1=-1.0, scalar2=1.0, op0=ALU.mult, op1=ALU.add),
             reads=["r_pc"], writes=["r_pc"])
        wst = ph.sb("r_wst", [128, 512])
        w2p = [ph.sb("r_w2p%d" % d, [128, 512], BF16) for d in range(2)]
        a2p = [ph.sb("r_a2p%d" % d, [128, 512], BF16) for d in range(2)]
        g2b = ph.sb("r_g2b", [128, 512], BF16)
        for name, dst in (("rwkv_w2", w2p), ("rwkv_a2", a2p)):
            P.dma("sp", lambda e, name=name: e.dma_start(out=wst[:], in_=I[name][l]), writes=["r_wst"])
            for d in range(2):
                P.op("dve", lambda e, d=d, dst=dst: e.memset(dst[d][:], 0.0), writes=["r_lw%s%d" % (name, d)])
                P.op("dve", lambda e, d=d, dst=dst: e.tensor_copy(out=dst[d][d * 64:(d + 1) * 64, :], in_=wst[d * 64:(d + 1) * 64, :]),
                     reads=["r_wst"], writes=["r_lw%s%d" % (name, d)])
        P.dma("sp", lambda e: e.dma_start(out=wst[:], in_=I["rwkv_g2"][l]), writes=["r_wst"])
        P.op("dve", lambda e: e.tensor_copy(out=g2b[:], in_=wst[:]), reads=["r_wst"], writes=["r_g2b"])
        lwk = ["r_lwrwkv_w2%d" % d for d in range(2)] + ["r_lwrwkv_a2%d" % d for d in range(2)] + ["r_g2b"]
        bones = ph.sb("r_bones", [128, 128])
        P.op("dve", lambda e: e.memset(bones[:], 0.0), writes=["r_bones"])
        P.op("dve", lambda e: e.memset(bones[0:64, 0:64], 1.0), writes=["r_bones"])
        P.op("dve", lambda e: e.memset(bones[64:128, 64:128], 1.0), writes=["r_bones"])
        ones = ph.sb("r_ones", [128, CH])
        P.op("dve", lambda e: e.memset(ones[:], 1.0), writes=["r_ones"])
        tiny = ph.sb("r_tiny", [128, 1])
        P.op("dve", lambda e: e.memset(tiny[:], 1e-24), writes=["r_tiny"])
        xr = [ph.sb("r_xr%d" % i, [128, TT + 2]) for i in range(2)]
        z = [ph.sb("r_z%d" % i, [128, TT]) for i in range(15)]
        twb = ph.sb("r_twb", [128, TT], BF16)
        adb = ph.sb("r_adb", [128, TT], BF16)
        sgb = ph.sb("r_sgb", [128, TT], BF16)
        ld = [ph.sb("r_ld%d" % d, [128, TT]) for d in range(2)]
        av = [ph.sb("r_a%d" % d, [128, TT]) for d in range(2)]
        kd = [ph.sb("r_kd%d" % d, [128, TT]) for d in range(2)]
        bb = [ph.sb("r_bb%d" % d, [128, TT]) for d in range(2)]
        gt_ = ph.sb("r_g", [128, TT])
        kq = ph.sb("r_kq", [128, TT])
        kkt = ph.sb("r_kk", [128, TT])
        t1 = ph.sb("r_t1", [128, TT])
        t2 = ph.sb("r_t2", [128, TT])
        lam = ph.sb("r_lam", [128, TT])
        Lt = ph.sb("r_L", [128, TT])
        E = [ph.sb("r_E%d" % i, [128, TT]) for i in range(4)]
        outs = [ph.sb("r_o%d" % i, [128, TT]) for i in range(6)]
        gc = ph.sb("r_gc", [128, NCH])
        tro = [ph.sb("r_tro%d" % i, [128, 128]) for i in range(2)]
        pm = [ph.ps("r_pm%d" % i, [128, 512]) for i in range(4)]
        ptr = [ph.ps("r_ptr%d" % i, [128, 512]) for i in range(2)]
        npm = [0]
        ntr = [0]

        def transpose_store(src, skey, dst_dram, t0):
            for tb in range(TT // 128):
                i = ntr[0] % 2
                ntr[0] += 1
                P.op("pe", lambda e, i=i, tb=tb: e.transpose(ptr[i][:, 0:128], src[:, tb * 128:(tb + 1) * 128], K.ident[:]),
                     reads=[skey, "c_ident"], writes=["r_ptr%d" % i])
                P.op("act", lambda e, i=i: e.copy(out=tro[i][:], in_=ptr[i][:, 0:128]), reads=["r_ptr%d" % i], writes=["r_tro%d" % i])
                P.dma("sp", lambda e, i=i, tb=tb: e.dma_start(out=dst_dram[t0 + tb * 128:t0 + (tb + 1) * 128, :], in_=tro[i][:]),
                      reads=["r_tro%d" % i], writes=["r_scr"])

        for st in range(S // TT):
            t0 = st * TT
            lo, hi = max(t0 - 1, 0), min(t0 + TT + 1, S)
            for rt in range(15):
                b = rt % 2
                k = "r_xr%d" % b
                P.op("pool", lambda e, b=b: e.memset(xr[b][:, 0:1], 0.0), writes=[k])
                P.op("pool", lambda e, b=b: e.memset(xr[b][:, TT + 1:TT + 2], 0.0), writes=[k])
                P.dma("sp", lambda e, b=b, rt=rt, lo=lo, hi=hi, t0=t0: e.dma_start(
                    out=xr[b][:, lo - (t0 - 1):hi - (t0 - 1)], in_=fm[R0 + rt * 128:R0 + (rt + 1) * 128, lo:hi]), reads=["fm"], writes=[k])
                zk = "r_z%d" % rt
                P.op("dve", lambda e, b=b, rt=rt: e.tensor_scalar(out=z[rt][:], in0=xr[b][:, 1:TT + 1], scalar1=mu[:, rt, 2:3], scalar2=None, op0=ALU.mult),
                     reads=[k, "r_mu"], writes=[zk])
                P.op("dve", lambda e, b=b, rt=rt: e.scalar_tensor_tensor(out=z[rt][:], in0=xr[b][:, 0:TT], scalar=mu[:, rt, 0:1], in1=z[rt][:],
                                                                        op0=ALU.mult, op1=ALU.add), reads=[k, zk], writes=[zk])
                P.op("dve", lambda e, b=b, rt=rt: e.scalar_tensor_tensor(out=z[rt][:], in0=xr[b][:, 2:TT + 2], scalar=mu[:, rt, 1:2], in1=z[rt][:],
                                                                        op0=ALU.mult, op1=ALU.add), reads=[k, zk], writes=[zk])
            P.op("act", lambda e: e.activation(out=twb[:], in_=z[12][:], func=AF.Tanh), reads=["r_z12"], writes=["r_twb"])
            P.op("act", lambda e: e.copy(out=adb[:], in_=z[13][:]), reads=["r_z13"], writes=["r_adb"])
            P.op("act", lambda e: e.activation(out=sgb[:], in_=z[14][:], func=AF.Sigmoid), reads=["r_z14"], writes=["r_sgb"])
            for ct in range(4):
                zr, zk_, zv = z[ct], z[4 + ct], z[8 + ct]
                kr, kk_, kv = "r_z%d" % ct, "r_z%d" % (4 + ct), "r_z%d" % (8 + ct)
                cs = slice(ct * 128, (ct + 1) * 128)
                for d in range(2):
                    pi = npm[0] % 4
                    npm[0] += 1
                    P.op("pe", lambda e, pi=pi, d=d, cs=cs: e.matmul(pm[pi][:, 0:TT], lhsT=w2p[d][:, cs], rhs=twb[:], start=True, stop=True),
                         reads=lwk + ["r_twb"], writes=["r_pm%d" % pi])
                    P.op("act", lambda e, pi=pi, d=d, ct=ct: e.activation(out=ld[d][:], in_=pm[pi][:, 0:TT], func=AF.Sigmoid, bias=pc[:, ct, d:d + 1]),
                         reads=["r_pm%d" % pi, "r_pc"], writes=["r_ld%d" % d])
                    P.op("dve", lambda e, d=d: e.tensor_scalar(out=ld[d][:], in0=ld[d][:], scalar1=-0.6065306597126334, scalar2=None, op0=ALU.mult),
                         reads=["r_ld%d" % d], writes=["r_ld%d" % d])
                    pi = npm[0] % 4
                    npm[0] += 1
                    P.op("pe", lambda e, pi=pi, d=d, cs=cs: e.matmul(pm[pi][:, 0:TT], lhsT=a2p[d][:, cs], rhs=adb[:], start=True, stop=True),
                         reads=lwk + ["r_adb"], writes=["r_pm%d" % pi])
                    P.op("act", lambda e, pi=pi, d=d, ct=ct: e.activation(out=av[d][:], in_=pm[pi][:, 0:TT], func=AF.Sigmoid, bias=pc[:, ct, 2 + d:3 + d]),
                         reads=["r_pm%d" % pi, "r_pc"], writes=["r_a%d" % d])
                pi = npm[0] % 4
                npm[0] += 1
                P.op("pe", lambda e, pi=pi, cs=cs: e.matmul(pm[pi][:, 0:TT], lhsT=g2b[:, cs], rhs=sgb[:], start=True, stop=True),
                     reads=lwk + ["r_sgb"], writes=["r_pm%d" % pi])
                P.op("act", lambda e, pi=pi: e.copy(out=gt_[:], in_=pm[pi][:, 0:TT]), reads=["r_pm%d" % pi], writes=["r_g"])
                P.dma("sp", lambda e, cs=cs, t0=t0: e.dma_start(out=K.Sc["rg"][cs, t0:t0 + TT], in_=gt_[:]), reads=["r_g"], writes=["r_scr"])
                P.op("dve", lambda e, ct=ct, zk_=zk_: e.tensor_scalar(out=kq[:], in0=zk_[:], scalar1=pc[:, ct, 4:5], scalar2=None, op0=ALU.mult),
                     reads=[kk_, "r_pc"], writes=["r_kq"])
                P.op("pool", lambda e: e.tensor_tensor(out=t1[:], in0=kq[:], in1=kq[:], op=ALU.mult), reads=["r_kq"], writes=["r_t1"])
                pi = npm[0] % 4
                npm[0] += 1
                P.op("pe", lambda e, pi=pi: e.matmul(pm[pi][:, 0:TT], lhsT=bones[:], rhs=t1[:], start=True, stop=True),
                     reads=["r_bones", "r_t1"], writes=["r_pm%d" % pi])
                P.op("act", lambda e, pi=pi: e.activation(out=t2[:], in_=pm[pi][:, 0:TT], func=AF.Sqrt, bias=tiny[:, 0:1]),
                     reads=["r_pm%d" % pi, "r_tiny"], writes=["r_t2"])
                P.op("dve", lambda e: e.reciprocal(out=t2[:], in_=t2[:]), reads=["r_t2"], writes=["r_t2"])
                P.op("dve", lambda e: e.tensor_tensor(out=kkt[:], in0=kq[:], in1=t2[:], op=ALU.mult), reads=["r_kq", "r_t2"], writes=["r_kk"])
                for d in range(2):
                    P.op("dve", lambda e, d=d, ct=ct: e.tensor_scalar(out=t1[:], in0=av[d][:], scalar1=pc[:, ct, 5:6], scalar2=pc[:, ct, 6:7],
                                                                    op0=ALU.mult, op1=ALU.add), reads=["r_a%d" % d, "r_pc"], writes=["r_t1"])
                    P.op("dve", lambda e, d=d, zk_=zk_: e.tensor_tensor(out=kd[d][:], in0=zk_[:], in1=t1[:], op=ALU.mult), reads=[kk_, "r_t1"], writes=["r_kd%d" % d])
                    P.op("pool", lambda e, d=d: e.tensor_tensor(out=bb[d][:], in0=av[d][:], in1=kkt[:], op=ALU.mult), reads=["r_a%d" % d, "r_kk"], writes=["r_bb%d" % d])
                P.op("pool", lambda e: e.tensor_tensor(out=t1[:], in0=kd[0][:], in1=kd[1][:], op=ALU.add), reads=["r_kd0", "r_kd1", "r_t1"], writes=["r_t1"])
                P.op("dve", lambda e, ct=ct, zr=zr: e.scalar_tensor_tensor(out=t1[:], in0=t1[:], scalar=pc[:, ct, 7:8], in1=zr[:], op0=ALU.mult, op1=ALU.mult),
                     reads=["r_t1", kr, "r_pc"], writes=["r_t1"])
                pi = npm[0] % 4
                npm[0] += 1
                P.op("pe", lambda e, pi=pi: e.matmul(pm[pi][:, 0:TT], lhsT=bones[:], rhs=t1[:], start=True, stop=True),
                     reads=["r_bones", "r_t1"], writes=["r_pm%d" % pi])
                P.op("dve", lambda e, pi=pi, zv=zv: e.tensor_tensor(out=t2[:], in0=pm[pi][:, 0:TT], in1=zv[:], op=ALU.mult), reads=["r_pm%d" % pi, kv], writes=["r_t2"])
                P.dma("sp", lambda e, cs=cs, t0=t0: e.dma_start(out=K.Sc["rbon"][cs, t0:t0 + TT], in_=t2[:]), reads=["r_t2"], writes=["r_scr"])
                transpose_store(zv, kv, K.Sc["rv"][:, cs], t0)
                for d in range(2):
                    for c in range(NCH):
                        P.op("dve", lambda e, c=c, d=d: e.tensor_tensor_scan(out=lam[:, c * CH:(c + 1) * CH], data0=ones[:, 0:CH], data1=ld[d][:, c * CH:(c + 1) * CH],
                                                                             initial=0.0, op0=ALU.mult, op1=ALU.add), reads=["r_ld%d" % d, "r_ones"], writes=["r_lam"])
                    lam3 = lam[:].rearrange("p (c t) -> p c t", t=CH)
                    lamC = lam3[:, :, CH - 1:CH].to_broadcast([128, NCH, CH])
                    L3 = Lt[:].rearrange("p (c t) -> p c t", t=CH)
                    if d == 0:
                        P.op("pool", lambda e: e.tensor_copy(out=Lt[:], in_=lam[:]), reads=["r_lam"], writes=["r_L"])
                    else:
                        P.op("dve", lambda e, L3=L3, lamC=lamC, lam3=lam3: e.tensor_tensor(out=L3, in0=lamC, in1=lam3, op=ALU.subtract), reads=["r_lam"], writes=["r_L"])
                        P.op("dve", lambda e, d=d: e.tensor_tensor(out=Lt[:], in0=Lt[:], in1=ld[d][:], op=ALU.add), reads=["r_L", "r_ld%d" % d], writes=["r_L"])
                    P.op("act", lambda e: e.activation(out=E[0][:], in_=Lt[:], func=AF.Exp, scale=-1.0), reads=["r_L"], writes=["r_E0"])
                    P.op("act", lambda e: e.activation(out=E[2][:], in_=Lt[:], func=AF.Exp), reads=["r_L"], writes=["r_E2"])
                    P.op("dve", lambda e, d=d: e.tensor_tensor(out=t1[:], in0=Lt[:], in1=ld[d][:], op=ALU.subtract), reads=["r_L", "r_ld%d" % d, "r_t1"], writes=["r_t1"])
                    P.op("act", lambda e: e.activation(out=E[1][:], in_=t1[:], func=AF.Exp), reads=["r_t1"], writes=["r_E1"])
                    P.op("dve", lambda e, L3=L3, lamC=lamC: e.tensor_tensor(out=t2[:].rearrange("p (c t) -> p c t", t=CH), in0=lamC, in1=L3, op=ALU.subtract),
                         reads=["r_L", "r_lam", "r_t2"], writes=["r_t2"])
                    P.op("act", lambda e: e.activation(out=E[3][:], in_=t2[:], func=AF.Exp), reads=["r_t2"], writes=["r_E3"])
                    P.op("act", lambda e, lam3=lam3: e.activation(out=gc[:], in_=lam3[:, :, CH - 1], func=AF.Exp), reads=["r_lam"], writes=["r_gc"])
                    P.dma("sp", lambda e, d=d, cs=cs, st=st: e.dma_start(out=K.Sc["rgc"][d][cs, st * NCH:(st + 1) * NCH], in_=gc[:]), reads=["r_gc"], writes=["r_scr"])
                    prods = [(kd[d], "r_kd%d" % d, 0, "dve"), (bb[d], "r_bb%d" % d, 0, "pool"), (kkt, "r_kk", 1, "dve"), (zr, kr, 2, "pool"),
                             (kd[d], "r_kd%d" % d, 3, "dve"), (bb[d], "r_bb%d" % d, 3, "pool")]
                    for oi, (src, skey, ei, eng) in enumerate(prods):
                        P.op(eng, lambda e, oi=oi, src=src, ei=ei: e.tensor_tensor(out=outs[oi][:], in0=src[:], in1=E[ei][:], op=ALU.mult),
                             reads=[skey, "r_E%d" % ei], writes=["r_o%d" % oi])
                    for oi in range(4):
                        P.dma("sp", lambda e, oi=oi, d=d, cs=cs, t0=t0: e.dma_start(out=K.Sc["rf"][d][oi][cs, t0:t0 + TT], in_=outs[oi][:]),
                              reads=["r_o%d" % oi], writes=["r_scr"])
                    transpose_store(outs[4], "r_o4", K.Sc["rt"][d][0][:, cs], t0)
                    transpose_store(outs[5], "r_o5", K.Sc["rt"][d][1][:, cs], t0)


def phase_rwkv_scan(K, l):
    P, S = K.P, K.S
    NC = S // CH
    with Phase(K) as ph:
        msk = {}
        for name, op in (("su", ALU.is_gt), ("iu", ALU.is_ge), ("sl", ALU.is_lt), ("il", ALU.is_le), ("eye", ALU.is_equal)):
            m = ph.sb("k_" + name, [CH, CH])
            P.op("dve", lambda e, m=m, op=op: e.tensor_scalar(out=m[:], in0=K.io[0:CH, 0:CH], scalar1=K.iop[0:CH, 0:1], scalar2=None, op0=op),
                 reads=["c_io", "c_iop"], writes=["k_msk"])
            msk[name] = m
        bc = lambda m: m[:].unsqueeze(1).to_broadcast([CH, 8, CH])
        spad = getattr(K.cfg, "spad", 0)
        PP = 128 if spad else CH

        class PadT:
            def __init__(self, t, flat):
                self.t, self.flat = t, flat

            def __getitem__(self, idx):
                v = self.t[0:CH, 0:8 * CH] if self.flat else self.t[0:CH, 0:8, :]
                return v[idx]

            def L(self, h):
                if self.flat:
                    return self.t[0:PP, h * CH:(h + 2) * CH] if spad else self.t[0:CH, h * CH:(h + 1) * CH]
                return self.t[0:PP, h:h + 2, :] if spad else self.t[0:CH, h, :]

            def R(self, h):
                if self.flat:
                    return self.t[0:PP, h * CH:(h + 1) * CH]
                return self.t[0:PP, h, :]

        def padsb(name, flat=False):
            t = ph.sb(name, [PP, 9 * CH] if flat else [PP, 9, CH])
            P.op("pool", lambda e: e.memset(t[:], 0.0), writes=[name])
            return PadT(t, flat)

        A = [ph.ps("k_A%d" % i, [PP, 8, CH]) for i in range(5)]
        Bp = [ph.ps("k_B%d" % i, [PP, 8, CH]) for i in range(3)]
        ST = [padsb("k_ST%d" % d) for d in range(2)]
        fmt = [[padsb("k_f%d_%d" % (d, i)) for i in range(4)] for d in range(2)]
        tmt = [[padsb("k_t%d_%d" % (d, i), flat=True) for i in range(3)] for d in range(2)]
        gcs = [ph.sb("k_gc%d" % d, [CH, 8]) for d in range(2)]
        Mk = padsb("k_Mk")
        Ak = padsb("k_Ak")
        Abn = padsb("k_Abn")
        Q = [padsb("k_Q%d" % i) for i in range(2)]
        QT = [padsb("k_QT%d" % i) for i in range(2)]
        Tc = [padsb("k_Tc%d" % i) for i in range(2)]
        W = padsb("k_W")
        Un = padsb("k_Un")
        Y = [ph.sb("k_Y%d" % i, [CH, 8 * CH]) for i in range(2)]
        tmpS = ph.sb("k_tmpS", [CH, 8, CH])

        def mm8(out_ps, okey, tl, tr, rkeys):
            for h in range(8):
                P.op("pe", lambda e, h=h: e.matmul(out_ps[0:PP, h, :], lhsT=tl.L(h), rhs=tr.R(h), start=True, stop=True), reads=rkeys, writes=[okey])

        for step in range(NC):
            for d in range(2):
                c = step if d == 0 else NC - 1 - step
                cs = slice(c * CH, (c + 1) * CH)
                mS, mI, mST = (msk["su"], msk["iu"], msk["sl"]) if d == 0 else (msk["sl"], msk["il"], msk["su"])
                f = fmt[d]
                t = tmt[d]
                fk = ["k_f%d_%d" % (d, i) for i in range(4)]
                tk = ["k_t%d_%d" % (d, i) for i in range(3)]
                for i in range(4):
                    P.dma("sp", lambda e, i=i, d=d, cs=cs, f=f: e.dma_start(out=f[i][:], in_=K.Sc["rf"][d][i].rearrange("(h j) s -> j h s", j=CH)[:, :, cs]),
                          reads=["r_scr"], writes=[fk[i]])
                for i in range(2):
                    P.dma("sp", lambda e, i=i, d=d, cs=cs, t=t: e.dma_start(out=t[i][:], in_=K.Sc["rt"][d][i][cs, :]), reads=["r_scr"], writes=[tk[i]])
                P.dma("sp", lambda e, cs=cs, t=t: e.dma_start(out=t[2][:], in_=K.Sc["rv"][cs, :]), reads=["r_scr"], writes=[tk[2]])
                P.dma("sp", lambda e, d=d, c=c: e.dma_start(out=gcs[d][:], in_=K.Sc["rgc"][d].rearrange("(h j) c -> j h c", j=CH)[:, :, c],
                                                                allow_slow_non_contiguous=True), reads=["r_scr"], writes=["k_gc%d" % d])
                Kt, Bt, Qt, Rt = f
                Kh, Bh, V = t
                hs = lambda h: slice(h * CH, (h + 1) * CH)
                mm8(A[0], "k_A0", Kt, Qt, [fk[0], fk[2]])
                mm8(A[1], "k_A1", Bt, Qt, [fk[1], fk[2]])
                mm8(A[2], "k_A2", Qt, Bt, [fk[1], fk[2]])
                mm8(A[3], "k_A3", Kt, Rt, [fk[0], fk[3]])
                mm8(A[4], "k_A4", Bt, Rt, [fk[1], fk[3]])
                P.op("dve", lambda e, mS=mS: e.tensor_tensor(out=Mk[:], in0=A[0][0:CH], in1=bc(mS), op=ALU.mult), reads=["k_A0", "k_msk"], writes=["k_Mk"])
                P.op("dve", lambda e, mS=mS: e.tensor_tensor(out=Q[0][:], in0=A[1][0:CH], in1=bc(mS), op=ALU.mult), reads=["k_A1", "k_msk"], writes=["k_Q0"])
                P.op("dve", lambda e, mST=mST: e.tensor_tensor(out=QT[0][:], in0=A[2][0:CH], in1=bc(mST), op=ALU.mult), reads=["k_A2", "k_msk"], writes=["k_QT0"])
                P.op("dve", lambda e, mI=mI: e.tensor_tensor(out=Ak[:], in0=A[3][0:CH], in1=bc(mI), op=ALU.mult), reads=["k_A3", "k_msk"], writes=["k_Ak"])
                P.op("dve", lambda e, mI=mI: e.tensor_tensor(out=Abn[:], in0=A[4][0:CH], in1=bc(mI), op=ALU.mult),
                     reads=["k_A4", "k_msk"], writes=["k_Abn"])
                P.op("pool", lambda e: e.tensor_tensor(out=Tc[0][:], in0=bc(msk["eye"]), in1=Q[0][:], op=ALU.subtract), reads=["k_Q0", "k_msk"], writes=["k_Tc0"])
                qi, ti = 0, 0
                for lev in range(1, 6):
                    qn = 1 - qi
                    if lev < 5:
                        mm8(A[0], "k_A0", QT[qi], Q[qi], ["k_Q%d" % qi, "k_QT%d" % qi])
                    mm8(A[1], "k_A1", Q[qi], QT[qi], ["k_Q%d" % qi, "k_QT%d" % qi])
                    if lev < 5:
                        P.op("act", lambda e, qn=qn: e.copy(out=Q[qn][:], in_=A[0][0:CH]), reads=["k_A0"], writes=["k_Q%d" % qn])
                    P.op("dve", lambda e, qn=qn: e.tensor_copy(out=QT[qn][:], in_=A[1][0:CH]), reads=["k_A1"], writes=["k_QT%d" % qn])
                    tn = 1 - ti
                    mm8(A[2], "k_A2", QT[qn], Tc[ti], ["k_QT%d" % qn, "k_Tc%d" % ti])
                    P.op("dve", lambda e, tn=tn, ti=ti: e.tensor_tensor(out=Tc[tn][:], in0=A[2][0:CH], in1=Tc[ti][:], op=ALU.add),
                         reads=["k_A2", "k_Tc%d" % ti], writes=["k_Tc%d" % tn])
                    qi, ti = qn, tn
                Tf, Tfk = Tc[ti], "k_Tc%d" % ti
                sk = "k_ST%d" % d
                for h in range(8):
                    P.op("pe", lambda e, h=h, d=d, Qt=Qt: e.matmul(Bp[0][0:PP, h, :], lhsT=Qt.L(h), rhs=ST[d].R(h), start=True, stop=False),
                         reads=[fk[2], sk], writes=["k_B0"])
                    P.op("pe", lambda e, h=h, V=V: e.matmul(Bp[0][0:PP, h, :], lhsT=Mk.L(h), rhs=V.R(h), start=False, stop=True),
                         reads=["k_Mk", tk[2]], writes=["k_B0"])
                P.op("act", lambda e: e.copy(out=W[:], in_=Bp[0][0:CH]), reads=["k_B0"], writes=["k_W"])
                mm8(Bp[0], "k_B0", Tf, W, [Tfk, "k_W"])
                P.op("act", lambda e: e.activation(out=Un[:], in_=Bp[0][0:CH], func=AF.Copy, scale=-1.0), reads=["k_B0"], writes=["k_Un"])
                for h in range(8):
                    P.op("pe", lambda e, h=h, Kh=Kh, V=V: e.matmul(Bp[1][0:PP, h, :], lhsT=Kh.L(h), rhs=V.R(h), start=True, stop=False),
                         reads=[tk[0], tk[2]], writes=["k_B1"])
                    P.op("pe", lambda e, h=h, Bh=Bh: e.matmul(Bp[1][0:PP, h, :], lhsT=Bh.L(h), rhs=Un.R(h), start=False, stop=True),
                         reads=[tk[1], "k_Un"], writes=["k_B1"])
                for h in range(8):
                    P.op("pe", lambda e, h=h, d=d, Rt=Rt: e.matmul(Bp[2][0:PP, h, :], lhsT=Rt.L(h), rhs=ST[d].R(h), start=True, stop=False),
                         reads=[fk[3], sk], writes=["k_B2"])
                    P.op("pe", lambda e, h=h, V=V: e.matmul(Bp[2][0:PP, h, :], lhsT=Ak.L(h), rhs=V.R(h), start=False, stop=False),
                         reads=["k_Ak", tk[2]], writes=["k_B2"])
                    P.op("pe", lambda e, h=h: e.matmul(Bp[2][0:PP, h, :], lhsT=Abn.L(h), rhs=Un.R(h), start=False, stop=True),
                         reads=["k_Abn", "k_Un"], writes=["k_B2"])
                yi = (step * 2 + d) % 2
                P.op("act", lambda e, yi=yi: e.copy(out=Y[yi][:], in_=Bp[2][0:CH].rearrange("p h i -> p (h i)")), reads=["k_B2"], writes=["k_Y%d" % yi])
                P.dma("sp", lambda e, yi=yi, d=d, cs=cs: e.dma_start(out=K.Sc["ry"][d][cs, :], in_=Y[yi][:]), reads=["k_Y%d" % yi], writes=["r_y"])
                P.op("dve", lambda e, d=d: e.tensor_tensor(out=tmpS[:], in0=ST[d][:], in1=gcs[d][:].unsqueeze(2).to_broadcast([CH, 8, CH]), op=ALU.mult),
                     reads=[sk, "k_gc%d" % d], writes=["k_tmpS"])
                P.op("dve", lambda e, d=d: e.tensor_tensor(out=ST[d][:], in0=tmpS[:], in1=Bp[1][0:CH], op=ALU.add), reads=["k_tmpS", "k_B1"], writes=[sk])


def phase_rwkv_post(K, l):
    P, I, S = K.P, K.I, K.S
    TT = min(512, S)
    with Phase(K) as ph:
        eps_t = ph.sb("q_eps", [128, 1])
        P.op("dve", lambda e: e.memset(eps_t[:], GN_EPS), writes=["q_eps"])
        lnp = ph.sb("q_lnp", [128, 4, 2])
        for ci, name in enumerate(("rwkv_ln_w", "rwkv_ln_b")):
            for ct in range(4):
                P.dma("sp", lambda e, ci=ci, ct=ct, name=name: e.dma_start(out=lnp[:, ct, ci:ci + 1],
                                                                         in_=I[name][l:l + 1, ct * 128:(ct + 1) * 128].rearrange("o p -> p o")), writes=["q_lnp"])
        y0 = [ph.sb("q_y0%d" % i, [128, 512]) for i in range(2)]
        y1 = [ph.sb("q_y1%d" % i, [128, 512]) for i in range(2)]
        sq = ph.sb("q_sq", [128, 512])
        mean = ph.sb("q_mean", [128, 8])
        var = ph.sb("q_var", [128, 8])
        bon = [ph.sb("q_bon%d" % i, [128, TT]) for i in range(2)]
        gg = [ph.sb("q_g%d" % i, [128, TT]) for i in range(2)]
        o1 = [ph.sb("q_o1%d" % i, [128, TT]) for i in range(2)]
        ob = [ph.sb("q_ob%d" % i, [128, TT], BF16) for i in range(2)]
        pT = [ph.ps("q_pT%d" % i, [128, 512]) for i in range(4)]
        for st in range(S // TT):
            t0 = st * TT
            for tb in range(TT // 128):
                b = tb % 2
                r0 = t0 + tb * 128
                P.dma("sp", lambda e, b=b, r0=r0: e.dma_start(out=y0[b][:], in_=K.Sc["ry"][0][r0:r0 + 128, :]), reads=["r_y"], writes=["q_y0%d" % b])
                P.dma("sp", lambda e, b=b, r0=r0: e.dma_start(out=y1[b][:], in_=K.Sc["ry"][1][r0:r0 + 128, :]), reads=["r_y"], writes=["q_y1%d" % b])
                yk = "q_y0%d" % b
                y3 = y0[b][:].rearrange("p (h i) -> p h i", h=8)
                P.op("pool", lambda e, b=b: e.tensor_tensor(out=y0[b][:], in0=y0[b][:], in1=y1[b][:], op=ALU.add), reads=[yk, "q_y1%d" % b], writes=[yk])
                P.op("dve", lambda e, y3=y3: e.tensor_reduce(out=mean[:], in_=y3, axis=AX.X, op=ALU.add), reads=[yk], writes=["q_mean"])
                P.op("dve", lambda e: e.tensor_scalar(out=mean[:], in0=mean[:], scalar1=1.0 / 64, scalar2=None, op0=ALU.mult), reads=["q_mean"], writes=["q_mean"])
                P.op("dve", lambda e, y3=y3: e.tensor_tensor(out=y3, in0=y3, in1=mean[:].unsqueeze(2).to_broadcast([128, 8, 64]), op=ALU.subtract),
                     reads=[yk, "q_mean"], writes=[yk])
                P.op("pool", lambda e, b=b: e.tensor_tensor(out=sq[:], in0=y0[b][:], in1=y0[b][:], op=ALU.mult), reads=[yk], writes=["q_sq"])
                P.op("dve", lambda e: e.tensor_reduce(out=var[:], in_=sq[:].rearrange("p (h i) -> p h i", h=8), axis=AX.X, op=ALU.add), reads=["q_sq"], writes=["q_var"])
                P.op("act", lambda e: e.activation(out=var[:], in_=var[:], func=AF.Sqrt, scale=1.0 / 64, bias=eps_t[:, 0:1]), reads=["q_var", "q_eps"], writes=["q_var"])
                P.op("dve", lambda e: e.reciprocal(out=var[:], in_=var[:]), reads=["q_var"], writes=["q_var"])
                P.op("dve", lambda e, y3=y3: e.tensor_tensor(out=y3, in0=y3, in1=var[:].unsqueeze(2).to_broadcast([128, 8, 64]), op=ALU.mult),
                     reads=[yk, "q_var"], writes=[yk])
                for ct in range(4):
                    P.op("pe", lambda e, ct=ct, b=b, tb=tb: e.transpose(pT[ct][:, tb * 128:(tb + 1) * 128], y0[b][:, ct * 128:(ct + 1) * 128], K.ident[:]),
                         reads=[yk, "c_ident"], writes=["q_pT%d" % ct])
            for ct in range(4):
                b = ct % 2
                cs = slice(ct * 128, (ct + 1) * 128)
                P.dma("sp", lambda e, b=b, cs=cs, t0=t0: e.dma_start(out=bon[b][:], in_=K.Sc["rbon"][cs, t0:t0 + TT]), reads=["r_scr"], writes=["q_bon%d" % b])
                P.dma("sp", lambda e, b=b, cs=cs, t0=t0: e.dma_start(out=gg[b][:], in_=K.Sc["rg"][cs, t0:t0 + TT]), reads=["r_scr"], writes=["q_g%d" % b])
                P.op("dve", lambda e, b=b, ct=ct: e.tensor_scalar(out=o1[b][:], in0=pT[ct][:, 0:TT], scalar1=lnp[:, ct, 0:1], scalar2=lnp[:, ct, 1:2],
                                                                op0=ALU.mult, op1=ALU.add), reads=["q_pT%d" % ct, "q_lnp"], writes=["q_o1%d" % b])
                P.op("pool", lambda e, b=b: e.tensor_tensor(out=o1[b][:], in0=o1[b][:], in1=bon[b][:], op=ALU.add), reads=["q_o1%d" % b, "q_bon%d" % b], writes=["q_o1%d" % b])
                P.op("dve", lambda e, b=b: e.tensor_tensor(out=ob[b][:], in0=o1[b][:], in1=gg[b][:], op=ALU.mult), reads=["q_o1%d" % b, "q_g%d" % b], writes=["q_ob%d" % b])
                P.dma("sp", lambda e, b=b, ct=ct, t0=t0: e.dma_start(out=K.Sc["br"][1024 + ct * 128:1024 + (ct + 1) * 128, t0:t0 + TT], in_=ob[b][:]),
                      reads=["q_ob%d" % b], writes=["br"])


def rope_tables(S):
    t = np.arange(S)
    row = (t // 64).astype(np.float32)
    col = (t % 64).astype(np.float32)
    outs = []
    for n_pairs in (32, 16):
        n_axis = n_pairs // 2
        freqs = np.power(np.float32(10000.0), -np.arange(n_axis, dtype=np.float32) / n_axis).astype(np.float32)
        ang = np.concatenate([row[:, None] * freqs, col[:, None] * freqs], axis=-1).astype(np.float32)
        outs.append(np.concatenate([np.cos(ang), np.sin(ang)], axis=-1).astype(np.float32))
    return outs[0], outs[1]


_NC_CACHE = {}


def kernel(**inputs):
    x = np.ascontiguousarray(np.asarray(inputs["x"], dtype=np.float32))
    B, S, _ = x.shape
    L = int(np.asarray(inputs["w_in"]).shape[0])
    key = (S, L)
    if key not in _NC_CACHE:
        _NC_CACHE[key] = build(Cfg(S=S, L=L))
    nc = _NC_CACHE[key]
    f32 = lambda a: np.ascontiguousarray(np.asarray(a, dtype=np.float32))
    shared = {}
    for name in ("w_mod", "norm1_g", "norm2_g", "w_in", "conv_w", "gqa_q_norm", "gqa_k_norm", "rwkv_mu", "rwkv_w0", "rwkv_a0",
                 "rwkv_g2", "rwkv_k_k", "rwkv_k_a", "rwkv_ln_w", "rwkv_ln_b", "mla_qc_norm", "mla_kvc_norm", "mla_w_uq",
                 "mla_w_ukv", "mla_q_norm", "mla_k_norm", "w_out", "w_router", "moe_w1", "moe_w3", "moe_w2"):
        shared[name] = f32(inputs[name])
    shared["mod_table"] = f32(inputs["mod_table"]).reshape(L, 6 * D)
    shared["rwkv_w2"] = f32(inputs["rwkv_w2"]).reshape(L, 128, 512)
    shared["rwkv_a2"] = f32(inputs["rwkv_a2"]).reshape(L, 128, 512)
    shared["rwkv_r_k"] = f32(inputs["rwkv_r_k"]).reshape(L, 512)
    shared["w_branch"] = f32(inputs["w_branch"]).reshape(L, 4 * 512, D)
    rg, rm = rope_tables(S)
    shared["rope_g"], shared["rope_m"] = rg, rm
    c = f32(inputs["c"])
    in_maps = []
    for b in range(B):
        m = dict(shared)
        m["x"] = x[b]
        m["c"] = c[b]
        in_maps.append(m)
    res = run_bass_kernel_spmd(nc, in_maps, core_ids=list(range(B)))
    return np.stack([np.asarray(r["out"], dtype=np.float32) for r in res.results], axis=0)
```
